# Optimizing a Trainium2 kernel written in Bass

```python
import jax, jax.numpy as jnp
from jax import lax
import numpy as np

D_MODEL = 1024
BATCH = 4
SEQ = 8192
DEPTH = 2

HEAD_DIM = 64
MOBA_HEADS = 6
MOBA_BLOCK = 256
MOBA_TOPK = 3
MOBA_QBLOCK = 64
CONV_CH = 256
CONV_WIDTH = 31
DSA_HEADS = 6
DSA_TOPK = 256
DSA_QBLOCK = 128
IDX_HEADS = 8
IDX_DIM = 64
MOBA_W = 384
DSA_W = 384
MIX_W = 1024
SPLIT_SIZES = (384, 384, 384, 256, 256, 384, 384, 384, 512, 64, 8)
N_IN = 3400
N_GROUPS = 4
EXPERTS_PER_GROUP = 8
N_EXPERTS = 32
EXPERT_TOPK = 2
D_EXPERT = 512
MOE_BLOCK = 256
EPS = 1e-6

kernel_name = "hymba_moba_conformer_dsa_hmoe_adaln"


def rms_norm(x, g):
    xf = x.astype(jnp.float32)
    y = xf * lax.rsqrt(jnp.mean(xf * xf, axis=-1, keepdims=True) + EPS)
    return (y * g.astype(jnp.float32)).astype(x.dtype)


def layer_norm(x, g, b):
    xf = x.astype(jnp.float32)
    mu = jnp.mean(xf, axis=-1, keepdims=True)
    xc = xf - mu
    y = xc * lax.rsqrt(jnp.mean(xc * xc, axis=-1, keepdims=True) + EPS)
    return (y * g.astype(jnp.float32) + b.astype(jnp.float32)).astype(x.dtype)


def moba_attention(q, k, v):
    B, S, H, Dh = q.shape
    nb = -(-S // MOBA_BLOCK)
    pad = nb * MOBA_BLOCK - S
    to_blocks = lambda t: jnp.pad(t, ((0, 0), (0, pad), (0, 0), (0, 0))).reshape(
        B, nb, MOBA_BLOCK, H, Dh).transpose(0, 3, 1, 2, 4)
    kb, vb = to_blocks(k), to_blocks(v)
    k_mean = jnp.mean(kb.astype(jnp.float32), axis=3).astype(q.dtype)
    topb = min(MOBA_TOPK, nb)
    n_sel = topb * MOBA_BLOCK
    nq = S // MOBA_QBLOCK
    QB = MOBA_QBLOCK
    scale = HEAD_DIM ** -0.5
    gather_blocks = jax.vmap(jax.vmap(lambda blocks, idx: blocks[idx]))
    qs = q.reshape(B, nq, QB, H, Dh).transpose(1, 0, 3, 2, 4)

    def one_block(args):
        qb, i = args
        t = i * QB + jnp.arange(QB)
        own = (i * QB) // MOBA_BLOCK
        gate = jnp.einsum('bhqd,bhnd->bhqn', qb, k_mean).astype(jnp.float32)
        gate = jnp.where(jnp.arange(nb) < own, gate, -jnp.inf)
        _, sel = lax.top_k(gate, topb)
        k_sel = gather_blocks(kb, sel)
        v_sel = gather_blocks(vb, sel)
        k_own = lax.dynamic_index_in_dim(kb, own, axis=2, keepdims=False)
        v_own = lax.dynamic_index_in_dim(vb, own, axis=2, keepdims=False)
        s_sel = jnp.einsum('bhqd,bhqjkd->bhqjk', qb, k_sel).astype(jnp.float32) * scale
        s_sel = jnp.where((jnp.arange(topb) < own)[:, None], s_sel, -jnp.inf)
        s_own = jnp.einsum('bhqd,bhkd->bhqk', qb, k_own).astype(jnp.float32) * scale
        kpos = own * MOBA_BLOCK + jnp.arange(MOBA_BLOCK)
        s_own = jnp.where(kpos[None, :] <= t[:, None], s_own, -jnp.inf)
        p = jax.nn.softmax(jnp.concatenate([s_sel.reshape(B, H, QB, n_sel), s_own], axis=-1),
                           axis=-1).astype(v.dtype)
        out = (jnp.einsum('bhqjk,bhqjkd->bhqd', p[..., :n_sel].reshape(B, H, QB, topb, MOBA_BLOCK), v_sel)
               + jnp.einsum('bhqk,bhkd->bhqd', p[..., n_sel:], v_own))
        return out

    out = lax.map(one_block, (qs, jnp.arange(nq)))
    return out.transpose(1, 0, 3, 2, 4).reshape(B, S, H * Dh)


def conformer_conv(a, g, conv_w, conv_b, ln_g, ln_b):
    u = a * jax.nn.sigmoid(g)
    u = jnp.pad(u, ((0, 0), (CONV_WIDTH - 1, 0), (0, 0)))
    y = lax.conv_general_dilated(u, conv_w[:, None, :], window_strides=(1,), padding='VALID',
                                 dimension_numbers=('NWC', 'WIO', 'NWC'),
                                 feature_group_count=CONV_CH) + conv_b
    return jax.nn.silu(layer_norm(y, ln_g, ln_b))


def dsa_attention(q, k, v, q_idx, k_idx, w_idx):
    B, S, H, Dh = q.shape
    topk = min(DSA_TOPK, S // 4)
    QB = DSA_QBLOCK
    nq = S // QB
    k_idx32 = k_idx.astype(jnp.float32)
    w32 = w_idx.astype(jnp.float32) * (IDX_HEADS ** -0.5 * IDX_DIM ** -0.5)
    split = lambda t: t.reshape(B, nq, QB, *t.shape[2:]).swapaxes(0, 1)
    gather_keys = jax.vmap(lambda kv, idx: kv[idx])
    kpos = jnp.arange(S)

    def one_block(args):
        qb, qib, wb, i = args
        t = i * QB + jnp.arange(QB)
        score = jnp.einsum('bqh,bqhs->bqs', wb,
                           jax.nn.relu(jnp.einsum('bqhd,bsd->bqhs', qib.astype(jnp.float32), k_idx32)))
        score = jnp.where(kpos[None, :] <= t[:, None], score, -jnp.inf)
        _, idx = lax.top_k(score, topk)
        valid = idx <= t[None, :, None]
        k_sel = gather_keys(k, idx)
        v_sel = gather_keys(v, idx)
        s = jnp.einsum('bqhd,bqkhd->bhqk', qb, k_sel).astype(jnp.float32) * HEAD_DIM ** -0.5
        s = jnp.where(valid[:, None], s, -jnp.inf)
        p = jax.nn.softmax(s, axis=-1).astype(v.dtype)
        return jnp.einsum('bhqk,bqkhd->bqhd', p, v_sel)

    out = lax.map(one_block, (split(q), split(q_idx), split(w32), jnp.arange(nq)))
    return out.swapaxes(0, 1).reshape(B, S, H * Dh)


def hier_moe(h, rg_w, rg_b, re_w, re_b, w1, w3, w2):
    T, D = h.shape
    g_logits = (h @ rg_w + rg_b).astype(jnp.float32)
    g_idx = jnp.argmax(g_logits, axis=-1)
    g_w = jnp.max(jax.nn.softmax(g_logits, axis=-1), axis=-1)
    e_all = (h @ re_w + re_b).astype(jnp.float32).reshape(T, N_GROUPS, EXPERTS_PER_GROUP)
    e_logits = jnp.take_along_axis(e_all, g_idx[:, None, None], axis=1)[:, 0]
    e_top, e_loc = lax.top_k(e_logits, EXPERT_TOPK)
    gates = g_w[:, None] * jax.nn.softmax(e_top, axis=-1)
    expert = g_idx[:, None] * EXPERTS_PER_GROUP + e_loc
    n_slots = T * EXPERT_TOPK
    flat_e = expert.reshape(-1)
    order = jnp.argsort(flat_e)
    sorted_e = flat_e[order]
    counts = jnp.bincount(flat_e, length=N_EXPERTS)
    padded = (counts + MOE_BLOCK - 1) // MOE_BLOCK * MOE_BLOCK
    pad_end = jnp.cumsum(padded)
    pad_start = pad_end - padded
    start = jnp.cumsum(counts) - counts
    dest = pad_start[sorted_e] + jnp.arange(n_slots) - start[sorted_e]
    P = n_slots + N_EXPERTS * MOE_BLOCK
    nblk = P // MOE_BLOCK
    buf = jnp.zeros((P, D), h.dtype).at[dest].set(h[order // EXPERT_TOPK])
    blk_e = jnp.minimum(jnp.searchsorted(pad_end, jnp.arange(nblk) * MOE_BLOCK, side='right'),
                        N_EXPERTS - 1)

    def expert_block(args):
        xb, e = args
        return (jax.nn.silu(xb @ w1[e]) * (xb @ w3[e])) @ w2[e]

    yb = lax.map(expert_block, (buf.reshape(nblk, MOE_BLOCK, D), blk_e)).reshape(P, D)
    y_slots = jnp.zeros((n_slots, D), h.dtype).at[order].set(yb[dest])
    return jnp.sum(y_slots.reshape(T, EXPERT_TOPK, D) * gates[..., None].astype(h.dtype), axis=1)


def hybrid_layer(x, c, ada_w, ada_b, n1g, w_in, conv_w, conv_b, ln_g, ln_b, mg, dg, w_out,
                 n2g, rgw, rgb, rew, reb, w1, w3, w2):
    B, S, D = x.shape
    mod = jax.nn.silu(c) @ ada_w + ada_b
    sh1, sc1, g1, sh2, sc2, g2 = [m[:, None, :] for m in jnp.split(mod, 6, axis=-1)]
    h = rms_norm(x, n1g) * (1 + sc1) + sh1
    proj = h @ w_in
    qm, km, vm, ca, cg, qd, kd, vd, qi, ki, wi = jnp.split(
        proj, np.cumsum(SPLIT_SIZES)[:-1].tolist(), axis=-1)
    heads = lambda t, n: t.reshape(B, S, n, HEAD_DIM)
    y_moba = moba_attention(heads(qm, MOBA_HEADS), heads(km, MOBA_HEADS), heads(vm, MOBA_HEADS))
    y_conv = conformer_conv(ca, cg, conv_w, conv_b, ln_g, ln_b)
    y_dsa = dsa_attention(heads(qd, DSA_HEADS), heads(kd, DSA_HEADS), heads(vd, DSA_HEADS),
                          qi.reshape(B, S, IDX_HEADS, IDX_DIM), ki, wi)
    mix = jnp.concatenate([rms_norm(y_moba, mg), y_conv, rms_norm(y_dsa, dg)], axis=-1)
    x = x + g1 * (mix @ w_out)
    h2 = rms_norm(x, n2g) * (1 + sc2) + sh2
    y = hier_moe(h2.reshape(B * S, D), rgw, rgb, rew, reb, w1, w3, w2).reshape(B, S, D)
    return x + g2 * y


def setup_inputs(seed: int = 0) -> dict:
    key = jax.random.key(seed)
    ks = jax.random.split(key, 24)
    L, D = DEPTH, D_MODEL
    nrm = lambda k, shape, s: jax.random.normal(k, shape, jnp.float32) * s
    return {
        "x": nrm(ks[0], (BATCH, SEQ, D), 1.0),
        "c": nrm(ks[1], (BATCH, D), 1.0),
        "ada_w": nrm(ks[2], (L, D, 6 * D), 0.5 * D ** -0.5),
        "ada_b": nrm(ks[3], (L, 6 * D), 0.01),
        "norm1_g": 1.0 + nrm(ks[4], (L, D), 0.01),
        "w_in": nrm(ks[5], (L, D, N_IN), D ** -0.5),
        "conv_w": nrm(ks[6], (L, CONV_WIDTH, CONV_CH), CONV_WIDTH ** -0.5),
        "conv_b": nrm(ks[7], (L, CONV_CH), 0.01),
        "conv_ln_g": 1.0 + nrm(ks[8], (L, CONV_CH), 0.01),
        "conv_ln_b": nrm(ks[9], (L, CONV_CH), 0.01),
        "moba_norm_g": 1.0 + nrm(ks[10], (L, MOBA_W), 0.01),
        "dsa_norm_g": 1.0 + nrm(ks[11], (L, DSA_W), 0.01),
        "w_out": nrm(ks[12], (L, MIX_W, D), MIX_W ** -0.5),
        "norm2_g": 1.0 + nrm(ks[13], (L, D), 0.01),
        "router_group_w": nrm(ks[14], (L, D, N_GROUPS), D ** -0.5),
        "router_group_b": nrm(ks[15], (L, N_GROUPS), 0.01),
        "router_expert_w": nrm(ks[16], (L, D, N_EXPERTS), D ** -0.5),
        "router_expert_b": nrm(ks[17], (L, N_EXPERTS), 0.01),
        "expert_w1": nrm(ks[18], (L, N_EXPERTS, D, D_EXPERT), D ** -0.5),
        "expert_w3": nrm(ks[19], (L, N_EXPERTS, D, D_EXPERT), D ** -0.5),
        "expert_w2": nrm(ks[20], (L, N_EXPERTS, D_EXPERT, D), D_EXPERT ** -0.5),
        "final_g": 1.0 + nrm(ks[21], (D,), 0.01),
    }


def reference(x, c, ada_w, ada_b, norm1_g, w_in, conv_w, conv_b, conv_ln_g, conv_ln_b,
              moba_norm_g, dsa_norm_g, w_out, norm2_g, router_group_w, router_group_b,
              router_expert_w, router_expert_b, expert_w1, expert_w3, expert_w2, final_g):
    for l in range(DEPTH):
        x = hybrid_layer(x, c, ada_w[l], ada_b[l], norm1_g[l], w_in[l], conv_w[l], conv_b[l],
                         conv_ln_g[l], conv_ln_b[l], moba_norm_g[l], dsa_norm_g[l], w_out[l],
                         norm2_g[l], router_group_w[l], router_group_b[l], router_expert_w[l],
                         router_expert_b[l], expert_w1[l], expert_w3[l], expert_w2[l])
    return rms_norm(x, final_g)
```

```python
import types
import numpy as np
from contextlib import ExitStack
import concourse.bass as bass
import concourse.mybir as mybir
from concourse.bass_utils import run_bass_kernel_spmd

F32 = mybir.dt.float32
BF16 = mybir.dt.bfloat16
I32 = mybir.dt.int32
ALU = mybir.AluOpType
AF = mybir.ActivationFunctionType
AX = mybir.AxisListType

NDMASEM = 8
D = 1024
KC = 8
NEG = -30000.0
NCW = 3400 + 390 + 390
NIT = 10


def _freeze(fn):
    if fn.__closure__ is None:
        return fn
    cells = []
    for c in fn.__closure__:
        try:
            cells.append(types.CellType(c.cell_contents))
        except ValueError:
            cells.append(c)
    g = types.FunctionType(fn.__code__, fn.__globals__, fn.__name__, fn.__defaults__, tuple(cells))
    g.__kwdefaults__ = fn.__kwdefaults__
    return g


class Prog:
    ENGS = ("pe", "act", "dve", "pool", "sp")

    def __init__(self, nc):
        self.nc = nc
        self.ops = []
        self.state = {}
        self.es = ExitStack()
        self.ph = []
        self.pending = {e: None for e in self.ENGS}
        self.lastc = {e: None for e in self.ENGS}
        self.lastd = {}
        self.dcount = {e: 0 for e in self.ENGS}
        self.uid = 0

    def sb(self, name, shape, dt, glob=False):
        self.uid += 1
        st = self.es if (glob or not self.ph) else self.ph[-1]
        return st.enter_context(self.nc.sbuf_tensor("%s_%d" % (name, self.uid), list(shape), dt))

    def ps(self, name, shape, dt):
        self.uid += 1
        st = self.es if not self.ph else self.ph[-1]
        return st.enter_context(self.nc.psum_tensor("%s_%d" % (name, self.uid), list(shape), dt))

    def phase_begin(self):
        self.ph.append(ExitStack())

    def phase_end(self):
        fence = set()
        for e in self.ENGS:
            if self.lastc[e] is not None:
                fence.add(self.lastc[e])
        fence.update(self.lastd.values())
        for e in self.ENGS:
            self.pending[e] = set(fence) | (self.pending[e] or set())
        self.ph.pop().close()

    def op(self, eng, fn, reads=(), writes=(), dma=False):
        i = len(self.ops)
        deps = set()
        rawset = set()
        for r in reads:
            st = self.state.setdefault(r, [None, []])
            if st[0] is not None:
                deps.add(st[0])
                rawset.add(st[0])
        for w in writes:
            st = self.state.setdefault(w, [None, []])
            if st[0] is not None:
                deps.add(st[0])
            deps.update(st[1])
        for r in reads:
            self.state[r][1].append(i)
        for w in writes:
            st = self.state[w]
            st[0] = i
            st[1] = []
        if self.pending[eng]:
            deps |= self.pending[eng]
            rawset |= self.pending[eng]
            self.pending[eng] = None
        deps.discard(i)
        if dma:
            n = self.dcount[eng]
            self.dcount[eng] += 1
            self.lastd[(eng, n % NDMASEM)] = i
        else:
            self.lastc[eng] = i
        self.ops.append(dict(eng=eng, fn=_freeze(fn), deps=deps, raw=rawset, dma=dma, sig=dma))
        return i

    def dma(self, eng, out, in_, reads=(), writes=(), **kw):
        return self.op(eng, lambda e: e.dma_start(out=out, in_=in_, **kw), reads, writes, dma=True)

    def emit(self):
        nc = self.nc
        ops = self.ops
        for i, o in enumerate(ops):
            keep = set()
            for j in o["deps"]:
                pj = ops[j]
                if (not pj["dma"]) and (not o["dma"]) and pj["eng"] == o["eng"]:
                    if o["eng"] == "pe":
                        continue
                keep.add(j)
            o["deps"] = keep
        for o in ops:
            for j in o["deps"]:
                ops[j]["sig"] = True
        LIM = 30000
        DLIM = 1800
        cnt = {e: 0 for e in self.ENGS}
        dcnt = {e: 0 for e in self.ENGS}
        dsemcnt = {}
        dprev = {}
        for o in ops:
            e = o["eng"]
            if o["dma"]:
                n = dcnt[e]
                dcnt[e] += 1
                slot = n % NDMASEM
                k = dsemcnt.get((e, slot), 0)
                dsemcnt[(e, slot)] = k + 1
                o["prev"] = dprev.get((e, slot))
                o["semkey"] = ("d", e, slot, k // DLIM)
                o["semval"] = 16 * (k % DLIM + 1)
                dprev[(e, slot)] = (o["semkey"], o["semval"])
            elif o["sig"]:
                n = cnt[e]
                cnt[e] += 1
                o["semkey"] = ("c", e, n // LIM)
                o["semval"] = n % LIM + 1
        es = self.es
        sems = {}
        finals = {}
        for o in ops:
            if "semkey" in o:
                k = o["semkey"]
                if k not in sems:
                    sems[k] = es.enter_context(nc.semaphore("q_" + "_".join(str(x) for x in k)))
                finals[k] = max(finals.get(k, 0), o["semval"])
        self.nsems = len(sems)
        byeng = {e: [o for o in ops if o["eng"] == e] for e in self.ENGS}
        self.stats = {e: len(byeng[e]) for e in self.ENGS}

        def run(eng_name, engobj):
            waited = {}
            for o in byeng[eng_name]:
                need = {}
                for j in o["deps"]:
                    pj = ops[j]
                    k, v = pj["semkey"], pj["semval"]
                    if v > need.get(k, 0):
                        need[k] = v
                if o["dma"] and o["prev"] is not None:
                    k, v = o["prev"]
                    if v > need.get(k, 0):
                        need[k] = v
                for k, v in need.items():
                    if v > waited.get(k, 0):
                        engobj.wait_ge(sems[k], v)
                        waited[k] = v
                ins = o["fn"](engobj)
                if o["sig"]:
                    ins.then_inc(sems[o["semkey"]], 16 if o["dma"] else 1)
            if eng_name == "sp":
                for k, v in finals.items():
                    if v > waited.get(k, 0) and v > 0:
                        engobj.wait_ge(sems[k], v)

        with nc.Block() as block:
            @block.tensor
            def _(e):
                run("pe", e)

            @block.scalar
            def _(e):
                run("act", e)

            @block.vector
            def _(e):
                run("dve", e)

            @block.gpsimd
            def _(e):
                run("pool", e)

            @block.sync
            def _(e):
                run("sp", e)
        es.close()


class Rot:
    def __init__(self, items):
        self.items = list(items)
        self.i = 0

    def next(self):
        it = self.items[self.i % len(self.items)]
        self.i += 1
        return it


FM_UNITS = [("qm", 0, 3, 128), ("km", 384, 3, 128), ("qd", 1664, 3, 128), ("kd", 2048, 3, 128),
            ("qi", 2816, 8, 64), ("ki", 3328, 1, 64)]
GLU_A = 1152
GLU_G = 1408


def build(S, NL, dbg=()):
    NT = S // 128
    NG = S // 512
    NB = S // 256
    nc = bass.Bass("TRN2", target_bir_lowering=False)

    def din(name, shape, dt=F32):
        return nc.dram_tensor(name, list(shape), dt, kind="ExternalInput").ap()

    def dscr(name, shape, dt):
        kind = "ExternalOutput" if name in dbg else "Internal"
        return nc.dram_tensor(name, list(shape), dt, kind=kind).ap()

    x_in = din("x", [S, D])
    c_col = din("c_col", [128, 8])
    ada_w = din("ada_w", [NL, D, 6 * D])
    ada_bB = din("ada_bB", [NL, 128, 6 * D])
    n1g_col = din("n1g_col", [NL, 128, 8])
    w_in = din("w_in", [NL, D, 3400])
    conv_wT = din("conv_wT", [NL, 128, 2, 31])
    conv_bB = din("conv_bB", [NL, 128, 256])
    ln_gB = din("ln_gB", [NL, 128, 256])
    ln_bB = din("ln_bB", [NL, 128, 256])
    w_out = din("w_out", [NL, D, D])
    n2gB = din("n2gB", [NL, 128, D])
    rw = din("rw", [NL, D, 36])
    rbBin = din("rbB", [NL, 128, 36])
    w1 = din("w1", [NL, 32 * D, 512])
    w3 = din("w3", [NL, 32 * D, 512])
    w2 = din("w2", [NL, 32 * 512, D])
    final_gB = din("final_gB", [128, D])
    mgB = din("mgB", [NL, 128, 384])
    dgB = din("dgB", [NL, 128, 384])
    y_out = nc.dram_tensor("y", [S, D], F32, kind="ExternalOutput").ap()

    scr = {}
    for nm, rows, M in [("qm", 384, 128), ("km", 384, 128), ("qd", 384, 128), ("kd", 384, 128), ("qi", 512, 64), ("ki", 64, 64)]:
        scr[nm] = dscr("s_" + nm, [rows, S], BF16)
    scr["u"] = dscr("s_u", [256, S + 32], BF16)
    scr["vm"] = dscr("s_vm", [S, 390], BF16)
    scr["vd"] = dscr("s_vd", [S, 390], BF16)
    scr["wi"] = dscr("s_wi", [S, 8], F32)
    scr["mix"] = dscr("s_mix", [S, D], BF16)
    NBLK_ = 2 * S // 512 + 32
    scr["xmid"] = dscr("s_xmid", [S, D], F32)
    scr["h2"] = dscr("s_h2", [S, D], BF16)
    scr["buf"] = dscr("s_buf", [NBLK_ * 512, D], BF16)
    scr["ybuf"] = dscr("s_ybuf", [NBLK_ * 512, D], F32)
    xbuf = [dscr("s_xbuf%d" % i, [S, D], F32) for i in range(2)]
    d_sel = [dscr("s_sel%d" % i, [128, NT, 32], F32) for i in range(2)]
    d_gates = dscr("s_gates", [128, NT, 2], F32)
    d_dest = dscr("s_dest", [128, NT, 2], I32)
    d_be = dscr("s_be", [128, NBLK_], F32)
    d_modB = dscr("s_modB", [128, 6 * D], F32)

    P = Prog(nc)
    identf = P.sb("identf", [128, 128], F32, glob=True)
    identb = P.sb("identb", [128, 128], BF16, glob=True)
    io = P.sb("io", [128, 128], F32, glob=True)
    pid = P.sb("pid", [128, 1], F32, glob=True)
    P.op("pool", lambda e: e.iota(io[:], pattern=[[1, 128]], base=0, channel_multiplier=0, allow_small_or_imprecise_dtypes=True), writes=["io"])
    P.op("pool", lambda e: e.iota(pid[:], pattern=[[0, 1]], base=0, channel_multiplier=1, allow_small_or_imprecise_dtypes=True), writes=["pid"])
    P.op("dve", lambda e: e.tensor_scalar(identf[:], io[:], pid[:, 0:1], None, op0=ALU.is_equal), reads=["io", "pid"], writes=["identf"])
    P.op("dve", lambda e: e.tensor_copy(identb[:], identf[:]), reads=["identf"], writes=["identb"])

    for l in range(NL):
        P.phase_begin()
        modB = P.sb("modB", [128, 6 * D], F32)
        P.phase_begin()
        cc = P.sb("cc", [128, 8], F32)
        sc = P.sb("sc", [128, 8], F32)
        screp = P.sb("screp", [128, 8, 128], F32)
        abB = P.sb("abB", [128, 6 * D], F32)
        P.dma("sp", cc[:], c_col, writes=["cc"])
        P.dma("sp", abB[:], ada_bB[l], writes=["abB"])
        P.op("act", lambda e: e.activation(sc[:], cc[:], AF.Silu), reads=["cc"], writes=["sc"])
        for kc in range(KC):
            P.op("dve", lambda e, kc=kc: e.tensor_scalar(screp[:, kc, :], io[:], 0.0, sc[:, kc:kc + 1], op0=ALU.mult, op1=ALU.add),
                 reads=["io", "sc"], writes=["screp"])
        awt = Rot([(P.sb("awt", [128, 8, 512], F32), "awt%d" % i) for i in range(2)])
        pm = Rot([(P.ps("pm", [128, 512], F32), "pm%d" % i) for i in range(2)])
        for fc in range(12):
            wt, wn = awt.next()
            pt, pn = pm.next()
            P.dma("sp", wt[:], ada_w[l, :, fc * 512:(fc + 1) * 512].rearrange("(kc p) n -> p kc n", p=128), writes=[wn])
            for kc in range(KC):
                P.op("pe", lambda e, kc=kc, wt=wt, pt=pt: e.matmul(pt[:], screp[:, kc, :], wt[:, kc, :], start=(kc == 0), stop=(kc == KC - 1)),
                     reads=["screp", wn], writes=[pn])
            P.op("dve", lambda e, pt=pt, fc=fc: e.tensor_tensor(out=modB[:, fc * 512:(fc + 1) * 512], in0=pt[:], in1=abB[:, fc * 512:(fc + 1) * 512], op=ALU.add),
                 reads=["abB"], writes=[pn, "modB"])
        P.dma("sp", d_modB, modB[:], reads=["modB"], writes=["s_modB"])
        P.phase_end()

        P.phase_begin()
        Wp = P.sb("Wp", [128, KC, NCW], BF16)
        sh1rep = P.sb("sh1rep", [128, KC, 128], F32)
        gmodT = P.sb("gmodT", [128, KC], F32)
        n1c = P.sb("n1c", [128, KC], F32)
        bB = P.sb("bB", [128, 3400], F32)
        bBv = P.sb("bBv", [128, 2, 6, 65], F32)
        biasT = P.sb("biasT", [128, 32], F32)
        P.dma("sp", n1c[:], n1g_col[l], writes=["n1c"])
        P.phase_begin()
        ptr = Rot([(P.ps("ptr", [128, 512], F32), "ptr%d" % i) for i in range(2)])
        for kc in range(KC):
            pt, pn = ptr.next()
            P.op("pe", lambda e, pt=pt, kc=kc: e.transpose(pt[:, 0:128], modB[:, kc * 128:(kc + 1) * 128], identf[:]), reads=["modB", "identf"], writes=[pn])
            P.op("act", lambda e, pt=pt, kc=kc: e.copy(sh1rep[:, kc, :], pt[:, 0:128]), reads=[], writes=[pn, "sh1rep"])
            pt, pn = ptr.next()
            P.op("pe", lambda e, pt=pt, kc=kc: e.transpose(pt[:, 0:128], modB[:, D + kc * 128:D + (kc + 1) * 128], identf[:]), reads=["modB", "identf"], writes=[pn])
            P.op("dve", lambda e, pt=pt, kc=kc: e.scalar_tensor_tensor(out=gmodT[:, kc:kc + 1], in0=pt[:, 0:1], scalar=1.0, in1=n1c[:, kc:kc + 1], op0=ALU.add, op1=ALU.mult),
                 reads=["n1c"], writes=[pn, "gmodT"])
        P.op("pool", lambda e: e.memset(Wp[:, :, 3400:NCW], 0.0), writes=["Wp"])
        wst = Rot([(P.sb("wst", [128, 3400], F32), "wst%d" % i) for i in range(2)])
        for kc in range(KC):
            wt, wn = wst.next()
            P.dma("sp", wt[:], w_in[l, kc * 128:(kc + 1) * 128, :], writes=[wn])
            P.op("dve", lambda e, wt=wt, kc=kc: e.tensor_scalar(Wp[:, kc, 0:3400], wt[:], gmodT[:, kc:kc + 1], None, op0=ALU.mult),
                 reads=[wn, "gmodT"], writes=["Wp"])
            for vi, c0 in ((0, 768), (1, 2432)):
                P.op("pool", lambda e, wt=wt, kc=kc, vi=vi, c0=c0: e.tensor_scalar(
                    Wp[:, kc, 3400 + vi * 390:3400 + (vi + 1) * 390].rearrange("p (h d) -> p h d", d=65)[:, :, 0:64],
                    wt[:, c0:c0 + 384].rearrange("p (h d) -> p h d", d=64), gmodT[:, kc:kc + 1], None, op0=ALU.mult),
                    reads=[wn, "gmodT"], writes=["Wp"])
        wbt = Rot([(P.sb("wbt", [128, KC, 512], F32), "wbt%d" % i) for i in range(2)])
        pbb = Rot([(P.ps("pbb", [128, 512], F32), "pbb%d" % i) for i in range(2)])
        for cc_ in range(7):
            c0 = cc_ * 512
            n = min(512, 3400 - c0)
            wt, wn = wbt.next()
            pt, pn = pbb.next()
            P.dma("sp", wt[:, :, 0:n], w_in[l, :, c0:c0 + n].rearrange("(kc p) n -> p kc n", p=128), writes=[wn])
            for kc in range(KC):
                P.op("pe", lambda e, kc=kc, wt=wt, pt=pt, n=n: e.matmul(pt[:, 0:n], sh1rep[:, kc, :], wt[:, kc, 0:n], start=(kc == 0), stop=(kc == KC - 1)),
                     reads=["sh1rep", wn], writes=[pn])
            P.op("act", lambda e, pt=pt, c0=c0, n=n: e.copy(bB[:, c0:c0 + n], pt[:, 0:n]), reads=[], writes=[pn, "bB"])
        P.op("pool", lambda e: e.memset(bBv[:], 1.0), writes=["bBv"])
        for vi, c0 in ((0, 768), (1, 2432)):
            P.op("dve", lambda e, vi=vi, c0=c0: e.tensor_copy(bBv[:, vi, :, 0:64], bB[:, c0:c0 + 384].rearrange("p (h d) -> p h d", d=64)),
                 reads=["bB"], writes=["bBv"])
        ucols = []
        for nm, c0, nu, M in FM_UNITS:
            for u in range(nu):
                ucols.append((nm, u, c0 + u * M, M))
        for u in range(2):
            ucols.append(("ca", u, GLU_A + u * 128, 128))
        for u in range(2):
            ucols.append(("cg", u, GLU_G + u * 128, 128))
        bidx = {}
        for k, (nm, u, c0, M) in enumerate(ucols):
            bidx[(nm, u)] = k
            pt, pn = ptr.next()
            P.op("pe", lambda e, pt=pt, c0=c0, M=M: e.transpose(pt[0:M, 0:128], bB[:, c0:c0 + M], identf[:]), reads=["bB", "identf"], writes=[pn])
            P.op("act", lambda e, pt=pt, k=k, M=M: e.copy(biasT[0:M, k:k + 1], pt[0:M, 0:1]), reads=[], writes=[pn, "biasT"])

        P.phase_end()
        xg = Rot([(P.sb("xg", [128, 4, D], F32), "xg%d" % i) for i in range(2)])
        xb = Rot([(P.sb("xb", [128, D], BF16), "xb%d" % i) for i in range(2)])
        xT = Rot([(P.sb("xT", [128, KC, 512], BF16), "xT%d" % i) for i in range(2)])
        junk = P.sb("junk", [128, D], F32)
        ss = Rot([(P.sb("ss", [128, 4], F32), "ss%d" % i) for i in range(2)])
        rs = Rot([(P.sb("rs", [128, 4], F32), "rs%d" % i) for i in range(2)])
        pT = Rot([(P.ps("pT", [128, KC, 128], BF16), "pT%d" % i) for i in range(2)])
        pu = Rot([(P.ps("pu", [128, 512], F32), "pu%d" % i) for i in range(4)])
        pv = Rot([(P.ps("pv", [128, 512], F32), "pv%d" % i) for i in range(2)])
        stg = Rot([(P.sb("stg", [128, 512], BF16), "stg%d" % i) for i in range(4)])
        sig = Rot([(P.sb("sig", [128, 512], F32), "sig%d" % i) for i in range(2)])
        stv = Rot([(P.sb("stv", [128, 390], BF16), "stv%d" % i) for i in range(3)])
        stw = Rot([(P.sb("stw", [128, 8], F32), "stw%d" % i) for i in range(2)])
        zt = P.sb("zt", [128, 32], BF16)
        P.op("pool", lambda e: e.memset(zt[:], 0.0), writes=["zt"])
        for h in range(2):
            P.dma("sp", scr["u"][h * 128:(h + 1) * 128, 0:32], zt[:], reads=["zt"], writes=["s_u"])

        def load_x(G):
            t, n = xg.next()
            P.dma("sp", t[:], (x_in if l == 0 else xbuf[(l - 1) % 2])[G * 512:(G + 1) * 512, :].rearrange("(j p) d -> p j d", p=128), reads=["xsrc"], writes=[n])
            return t, n

        nxt = load_x(0)
        for G in range(NG):
            xt_, xn = nxt
            if G + 1 < NG:
                nxt = load_x(G + 1)
            sst, ssn = ss.next()
            rst, rsn = rs.next()
            xTt, xTn = xT.next()
            for j in range(4):
                P.op("act", lambda e, j=j, xt_=xt_, sst=sst: e.activation(junk[:], xt_[:, j, :], AF.Square, accum_out=sst[:, j:j + 1]),
                     reads=[xn], writes=["junk", ssn])
            P.op("act", lambda e, sst=sst, rst=rst: e.activation(rst[:], sst[:], AF.Sqrt, bias=1e-6, scale=1.0 / D), reads=[ssn], writes=[rsn])
            P.op("dve", lambda e, rst=rst: e.reciprocal(rst[:], rst[:]), reads=[rsn], writes=[rsn])
            for j in range(4):
                xbt, xbn = xb.next()
                ptt, ptn = pT.next()
                P.op("dve", lambda e, j=j, xt_=xt_, xbt=xbt, rst=rst: e.tensor_scalar(xbt[:], xt_[:, j, :], rst[:, j:j + 1], None, op0=ALU.mult),
                     reads=[xn, rsn], writes=[xbn])
                for kc in range(KC):
                    P.op("pe", lambda e, kc=kc, xbt=xbt, ptt=ptt: e.transpose(ptt[:, kc, :], xbt[:, kc * 128:(kc + 1) * 128], identb[:]),
                         reads=[xbn, "identb"], writes=[ptn])
                P.op("act", lambda e, j=j, xTt=xTt, ptt=ptt: e.copy(xTt[:, :, j * 128:(j + 1) * 128], ptt[:]), reads=[], writes=[ptn, xTn])
            for nm, c0, nu, M in FM_UNITS:
                for u in range(nu):
                    put, pun = pu.next()
                    sgt, sgn = stg.next()
                    cs = c0 + u * M
                    for kc in range(KC):
                        P.op("pe", lambda e, kc=kc, put=put, cs=cs, M=M, xTt=xTt: e.matmul(put[0:M, :], Wp[:, kc, cs:cs + M], xTt[:, kc, :], start=(kc == 0), stop=(kc == KC - 1)),
                             reads=["Wp", xTn], writes=[pun])
                    k = bidx[(nm, u)]
                    P.op("act", lambda e, put=put, sgt=sgt, M=M, k=k: e.activation(sgt[0:M, :], put[0:M, :], AF.Identity, bias=biasT[0:M, k:k + 1]),
                         reads=["biasT"], writes=[pun, sgn])
                    P.dma("sp", scr[nm][u * M:(u + 1) * M, G * 512:(G + 1) * 512], sgt[0:M, :], reads=[sgn], writes=["s_" + nm])
            for u in range(2):
                pa, pan = pu.next()
                pg, pgn = pu.next()
                sgt, sgn = stg.next()
                sit, sin_ = sig.next()
                for kc in range(KC):
                    P.op("pe", lambda e, kc=kc, pa=pa, u=u, xTt=xTt: e.matmul(pa[:], Wp[:, kc, GLU_A + u * 128:GLU_A + (u + 1) * 128], xTt[:, kc, :], start=(kc == 0), stop=(kc == KC - 1)),
                         reads=["Wp", xTn], writes=[pan])
                for kc in range(KC):
                    P.op("pe", lambda e, kc=kc, pg=pg, u=u, xTt=xTt: e.matmul(pg[:], Wp[:, kc, GLU_G + u * 128:GLU_G + (u + 1) * 128], xTt[:, kc, :], start=(kc == 0), stop=(kc == KC - 1)),
                         reads=["Wp", xTn], writes=[pgn])
                kg = bidx[("cg", u)]
                ka = bidx[("ca", u)]
                P.op("act", lambda e, pg=pg, sit=sit, kg=kg: e.activation(sit[:], pg[:], AF.Sigmoid, bias=biasT[:, kg:kg + 1]),
                     reads=["biasT"], writes=[pgn, sin_])
                P.op("dve", lambda e, pa=pa, sit=sit, sgt=sgt, ka=ka: e.scalar_tensor_tensor(out=sgt[:], in0=pa[:], scalar=biasT[:, ka:ka + 1], in1=sit[:], op0=ALU.add, op1=ALU.mult),
                     reads=["biasT", sin_], writes=[pan, sgn])
                P.dma("sp", scr["u"][u * 128:(u + 1) * 128, 32 + G * 512:32 + (G + 1) * 512], sgt[:], reads=[sgn], writes=["s_u"])
            for j in range(4):
                r0 = G * 512 + j * 128
                for vi, nm in ((0, "vm"), (1, "vd")):
                    pvt, pvn = pv.next()
                    svt, svn = stv.next()
                    for kc in range(KC):
                        P.op("pe", lambda e, kc=kc, pvt=pvt, vi=vi, j=j, xTt=xTt: e.matmul(pvt[:, 0:390], xTt[:, kc, j * 128:(j + 1) * 128], Wp[:, kc, 3400 + vi * 390:3400 + (vi + 1) * 390], start=(kc == 0), stop=(kc == KC - 1)),
                             reads=["Wp", xTn], writes=[pvn])
                    P.op("dve", lambda e, pvt=pvt, svt=svt, vi=vi: e.tensor_tensor(out=svt[:], in0=pvt[:, 0:390], in1=bBv[:, vi].rearrange("p h d -> p (h d)"), op=ALU.add),
                         reads=["bBv"], writes=[pvn, svn])
                    P.dma("sp", scr[nm][r0:r0 + 128, :], svt[:], reads=[svn], writes=["s_" + nm])
                pvt, pvn = pv.next()
                swt, swn = stw.next()
                for kc in range(KC):
                    P.op("pe", lambda e, kc=kc, pvt=pvt, j=j, xTt=xTt: e.matmul(pvt[:, 0:8], xTt[:, kc, j * 128:(j + 1) * 128], Wp[:, kc, 3392:3400], start=(kc == 0), stop=(kc == KC - 1)),
                         reads=["Wp", xTn], writes=[pvn])
                P.op("dve", lambda e, pvt=pvt, swt=swt: e.tensor_tensor(out=swt[:], in0=pvt[:, 0:8], in1=bB[:, 3392:3400], op=ALU.add),
                     reads=["bB"], writes=[pvn, swn])
                P.dma("sp", scr["wi"][r0:r0 + 128, :], swt[:], reads=[swn], writes=["s_wi"])
        P.phase_end()
        P.phase_end()


        for kind in ("moba", "dsa"):
            P.phase_begin()
            qn, kn, vn = ("qm", "km", "vm") if kind == "moba" else ("qd", "kd", "vd")
            kT = P.sb("kT", [128, 3, S], BF16)
            V = P.sb("V", [128, NT, 390], BF16)
            gB = P.sb("gB", [128, 384], F32)
            for p in range(3):
                P.dma("sp", kT[:, p, :], scr[kn][p * 128:(p + 1) * 128, :], reads=["s_" + kn], writes=["kT"])
            for c8 in range(0, NT, 8):
                P.dma("sp", V[:, c8:c8 + 8, :], scr[vn][c8 * 128:(c8 + 8) * 128, :].rearrange("(c p) n -> p c n", p=128), reads=["s_" + vn], writes=["V"])
            P.dma("sp", gB[:], (mgB if kind == "moba" else dgB)[l], writes=["gB"])
            tri = P.sb("tri", [128, 128], BF16)
            P.op("dve", lambda e: e.tensor_scalar(tri[:], io[:], pid[:, 0:1], NEG, op0=ALU.is_gt, op1=ALU.mult), reads=["io", "pid"], writes=["tri"])
            nbuf = 2 if kind == "moba" else 1
            QW = 512 if kind == "moba" else 128
            qpads = [P.sb("qpad", [128, 6, QW], BF16) for _ in range(2)]
            for i in range(2):
                P.op("pool", lambda e, i=i: e.memset(qpads[i][:], 0.0), writes=["qpad%d" % i])
            qgs = Rot([(P.sb("qg", [128, 3, QW], BF16), "qg%d" % i) for i in range(2)])
            junk2 = P.sb("junk2", [128, 384], BF16)
            Sps = Rot([(P.ps("Sps", [128, 4, 128], F32), "Sps%d" % i) for i in range(3)])
            Ops = Rot([(P.ps("Ops", [128, 512], F32), "Ops%d" % i) for i in range(2)])
            if kind == "dsa":
                Xps = Rot([(P.ps("Xps", [128, 512], F32), "Xps%d" % i) for i in range(2)])
            else:
                pmisc = P.ps("pmisc", [128, 1024], BF16)
                pgate = P.ps("pgate", [128, 16, 32], F32)
            PTs = Rot([(P.sb("PT", [128, 4, 128], BF16), "PT%d" % i) for i in range(3 if kind == "moba" else 2)])
            osbs = Rot([(P.sb("osb", [128, 6, 65], F32), "osb%d" % i) for i in range(2 if kind == "moba" else 1)])
            ym = P.sb("ym", [128, 6, 64], F32)
            rden = P.sb("rden", [128, 6], F32)
            sst = P.sb("sst2", [128, 2], F32)
            mixo = Rot([(P.sb("mixo", [128, 384], BF16), "mixo%d" % i) for i in range(2 if kind == "moba" else 1)])
            if kind == "moba":
                ksum = P.sb("ksum", [128, 3, NB], F32)
                kmeanb = P.sb("kmeanb", [128, 3, 32], BF16)
                Eall = P.sb("Eall", [128, 32, 128], BF16)
                gate_sb = P.sb("gate_sb", [128, 6, 32], F32)
                mx = P.sb("mx", [128, 6, 8], F32)
                selb = P.sb("selb", [128, 6, 32], BF16)
                sbTs = Rot([(P.sb("sbT", [128, 6, 128], BF16), "sbT%d" % i) for i in range(2)])
                for (t_, n_) in sbTs.items:
                    P.op("pool", lambda e, t_=t_: e.memset(t_[:], 0.0), writes=[n_])
                for p in range(3):
                    P.op("dve", lambda e, p=p: e.tensor_reduce(out=ksum[:, p, :], in_=kT[:, p, :].rearrange("r (n k) -> r n k", k=256), axis=AX.X, op=ALU.add),
                         reads=["kT"], writes=["ksum"])
                P.op("pool", lambda e: e.memset(kmeanb[:], 0.0), writes=["kmeanb"])
                P.op("dve", lambda e: e.tensor_scalar(kmeanb[:, :, 0:NB], ksum[:], 1.0 / 256, None, op0=ALU.mult), reads=["ksum"], writes=["kmeanb"])
                P.op("pool", lambda e: e.memset(Eall[:], 0.0), writes=["Eall"])
                P.op("dve", lambda e: e.tensor_copy(Eall[0:32], identf[0:32, 0:32, None].to_broadcast([32, 32, 128])), reads=["identf"], writes=["Eall"])
                P.op("pool", lambda e: e.memset(gate_sb[:], -1e30), writes=["gate_sb"])
            else:
                kiT = P.sb("kiT", [128, S], BF16)
                P.op("pool", lambda e: e.memset(kiT[64:128, :], 0.0), writes=["kiT"])
                P.dma("sp", kiT[0:64, :], scr["ki"], reads=["s_ki"], writes=["kiT"])
                qis = Rot([(P.sb("qi", [128, 8, 128], BF16), "qi%d" % i) for i in range(2)])
                for (t_, n_) in qis.items:
                    P.op("pool", lambda e, t_=t_: e.memset(t_[:], 0.0), writes=[n_])
                wis = Rot([(P.sb("wi", [128, 4, 8], F32), "wi%d" % i) for i in range(2)])
                acc = P.sb("acc", [128, S], F32)
                dbs = Rot([(P.sb("dbias", [128, S], BF16), "dbias%d" % i) for i in range(2)])
                rls = Rot([(P.sb("rl", [128, 512], BF16), "rl%d" % i) for i in range(8)])
                dWs = Rot([(P.sb("dW", [128, 8, 128], BF16), "dW%d" % i) for i in range(1)])
                accp = P.ps("accp", [128, 512], F32)
                cntA = Rot([(P.sb("cntA", [128, 1], F32), "cntA%d" % i) for i in range(2)])
                tmpc = Rot([(P.sb("tmpc", [128, 1], F32), "tmpc%d" % i) for i in range(2)])
                triD = P.sb("triD", [128, 128], F32)
                P.op("dve", lambda e: e.tensor_scalar(triD[:], io[:], pid[:, 0:1], -1e30, op0=ALU.is_gt, op1=ALU.mult), reads=["io", "pid"], writes=["triD"])
                pw2 = P.sb("pw2", [128, NIT + 1], F32)
                for k in range(NIT + 1):
                    P.op("pool", lambda e, k=k: e.memset(pw2[:, k:k + 1], 2.0 ** -(k + 1)), writes=["pw2"])
                Wk = P.sb("Wk", [128, NIT + 1], F32)
                bs = P.sb("bs", [128, 8], F32)
                los = Rot([(P.sb("lo", [128, 1], F32), "lo%d" % i) for i in range(2)])
                mids = Rot([(P.sb("mid", [128, 1], F32), "mid%d" % i) for i in range(2)])
                cnts = Rot([(P.sb("cnt", [128, 1], F32), "cnt%d" % i) for i in range(2)])
                tts = Rot([(P.sb("tt", [128, 1], F32), "tt%d" % i) for i in range(2)])

            def load_q(G, idx):
                if not idx:
                    t, n = qgs.next()
                    P.dma("sp", t[:], scr[qn][:, G * QW:(G + 1) * QW].rearrange("(p r) t -> r p t", r=128), reads=["s_" + qn], writes=[n])
                    return [(t, n)]
                t2, n2 = None, None
                t3, n3 = wis.next()
                P.dma("sp", t3[:], scr["wi"][G * 512:(G + 1) * 512, :].rearrange("(j p) h -> p j h", p=128), reads=["s_wi"], writes=[n3])
                return [(t2, n2), (t3, n3)]

            grp = {}

            def load_att(G):
                cur = load_q(G, False)
                qg, qgn = cur[0]
                qpad = qpads[G % 2]
                qpn = "qpad%d" % (G % 2)
                P.op("act", lambda e, qpad=qpad, qg=qg: e.copy(qpad[0:64].rearrange("r (p two) t -> r p two t", two=2)[:, :, 0, :], qg[0:64, :, :]), reads=[qgn], writes=[qpn])
                P.op("dve", lambda e, qpad=qpad, qg=qg: e.tensor_copy(qpad[64:128].rearrange("r (p two) t -> r p two t", two=2)[:, :, 1, :], qg[64:128, :, :]), reads=[qgn], writes=[qpn])
                grp[G] = (qpad, qpn)

            tst = {}

            def pre(G, j):
                qt = 4 * G + j
                nch = qt + 1
                if kind == "moba":
                    if G not in grp:
                        load_att(G)
                    qpad, qpn = grp[G]
                else:
                    if j == 0:
                        cur = load_q(G, True)
                        tst["qi"] = cur
                    qi, qin = qis.next()
                    P.dma("sp", qi[0:64], scr["qi"][:, qt * 128:(qt + 1) * 128].rearrange("(h d) t -> d h t", d=64), reads=["s_qi"], writes=[qin])
                    wi, win = tst["qi"][1]
                b = qt // 2
                sbT = sbTn = dbias = dbn = None
                qt = 4 * G + j
                nch = qt + 1
                if kind == "moba":
                    b = qt // 2
                    sbT, sbTn = sbTs.next()
                    if b > 0:
                        for h in range(6):
                            P.op("pe", lambda e, h=h, j=j, qpad=qpad: e.matmul(pgate[:, h, :], qpad[:, h, j * 128:(j + 1) * 128], kmeanb[:, h // 2, :], start=True, stop=True),
                                 reads=[qpn, "kmeanb"], writes=["Xg"])
                        P.op("dve", lambda e, b=b: e.tensor_copy(gate_sb[:, :, 0:b], pgate[:, 0:6, 0:b]), reads=[], writes=["Xg", "gate_sb"])
                        for h in range(6):
                            P.op("dve", lambda e, h=h: e.max(out=mx[:, h, :], in_=gate_sb[:, h, :]), reads=["gate_sb"], writes=["mx"])
                        for h in range(6):
                            P.op("dve", lambda e, h=h: e.tensor_scalar(selb[:, h, :], gate_sb[:, h, :], mx[:, h, 2:3], NEG, op0=ALU.is_lt, op1=ALU.mult),
                                 reads=["gate_sb", "mx"], writes=["selb"])
                        for h in range(6):
                            P.op("pe", lambda e, h=h: e.transpose(pmisc[0:32, h * 128:(h + 1) * 128], selb[:, h, :], identb[:]), reads=["selb", "identb"], writes=["pmisc"])
                        P.op("act", lambda e, sbT=sbT: e.copy(sbT[0:32], pmisc[0:32, 0:768].rearrange("n (h t) -> n h t", t=128)), reads=[], writes=["pmisc", sbTn])
                else:
                    L = nch * 128
                    dbias, dbn = dbs.next()
                    dW, dWn = dWs.next()
                    for h in range(8):
                        P.op("dve", lambda e, dW=dW, wi=wi, h=h, j=j: e.tensor_scalar(dW[:, h, :], identf[:], wi[:, j, h:h + 1], None, op0=ALU.mult),
                             reads=["identf", win], writes=[dWn])
                    for cc in range((L + 511) // 512):
                        n = min(512, L - cc * 512)
                        rr = []
                        for h in range(8):
                            xp, xpn = Xps.next()
                            rl, rln = rls.next()
                            rr.append((rl, rln))
                            P.op("pe", lambda e, xp=xp, h=h, cc=cc, n=n, qi=qi, j=j: e.matmul(xp[:, 0:n], qi[:, h, :], kiT[:, cc * 512:cc * 512 + n], start=True, stop=True),
                                 reads=[qin, "kiT"], writes=[xpn])
                            if h % 3 != 2:
                                P.op("act", lambda e, xp=xp, rl=rl, n=n: e.activation(rl[:, 0:n], xp[:, 0:n], AF.Relu), reads=[], writes=[xpn, rln])
                            else:
                                P.op("dve", lambda e, xp=xp, rl=rl, n=n: e.tensor_scalar(rl[:, 0:n], xp[:, 0:n], 0.0, None, op0=ALU.max), reads=[], writes=[xpn, rln])
                        for h in range(8):
                            rl, rln = rr[h]
                            P.op("pe", lambda e, rl=rl, dW=dW, h=h, n=n: e.matmul(accp[:, 0:n], dW[:, h, :], rl[:, 0:n], start=(h == 0), stop=(h == 7)),
                                 reads=[rln, dWn], writes=["accp"])
                        if cc % 2 == 0:
                            P.op("act", lambda e, cc=cc, n=n: e.copy(acc[:, cc * 512:cc * 512 + n], accp[:, 0:n]), reads=[], writes=["accp", "acc"])
                        else:
                            P.op("dve", lambda e, cc=cc, n=n: e.tensor_copy(acc[:, cc * 512:cc * 512 + n], accp[:, 0:n]), reads=[], writes=["accp", "acc"])
                        yield
                    P.op("dve", lambda e, L=L: e.tensor_reduce(out=bs[:, 0:1], in_=acc[:, 0:L], axis=AX.X, op=ALU.max, apply_absolute_value=True), reads=["acc"], writes=["bs"])
                    P.op("dve", lambda e: e.tensor_scalar(bs[:, 1:2], bs[:, 0:1], -1.0, None, op0=ALU.mult), reads=["bs"], writes=["bs"])
                    P.op("dve", lambda e, L=L: e.tensor_tensor(out=acc[:, L - 128:L], in0=acc[:, L - 128:L], in1=triD[:], op=ALU.add), reads=["triD"], writes=["acc"])
                    lo, lon = los.next()
                    P.op("dve", lambda e, lo=lo: e.tensor_scalar(lo[:], bs[:, 1:2], -1.0, None, op0=ALU.add), reads=["bs"], writes=[lon])
                    P.op("dve", lambda e: e.scalar_tensor_tensor(out=bs[:, 2:3], in0=bs[:, 0:1], scalar=2.0, in1=bs[:, 1:2], op0=ALU.add, op1=ALU.subtract), reads=["bs"], writes=["bs"])
                    P.op("dve", lambda e: e.tensor_scalar(Wk[:], pw2[:], bs[:, 2:3], None, op0=ALU.mult), reads=["pw2", "bs"], writes=["Wk"])
                    yield
                    for k in range(NIT):
                        mid, midn = mids.next()
                        cnt, cntn = cnts.next()
                        tt, ttn = tts.next()
                        lo2, lo2n = los.next()
                        P.op("dve", lambda e, lo=lo, mid=mid, k=k: e.tensor_tensor(out=mid[:], in0=lo[:], in1=Wk[:, k:k + 1], op=ALU.add), reads=[lon, "Wk"], writes=[midn])
                        P.op("dve", lambda e, mid=mid, cnt=cnt, L=L: e.tensor_scalar(dbias[:, 0:L], acc[:, 0:L], mid[:, 0:1], 0.0, op0=ALU.is_ge, op1=ALU.add, accum_out=cnt[:]),
                             reads=["acc", midn], writes=[dbn + "lo", cntn])
                        P.op("dve", lambda e, cnt=cnt, tt=tt, k=k: e.scalar_tensor_tensor(out=tt[:], in0=cnt[:], scalar=255.5, in1=Wk[:, k:k + 1], op0=ALU.is_ge, op1=ALU.mult), reads=[cntn, "Wk"], writes=[ttn])
                        P.op("dve", lambda e, lo=lo, lo2=lo2, tt=tt: e.tensor_tensor(out=lo2[:], in0=lo[:], in1=tt[:], op=ALU.add), reads=[lon, ttn], writes=[lo2n])
                        lo, lon = lo2, lo2n
                        yield
                    P.op("dve", lambda e, lo=lo, L=L: e.tensor_scalar(dbias[:, 0:L], acc[:, 0:L], lo[:, 0:1], NEG, op0=ALU.is_lt, op1=ALU.mult), reads=["acc", lon], writes=[dbn, dbn + "lo", dbn + "hi"])

                tst[qt] = (b, sbT, sbTn, dbias, dbn)
                yield

            def att(G, j, inter=None, nsteps=1):
                qt = 4 * G + j
                nch = qt + 1
                gk = G if kind == "moba" else qt
                if gk not in grp:
                    load_att(gk)
                qpad, qpn = grp[gk]
                jo = j if kind == "moba" else 0
                b, sbT, sbTn, dbias, dbn = tst.pop(qt)
                osb, osbn = osbs.next()
                units = []
                for h in range(6):
                    for c0 in range(0, nch, 4):
                        units.append((h, list(range(c0, min(c0 + 4, nch)))))

                def emitS(h, cs):
                    sp_, spn = Sps.next()
                    for ci, c in enumerate(cs):
                        if kind == "moba":
                            blk = c // 2
                            if blk < b:
                                bias = (Eall[:, blk, :], sbT[:, h, :], ["Eall", sbTn])
                            elif c == qt:
                                bias = (tri[:], identb[:], ["tri", "identb"])
                            else:
                                bias = None
                        else:
                            bias = (dbias[:, c * 128:(c + 1) * 128], identb[:], [dbn, "identb"])
                        P.op("pe", lambda e, sp_=sp_, ci=ci, c=c, h=h, jo=jo, qpad=qpad, bias=bias: e.matmul(sp_[:, ci, :], kT[:, h // 2, c * 128:(c + 1) * 128], qpad[:, h, jo * 128:(jo + 1) * 128], start=True, stop=(bias is None)),
                             reads=["kT", qpn], writes=[spn])
                        if bias is not None:
                            P.op("pe", lambda e, sp_=sp_, ci=ci, bias=bias: e.matmul(sp_[:, ci, :], bias[0], bias[1], start=False, stop=True),
                                 reads=bias[2], writes=[spn])
                    return sp_, spn

                pend = emitS(*units[0])
                ops_ = opn = None
                sdone = 0
                for ui, (h, cs) in enumerate(units):
                    if inter is not None:
                        want = ((ui + 1) * nsteps + len(units) - 1) // len(units)
                        while sdone < want:
                            next(inter, None)
                            sdone += 1
                    sp_, spn = pend
                    if ui + 1 < len(units):
                        pend = emitS(*units[ui + 1])
                    if cs[0] == 0:
                        ops_, opn = Ops.next()
                    pt_, ptn = PTs.next()
                    ncs = len(cs)
                    P.op("act", lambda e, sp_=sp_, pt_=pt_, ncs=ncs: e.activation(pt_[:, 0:ncs, :], sp_[:, 0:ncs, :], AF.Exp, scale=0.125), reads=[], writes=[spn, ptn])
                    for ci, c in enumerate(cs):
                        P.op("pe", lambda e, ops_=ops_, pt_=pt_, ci=ci, c=c, h=h: e.matmul(ops_[:, 0:65], pt_[:, ci, :], V[:, c, h * 65:(h + 1) * 65], start=(c == 0), stop=(c == nch - 1)),
                             reads=[ptn, "V"], writes=[opn])
                    if cs[-1] == nch - 1:
                        if h % 2 == 0:
                            P.op("dve", lambda e, ops_=ops_, osb=osb, h=h: e.tensor_copy(osb[:, h, :], ops_[:, 0:65]), reads=[], writes=[opn, osbn])
                        else:
                            P.op("act", lambda e, ops_=ops_, osb=osb, h=h: e.copy(osb[:, h, :], ops_[:, 0:65]), reads=[], writes=[opn, osbn])
                P.op("dve", lambda e, osb=osb: e.reciprocal(rden[:], osb[:, :, 64]), reads=[osbn], writes=["rden"])
                P.op("dve", lambda e, osb=osb: e.tensor_tensor(out=ym[:], in0=osb[:, :, 0:64], in1=rden[:, :, None].to_broadcast([128, 6, 64]), op=ALU.mult), reads=[osbn, "rden"], writes=["ym"])
                P.op("act", lambda e: e.activation(junk2[:], ym[:].rearrange("p h d -> p (h d)"), AF.Square, accum_out=sst[:, 0:1]), reads=["ym"], writes=["junk2", "sst2"])
                P.op("act", lambda e: e.activation(sst[:, 1:2], sst[:, 0:1], AF.Sqrt, bias=1e-6, scale=1.0 / 384), reads=["sst2"], writes=["sst2b"])
                P.op("dve", lambda e: e.reciprocal(sst[:, 1:2], sst[:, 1:2]), reads=["sst2b"], writes=["sst2b"])
                mo, mon = mixo.next()
                P.op("dve", lambda e, mo=mo: e.scalar_tensor_tensor(out=mo[:], in0=ym[:].rearrange("p h d -> p (h d)"), scalar=sst[:, 1:2], in1=gB[:], op0=ALU.mult, op1=ALU.mult), reads=["ym", "sst2b", "gB"], writes=[mon])
                c0m = 0 if kind == "moba" else 640
                P.dma("sp", scr["mix"][qt * 128:(qt + 1) * 128, c0m:c0m + 384], mo[:], reads=[mon], writes=["s_mix"])

            tiles = [(G, j) for G in range(NG) for j in range(4)]
            for _ in pre(*tiles[0]):
                pass
            for ti, (G, j) in enumerate(tiles):
                if ti + 1 < len(tiles):
                    gen = pre(*tiles[ti + 1])
                    nst = (4 * tiles[ti + 1][0] + tiles[ti + 1][1] + 1 + 3) // 4
                    if kind == "moba":
                        for _ in gen:
                            pass
                        att(G, j)
                    else:
                        next(gen, None)
                        att(G, j, gen, nst + NIT + 1)
                        for _ in gen:
                            pass
                else:
                    att(G, j)
            P.phase_end()


        NBLK = 2 * S // 512 + 32
        x_src = x_in if l == 0 else xbuf[(l - 1) % 2]
        x_dst = xbuf[l % 2]
        P.phase_begin()
        SEL1 = P.sb("SEL1", [128, NT, 32], F32)
        SEL2 = P.sb("SEL2", [128, NT, 32], F32)
        GATES = P.sb("GATES", [128, NT, 2], F32)
        P.phase_begin()
        ub = P.sb("ub", [128, 2, S + 32], BF16)
        for hh in range(2):
            P.dma("sp", ub[:, hh, :], scr["u"][hh * 128:(hh + 1) * 128, :], reads=["s_u"], writes=["ub"])
        cw = P.sb("cw", [128, 2, 31], F32)
        P.dma("sp", cw[:], conv_wT[l], writes=["cw"])
        dgw = P.sb("dgw", [128, 2, 31, 128], BF16)
        for hh in range(2):
            for jj in range(31):
                P.op("dve" if jj % 2 else "pool", lambda e, hh=hh, jj=jj: e.tensor_scalar(dgw[:, hh, jj, :], identf[:], cw[:, hh, jj:jj + 1], None, op0=ALU.mult),
                     reads=["identf", "cw"], writes=["dgw"])
        woB = P.sb("woB", [128, KC, D], BF16)
        P.dma("pool", woB[:], w_out[l].rearrange("(kc p) n -> p kc n", p=128), writes=["woB"])
        cbB = P.sb("cbB", [128, 256], F32)
        lgB = P.sb("lgB", [128, 256], F32)
        lbB = P.sb("lbB", [128, 256], F32)
        g1B = P.sb("g1B", [128, D], F32)
        gm2B = P.sb("gm2B", [128, D], F32)
        sh2B = P.sb("sh2B", [128, D], F32)
        n2B = P.sb("n2B", [128, D], F32)
        wr = P.sb("wr", [128, KC, 36], F32)
        rbB = P.sb("rbB", [128, 36], F32)
        P.dma("sp", cbB[:], conv_bB[l], writes=["cbB"])
        P.dma("sp", lgB[:], ln_gB[l], writes=["lgB"])
        P.dma("sp", lbB[:], ln_bB[l], writes=["lbB"])
        P.dma("sp", g1B[:], d_modB[:, 2 * D:3 * D], reads=["s_modB"], writes=["g1B"])
        P.dma("sp", sh2B[:], d_modB[:, 3 * D:4 * D], reads=["s_modB"], writes=["sh2B"])
        P.dma("sp", gm2B[:], d_modB[:, 4 * D:5 * D], reads=["s_modB"], writes=["gm2B"])
        P.dma("sp", n2B[:], n2gB[l], writes=["n2B"])
        P.dma("sp", wr[:], rw[l].rearrange("(kc p) n -> p kc n", p=128), writes=["wr"])
        P.dma("sp", rbB[:], rbBin[l], writes=["rbB"])
        P.op("dve", lambda e: e.scalar_tensor_tensor(out=gm2B[:], in0=gm2B[:], scalar=1.0, in1=n2B[:], op0=ALU.add, op1=ALU.mult), reads=["n2B"], writes=["gm2B"])
        pc = Rot([(P.ps("pc", [128, 512], F32), "pc%d" % i) for i in range(2)])
        pT4 = Rot([(P.ps("pT4", [128, KC, 128], BF16), "pT4%d" % i) for i in range(1)])
        po = Rot([(P.ps("po", [128, 512], F32), "po%d" % i) for i in range(2)])
        pf = Rot([(P.ps("pf", [128, 4, 128], F32), "pf%d" % i) for i in range(2)])
        pl = P.ps("pl", [128, 512], F32)
        ycs = Rot([(P.sb("yc", [128, 256], F32), "yc%d" % i) for i in range(2)])
        st4 = P.sb("st4", [128, 16], F32)
        junk4 = P.sb("junk4", [128, D], F32)
        mixt = Rot([(P.sb("mixt", [128, D], BF16), "mixt%d" % i) for i in range(2)])
        mixT = P.sb("mixT", [128, KC, 128], BF16)
        xts = Rot([(P.sb("xt4", [128, D], F32), "xt4%d" % i) for i in range(2)])
        xms = Rot([(P.sb("xm", [128, D], F32), "xm%d" % i) for i in range(2)])
        h2fs = Rot([(P.sb("h2f", [128, D], F32), "h2f%d" % i) for i in range(2)])
        h2bs = Rot([(P.sb("h2b", [128, D], BF16), "h2b%d" % i) for i in range(2)])
        h2T = P.sb("h2T", [128, KC, 128], F32)
        lg = P.sb("lg", [128, 36], F32)
        rt = P.sb("rt", [128, 96], F32)
        def tile_gen(i):
            r0 = i * 128
            mt, mtn = mixt.next()
            xt_, xtn = xts.next()
            P.dma("sp", mt[:, 0:384], scr["mix"][r0:r0 + 128, 0:384], reads=["s_mix"], writes=[mtn])
            P.dma("sp", mt[:, 640:1024], scr["mix"][r0:r0 + 128, 640:1024], reads=["s_mix"], writes=[mtn])
            P.dma("sp", xt_[:], x_src[r0:r0 + 128, :], reads=["xsrc"], writes=[xtn])
            pct, pcn = pc.next()
            for hh in range(2):
                for jj in range(31):
                    P.op("pe", lambda e, pct=pct, hh=hh, jj=jj, r0=r0: e.matmul(pct[:, hh * 128:(hh + 1) * 128], ub[:, hh, r0 + 2 + jj:r0 + 2 + jj + 128], dgw[:, hh, jj, :], start=(jj == 0), stop=(jj == 30)),
                         reads=["ub", "dgw"], writes=[pcn])
            yc, ycn = ycs.next()
            P.op("dve", lambda e, pct=pct, yc=yc: e.tensor_tensor(out=yc[:], in0=pct[:, 0:256], in1=cbB[:], op=ALU.add), reads=["cbB"], writes=[pcn, ycn])
            yield
            P.op("dve", lambda e: e.tensor_reduce(out=st4[:, 0:1], in_=yc[:], axis=AX.X, op=ALU.add), reads=[ycn], writes=["st4a"])
            P.op("dve", lambda e: e.tensor_scalar(st4[:, 1:2], st4[:, 0:1], -1.0 / 256, None, op0=ALU.mult), reads=["st4a"], writes=["st4b"])
            P.op("dve", lambda e: e.tensor_scalar(yc[:], yc[:], st4[:, 1:2], None, op0=ALU.add), reads=["st4b"], writes=[ycn])
            P.op("act", lambda e: e.activation(junk4[:, 0:256], yc[:], AF.Square, accum_out=st4[:, 2:3]), reads=[ycn], writes=["junk4", "st4c"])
            P.op("act", lambda e: e.activation(st4[:, 3:4], st4[:, 2:3], AF.Sqrt, bias=1e-6, scale=1.0 / 256), reads=["st4c"], writes=["st4d"])
            P.op("dve", lambda e: e.reciprocal(st4[:, 3:4], st4[:, 3:4]), reads=["st4d"], writes=["st4d"])
            P.op("dve", lambda e: e.scalar_tensor_tensor(out=yc[:], in0=yc[:], scalar=st4[:, 3:4], in1=lgB[:], op0=ALU.mult, op1=ALU.mult), reads=["st4d", "lgB"], writes=[ycn])
            P.op("dve", lambda e: e.tensor_tensor(out=yc[:], in0=yc[:], in1=lbB[:], op=ALU.add), reads=["lbB"], writes=[ycn])
            P.op("act", lambda e, mt=mt: e.activation(mt[:, 384:640], yc[:], AF.Silu), reads=[ycn], writes=[mtn])
            ptt, ptn = pT4.next()
            for kc in range(KC):
                P.op("pe", lambda e, kc=kc, mt=mt, ptt=ptt: e.transpose(ptt[:, kc, :], mt[:, kc * 128:(kc + 1) * 128], identb[:]), reads=[mtn, "identb"], writes=[ptn])
            P.op("act", lambda e, ptt=ptt: e.copy(mixT[:], ptt[:]), reads=[], writes=[ptn, "mixT"])
            xm, xmn = xms.next()
            for hf in range(2):
                pot, pon = po.next()
                for kc in range(KC):
                    P.op("pe", lambda e, kc=kc, pot=pot, hf=hf: e.matmul(pot[:], mixT[:, kc, :], woB[:, kc, hf * 512:(hf + 1) * 512], start=(kc == 0), stop=(kc == KC - 1)),
                         reads=["mixT", "woB"], writes=[pon])
                P.op("dve", lambda e, pot=pot, hf=hf, xm=xm: e.tensor_tensor(out=xm[:, hf * 512:(hf + 1) * 512], in0=pot[:], in1=g1B[:, hf * 512:(hf + 1) * 512], op=ALU.mult),
                     reads=["g1B"], writes=[pon, xmn])
            P.op("pool", lambda e, xm=xm, xt_=xt_: e.tensor_tensor(out=xm[:], in0=xm[:], in1=xt_[:], op=ALU.add), reads=[xtn], writes=[xmn])
            P.dma("sp", scr["xmid"][r0:r0 + 128, :], xm[:], reads=[xmn], writes=["s_xmid"])
            P.op("act", lambda e, xm=xm: e.activation(junk4[:], xm[:], AF.Square, accum_out=st4[:, 4:5]), reads=[xmn], writes=["junk4", "st4e"])
            P.op("act", lambda e: e.activation(st4[:, 5:6], st4[:, 4:5], AF.Sqrt, bias=1e-6, scale=1.0 / D), reads=["st4e"], writes=["st4f"])
            P.op("dve", lambda e: e.reciprocal(st4[:, 5:6], st4[:, 5:6]), reads=["st4f"], writes=["st4f"])
            h2f, h2fn = h2fs.next()
            h2b, h2bn = h2bs.next()
            P.op("dve", lambda e, h2f=h2f, xm=xm: e.scalar_tensor_tensor(out=h2f[:], in0=xm[:], scalar=st4[:, 5:6], in1=gm2B[:], op0=ALU.mult, op1=ALU.mult), reads=[xmn, "st4f", "gm2B"], writes=[h2fn])
            P.op("pool", lambda e, h2f=h2f: e.tensor_tensor(out=h2f[:], in0=h2f[:], in1=sh2B[:], op=ALU.add), reads=["sh2B"], writes=[h2fn])
            P.op("act", lambda e, h2f=h2f, h2b=h2b: e.copy(h2b[:], h2f[:]), reads=[h2fn], writes=[h2bn])
            P.dma("sp", scr["h2"][r0:r0 + 128, :], h2b[:], reads=[h2bn], writes=["s_h2"])
            yield
            for q4 in range(2):
                pft, pfn = pf.next()
                for k4 in range(4):
                    kc = q4 * 4 + k4
                    P.op("pe", lambda e, pft=pft, k4=k4, kc=kc, h2f=h2f: e.transpose(pft[:, k4, :], h2f[:, kc * 128:(kc + 1) * 128], identf[:]), reads=[h2fn, "identf"], writes=[pfn])
                if q4 == 0:
                    P.op("act", lambda e, pft=pft, q4=q4: e.copy(h2T[:, q4 * 4:(q4 + 1) * 4, :], pft[:]), reads=[], writes=[pfn, "h2T"])
                else:
                    P.op("dve", lambda e, pft=pft, q4=q4: e.tensor_copy(h2T[:, q4 * 4:(q4 + 1) * 4, :], pft[:]), reads=[], writes=[pfn, "h2T"])
            for kc in range(KC):
                P.op("pe", lambda e, kc=kc: e.matmul(pl[:, 0:36], h2T[:, kc, :], wr[:, kc, :], start=(kc == 0), stop=(kc == KC - 1)), reads=["h2T", "wr"], writes=["pl"])
            P.op("dve", lambda e: e.tensor_tensor(out=lg[:], in0=pl[:, 0:36], in1=rbB[:], op=ALU.add), reads=["rbB"], writes=["pl", "lg"])
            V_ = lambda a, b: rt[:, a:b]
            P.op("dve", lambda e: e.tensor_reduce(out=V_(0, 1), in_=lg[:, 0:4], axis=AX.X, op=ALU.max), reads=["lg"], writes=["rt0"])
            P.op("dve", lambda e: e.tensor_scalar(V_(1, 2), V_(0, 1), -1.0, None, op0=ALU.mult), reads=["rt0"], writes=["rt1"])
            P.op("act", lambda e: e.activation(V_(40, 44), lg[:, 0:4], AF.Exp, bias=V_(1, 2), accum_out=V_(2, 3)), reads=["lg", "rt1"], writes=["rt40", "rt2"])
            P.op("dve", lambda e: e.reciprocal(V_(3, 4), V_(2, 3)), reads=["rt2"], writes=["rt3"])
            P.op("dve", lambda e: e.tensor_scalar(V_(4, 8), lg[:, 0:4], V_(0, 1), None, op0=ALU.is_equal), reads=["lg", "rt0"], writes=["rt4"])
            P.op("dve", lambda e: e.tensor_tensor(out=V_(48, 80).rearrange("p (g x) -> p g x", x=8), in0=lg[:, 4:36].rearrange("p (g x) -> p g x", x=8), in1=V_(4, 8)[:, :, None].to_broadcast([128, 4, 8]), op=ALU.mult),
                 reads=["lg", "rt4"], writes=["rt48"])
            P.op("dve", lambda e: e.tensor_reduce(out=V_(8, 16), in_=V_(48, 80).rearrange("p (g x) -> p x g", x=8), axis=AX.X, op=ALU.add), reads=["rt48"], writes=["rt8"])
            P.op("dve", lambda e: e.tensor_reduce(out=V_(16, 17), in_=V_(8, 16), axis=AX.X, op=ALU.max), reads=["rt8"], writes=["rt16"])
            P.op("dve", lambda e: e.tensor_scalar(V_(17, 18), V_(16, 17), -1.0, None, op0=ALU.mult), reads=["rt16"], writes=["rt17"])
            P.op("dve", lambda e: e.tensor_scalar(V_(18, 26), V_(8, 16), V_(16, 17), None, op0=ALU.is_equal), reads=["rt8", "rt16"], writes=["rt18"])
            P.op("dve", lambda e: e.scalar_tensor_tensor(out=V_(26, 34), in0=V_(18, 26), scalar=-1e30, in1=V_(8, 16), op0=ALU.mult, op1=ALU.add), reads=["rt18", "rt8"], writes=["rt26"])
            P.op("dve", lambda e: e.tensor_reduce(out=V_(34, 35), in_=V_(26, 34), axis=AX.X, op=ALU.max), reads=["rt26"], writes=["rt34"])
            P.op("dve", lambda e: e.tensor_scalar(V_(80, 88), V_(26, 34), V_(34, 35), None, op0=ALU.is_equal), reads=["rt26", "rt34"], writes=["rt80"])
            P.op("act", lambda e: e.activation(V_(35, 36), V_(34, 35), AF.Exp, bias=V_(17, 18)), reads=["rt34", "rt17"], writes=["rt35"])
            P.op("dve", lambda e: e.tensor_scalar(V_(36, 37), V_(35, 36), 1.0, None, op0=ALU.add), reads=["rt35"], writes=["rt36"])
            P.op("dve", lambda e: e.reciprocal(V_(36, 37), V_(36, 37)), reads=["rt36"], writes=["rt36"])
            P.op("dve", lambda e, i=i: e.tensor_tensor(out=GATES[:, i, 0:1], in0=V_(36, 37), in1=V_(3, 4), op=ALU.mult), reads=["rt36", "rt3"], writes=["GATES"])
            P.op("dve", lambda e, i=i: e.tensor_tensor(out=GATES[:, i, 1:2], in0=V_(3, 4), in1=GATES[:, i, 0:1], op=ALU.subtract), reads=["rt3"], writes=["GATES"])
            P.op("dve", lambda e, i=i: e.tensor_tensor(out=SEL1[:, i, :].rearrange("p (g x) -> p g x", x=8), in0=V_(4, 8)[:, :, None].to_broadcast([128, 4, 8]), in1=V_(18, 26)[:, None, :].to_broadcast([128, 4, 8]), op=ALU.mult),
                 reads=["rt4", "rt18"], writes=["SEL1"])
            P.op("dve", lambda e, i=i: e.tensor_tensor(out=SEL2[:, i, :].rearrange("p (g x) -> p g x", x=8), in0=V_(4, 8)[:, :, None].to_broadcast([128, 4, 8]), in1=V_(80, 88)[:, None, :].to_broadcast([128, 4, 8]), op=ALU.mult),
                 reads=["rt4", "rt80"], writes=["SEL2"])
        gens = [tile_gen(i) for i in range(NT)]
        next(gens[0])
        for i in range(NT):
            if i + 1 < NT:
                next(gens[i + 1])
            next(gens[i])
            next(gens[i], None)
        if "s_sel" in dbg:
            P.dma("sp", d_sel[0], SEL1[:], reads=["SEL1"])
            P.dma("sp", d_sel[1], SEL2[:], reads=["SEL2"])
            P.dma("sp", d_gates, GATES[:], reads=["GATES"])
        P.phase_end()

        P.phase_begin()
        W1I = P.sb("W1I", [128, NBLK, 8], I32)
        W2I = P.sb("W2I", [128, NBLK, 4], I32)
        DESTI = P.sb("DESTI", [128, NT, 2], I32)
        P.phase_begin()
        SELS = P.sb("SELS", [128, NT, 32], F32)
        CUM = P.sb("CUM", [128, NT + 1, 32], F32)
        P.op("dve", lambda e: e.tensor_tensor(out=SELS[:], in0=SEL1[:], in1=SEL2[:], op=ALU.add), reads=["SEL1", "SEL2"], writes=["SELS"])
        P.op("pool", lambda e: e.memset(CUM[:, 0, :], 0.0), writes=["CUM"])
        for i in range(NT):
            P.op("dve", lambda e, i=i: e.tensor_tensor(out=CUM[:, i + 1, :], in0=CUM[:, i, :], in1=SELS[:, i, :], op=ALU.add), reads=["SELS"], writes=["CUM"])
        onesf = P.sb("onesf", [128, 128], F32)
        UT = P.sb("UT", [128, 128], F32)
        P.op("pool", lambda e: e.memset(onesf[:], 1.0), writes=["onesf"])
        P.op("dve", lambda e: e.tensor_scalar(UT[:], io[:], pid[:, 0:1], None, op0=ALU.is_gt), reads=["io", "pid"], writes=["UT"])
        pq = Rot([(P.ps("pq", [128, 512], F32), "pq%d" % i) for i in range(2)])
        pqt, pqn = pq.next()
        P.op("pe", lambda e, pqt=pqt: e.matmul(pqt[:, 0:32], onesf[:], CUM[:, NT, :], start=True, stop=True), reads=["onesf", "CUM"], writes=[pqn])
        ms = P.sb("ms", [128, 512], F32)
        cntE = ms[:, 0:32]
        nblk = ms[:, 32:64]
        pendA = ms[:, 64:96]
        pendB = ms[:, 96:128]
        pst = ms[:, 128:160]
        thr = ms[:, 160:192]
        j32 = ms[:, 192:224]
        NM = S // 512 + 1
        P.op("dve", lambda e, pqt=pqt: e.tensor_copy(cntE, pqt[:, 0:32]), reads=[], writes=[pqn, "cntE"])
        P.op("dve", lambda e: e.tensor_scalar(thr[:, 0:NM], io[:, 0:NM], 512.0, None, op0=ALU.mult), reads=["io"], writes=["thr"])
        P.op("pool", lambda e: e.memset(nblk, 0.0), writes=["nblk"])
        for ex in range(32):
            P.op("dve", lambda e, ex=ex: e.tensor_scalar(j32[:, 0:NM], thr[:, 0:NM], cntE[:, ex:ex + 1], 0.0, op0=ALU.is_lt, op1=ALU.add, accum_out=nblk[:, ex:ex + 1]),
                 reads=["thr", "cntE"], writes=["j32", "nblk"])
        src, srcn, dst, dstn = nblk, "nblk", pendA, "pendA"
        for d_ in (1, 2, 4, 8, 16):
            P.op("dve", lambda e, src=src, dst=dst, d_=d_: e.tensor_copy(dst[:, 0:d_], src[:, 0:d_]), reads=[srcn], writes=[dstn])
            P.op("dve", lambda e, src=src, dst=dst, d_=d_: e.tensor_tensor(out=dst[:, d_:32], in0=src[:, d_:32], in1=src[:, 0:32 - d_], op=ALU.add), reads=[srcn], writes=[dstn])
            if dstn == "pendA":
                src, srcn, dst, dstn = pendA, "pendA", pendB, "pendB"
            else:
                src, srcn, dst, dstn = pendB, "pendB", pendA, "pendA"
        pend, pendn = src, srcn
        P.op("dve", lambda e, pend=pend: e.tensor_tensor(out=pst, in0=pend, in1=nblk, op=ALU.subtract), reads=[pendn, "nblk"], writes=["pst"])
        P.op("dve", lambda e: e.tensor_scalar(pst, pst, 512.0, None, op0=ALU.mult), reads=[], writes=["pst"])
        BE = P.sb("BE", [128, NBLK], F32)
        P.op("pool", lambda e: e.memset(BE[:], 0.0), writes=["BE"])
        for b in range(NBLK):
            P.op("dve", lambda e, b=b, pend=pend: e.tensor_scalar(j32, pend, float(b), 0.0, op0=ALU.is_le, op1=ALU.add, accum_out=BE[:, b:b + 1]), reads=[pendn], writes=["j32", "BE"])
        P.op("dve", lambda e: e.tensor_scalar(BE[:], BE[:], 31.0, None, op0=ALU.min), reads=[], writes=["BE"])
        iotaK = P.sb("iotaK", [128, 8], F32)
        P.op("dve", lambda e: e.tensor_scalar(iotaK[:], io[:, 0:8], 128.0, pid[:, 0:1], op0=ALU.mult, op1=ALU.add), reads=["io", "pid"], writes=["iotaK"])
        WF = P.sb("WF", [128, NBLK, 8], F32)
        BEs = P.sb("BEs", [128, NBLK], F32)
        P.op("dve", lambda e: e.tensor_scalar(BEs[:], BE[:], 1024.0, float(l * 32 * 1024), op0=ALU.mult, op1=ALU.add), reads=["BE"], writes=["BEs"])
        P.op("dve", lambda e: e.tensor_tensor(out=WF[:], in0=BEs[:, :, None].to_broadcast([128, NBLK, 8]), in1=iotaK[:, None, :].to_broadcast([128, NBLK, 8]), op=ALU.add), reads=["BEs", "iotaK"], writes=["WF"])
        P.op("dve", lambda e: e.tensor_copy(W1I[:], WF[:]), reads=["WF"], writes=["W1I"])
        P.op("dve", lambda e: e.tensor_scalar(BEs[:], BE[:], 512.0, float(l * 32 * 512), op0=ALU.mult, op1=ALU.add), reads=["BE", "WF"], writes=["BEs"])
        P.op("dve", lambda e: e.tensor_tensor(out=WF[:, :, 0:4], in0=BEs[:, :, None].to_broadcast([128, NBLK, 4]), in1=iotaK[:, None, 0:4].to_broadcast([128, NBLK, 4]), op=ALU.add), reads=["BEs", "iotaK", "W1I"], writes=["WF"])
        P.op("dve", lambda e: e.tensor_copy(W2I[:], WF[:, :, 0:4]), reads=["WF"], writes=["W2I"])
        DEST = P.sb("DEST", [128, NT, 2], F32)
        P.op("pool", lambda e: e.memset(DEST[:], 0.0), writes=["DEST"])
        tq = Rot([(P.sb("tq", [128, 32], F32), "tq%d" % i) for i in range(2)])
        for i in range(NT):
            pqt, pqn = pq.next()
            tqt, tqn = tq.next()
            P.op("pe", lambda e, pqt=pqt, i=i: e.matmul(pqt[:, 0:32], UT[:], SELS[:, i, :], start=True, stop=False), reads=["UT", "SELS"], writes=[pqn])
            P.op("pe", lambda e, pqt=pqt, i=i: e.matmul(pqt[:, 0:32], onesf[:], CUM[:, i, :], start=False, stop=True), reads=["onesf", "CUM"], writes=[pqn])
            P.op("dve", lambda e, pqt=pqt, tqt=tqt: e.tensor_tensor(out=tqt[:], in0=pqt[:, 0:32], in1=pst, op=ALU.add), reads=["pst"], writes=[pqn, tqn])
            for sl, SEL, seln in ((0, SEL1, "SEL1"), (1, SEL2, "SEL2")):
                P.op("dve", lambda e, tqt=tqt, i=i, sl=sl, SEL=SEL: e.scalar_tensor_tensor(out=j32, in0=tqt[:], scalar=1.0, in1=SEL[:, i, :], op0=ALU.mult, op1=ALU.mult, accum_out=DEST[:, i, sl:sl + 1]),
                     reads=[tqn, seln], writes=["j32", "DEST"])
        P.op("dve", lambda e: e.tensor_copy(DESTI[:], DEST[:]), reads=["DEST"], writes=["DESTI"])
        if "s_dest" in dbg:
            P.dma("sp", d_dest, DESTI[:], reads=["DESTI"])
            P.dma("sp", d_be, BE[:], reads=["BE"])
        zb = P.sb("zb", [128, 4, D], BF16)
        P.op("pool", lambda e: e.memset(zb[:], 0.0), writes=["zb"])
        for b in range(NBLK):
            P.dma("sp", scr["buf"][b * 512:(b + 1) * 512, :].rearrange("(s p) d -> p s d", p=128), zb[:], reads=["zb"], writes=["s_buf"])
        h2l = Rot([(P.sb("h2l", [128, D], BF16), "h2l%d" % i) for i in range(3)])
        for i in range(NT):
            ht, htn = h2l.next()
            P.dma("sp", ht[:], scr["h2"][i * 128:(i + 1) * 128, :], reads=["s_h2"], writes=[htn])
            for sl in range(2):
                P.op("pool", lambda e, ht=ht, i=i, sl=sl: e.indirect_dma_start(out=scr["buf"], out_offset=bass.IndirectOffsetOnAxis(ap=DESTI[:, i, sl:sl + 1], axis=0), in_=ht[:], in_offset=None),
                     reads=[htn, "DESTI"], writes=["s_buf"], dma=True)
        P.phase_end()
        P.phase_begin()
        w1v = w1.rearrange("l r n -> (l r) n")
        w3v = w3.rearrange("l r n -> (l r) n")
        w2v = w2.rearrange("l r n -> (l r) n")
        w1f = Rot([(P.sb("w1f", [128, KC, 512], F32), "w1f%d" % i) for i in range(1)])
        w3f = Rot([(P.sb("w3f", [128, KC, 512], F32), "w3f%d" % i) for i in range(1)])
        w2f = Rot([(P.sb("w2f", [128, 4, D], F32), "w2f%d" % i) for i in range(1)])
        w1b = Rot([(P.sb("w1b", [128, KC, 512], BF16), "w1b%d" % i) for i in range(2)])
        w3b = Rot([(P.sb("w3b", [128, KC, 512], BF16), "w3b%d" % i) for i in range(2)])
        w2b = Rot([(P.sb("w2b", [128, 4, D], BF16), "w2b%d" % i) for i in range(2)])
        hbs = Rot([(P.sb("hb", [128, 4, D], BF16), "hb%d" % i) for i in range(2)])
        hTs = Rot([(P.sb("hT", [128, KC, 512], BF16), "hT%d" % i) for i in range(2)])
        sgs = Rot([(P.sb("sg", [128, 512], F32), "sg%d" % i) for i in range(2)])
        aTs = Rot([(P.sb("aT", [128, 4, 512], BF16), "aT%d" % i) for i in range(2)])
        ybs = Rot([(P.sb("yb", [128, 512], F32), "yb%d" % i) for i in range(4)])
        pT5 = Rot([(P.ps("pT5", [128, KC, 128], BF16), "pT5%d" % i) for i in range(2)])
        ph1 = Rot([(P.ps("ph1", [128, 512], F32), "ph1%d" % i) for i in range(1)])
        ph3 = Rot([(P.ps("ph3", [128, 512], F32), "ph3%d" % i) for i in range(1)])
        py = Rot([(P.ps("py", [128, 512], F32), "py%d" % i) for i in range(2)])
        for b in range(NBLK):
            a1, a1n = w1f.next()
            a3, a3n = w3f.next()
            a2, a2n = w2f.next()
            for kc in range(KC):
                P.op("pool", lambda e, a1=a1, b=b, kc=kc: e.indirect_dma_start(out=a1[:, kc, :], out_offset=None, in_=w1v, in_offset=bass.IndirectOffsetOnAxis(ap=W1I[:, b, kc:kc + 1], axis=0)),
                     reads=["W1I"], writes=[a1n], dma=True)
                P.op("pool", lambda e, a3=a3, b=b, kc=kc: e.indirect_dma_start(out=a3[:, kc, :], out_offset=None, in_=w3v, in_offset=bass.IndirectOffsetOnAxis(ap=W1I[:, b, kc:kc + 1], axis=0)),
                     reads=["W1I"], writes=[a3n], dma=True)
            for dc in range(4):
                P.op("pool", lambda e, a2=a2, b=b, dc=dc: e.indirect_dma_start(out=a2[:, dc, :], out_offset=None, in_=w2v, in_offset=bass.IndirectOffsetOnAxis(ap=W2I[:, b, dc:dc + 1], axis=0)),
                     reads=["W2I"], writes=[a2n], dma=True)
            hb, hbn = hbs.next()
            P.dma("sp", hb[:], scr["buf"][b * 512:(b + 1) * 512, :].rearrange("(s p) d -> p s d", p=128), reads=["s_buf"], writes=[hbn])
            b1, b1n = w1b.next()
            b3, b3n = w3b.next()
            b2, b2n = w2b.next()
            P.op("act", lambda e, a1=a1, b1=b1: e.copy(b1[:], a1[:]), reads=[a1n], writes=[b1n])
            P.op("dve", lambda e, a3=a3, b3=b3: e.tensor_copy(b3[:], a3[:]), reads=[a3n], writes=[b3n])
            P.op("dve", lambda e, a2=a2, b2=b2: e.tensor_copy(b2[:], a2[:]), reads=[a2n], writes=[b2n])
            hT, hTn = hTs.next()
            for sub in range(4):
                ptt, ptn = pT5.next()
                for kc in range(KC):
                    P.op("pe", lambda e, ptt=ptt, hb=hb, sub=sub, kc=kc: e.transpose(ptt[:, kc, :], hb[:, sub, kc * 128:(kc + 1) * 128], identb[:]), reads=[hbn, "identb"], writes=[ptn])
                if sub % 2 == 0:
                    P.op("act", lambda e, ptt=ptt, hT=hT, sub=sub: e.copy(hT[:, :, sub * 128:(sub + 1) * 128], ptt[:]), reads=[], writes=[ptn, hTn])
                else:
                    P.op("dve", lambda e, ptt=ptt, hT=hT, sub=sub: e.tensor_copy(hT[:, :, sub * 128:(sub + 1) * 128], ptt[:]), reads=[], writes=[ptn, hTn])
            aT, aTn = aTs.next()
            for dc in range(4):
                p1, p1n = ph1.next()
                p3, p3n = ph3.next()
                sg, sgn = sgs.next()
                for kc in range(KC):
                    P.op("pe", lambda e, p1=p1, b1=b1, hT=hT, kc=kc, dc=dc: e.matmul(p1[:], b1[:, kc, dc * 128:(dc + 1) * 128], hT[:, kc, :], start=(kc == 0), stop=(kc == KC - 1)), reads=[b1n, hTn], writes=[p1n])
                for kc in range(KC):
                    P.op("pe", lambda e, p3=p3, b3=b3, hT=hT, kc=kc, dc=dc: e.matmul(p3[:], b3[:, kc, dc * 128:(dc + 1) * 128], hT[:, kc, :], start=(kc == 0), stop=(kc == KC - 1)), reads=[b3n, hTn], writes=[p3n])
                P.op("act", lambda e, p1=p1, sg=sg: e.activation(sg[:], p1[:], AF.Silu), reads=[], writes=[p1n, sgn])
                P.op("dve", lambda e, p3=p3, sg=sg, aT=aT, dc=dc: e.tensor_tensor(out=aT[:, dc, :], in0=p3[:], in1=sg[:], op=ALU.mult), reads=[sgn], writes=[p3n, aTn])
            for sub in range(4):
                for hf in range(2):
                    pyt, pyn = py.next()
                    yb, ybn = ybs.next()
                    for dc in range(4):
                        P.op("pe", lambda e, pyt=pyt, aT=aT, b2=b2, dc=dc, sub=sub, hf=hf: e.matmul(pyt[:], aT[:, dc, sub * 128:(sub + 1) * 128], b2[:, dc, hf * 512:(hf + 1) * 512], start=(dc == 0), stop=(dc == 3)), reads=[aTn, b2n], writes=[pyn])
                    if hf == 0:
                        P.op("act", lambda e, pyt=pyt, yb=yb: e.copy(yb[:], pyt[:]), reads=[], writes=[pyn, ybn])
                    else:
                        P.op("dve", lambda e, pyt=pyt, yb=yb: e.tensor_copy(yb[:], pyt[:]), reads=[], writes=[pyn, ybn])
                    P.dma("sp", scr["ybuf"][b * 512 + sub * 128:b * 512 + (sub + 1) * 128, hf * 512:(hf + 1) * 512], yb[:], reads=[ybn], writes=["s_ybuf"])
        P.phase_end()
        P.phase_begin()
        g2B = P.sb("g2B", [128, D], F32)
        fgB = P.sb("fgB", [128, D], F32)
        P.dma("sp", g2B[:], d_modB[:, 5 * D:6 * D], reads=["s_modB"], writes=["g2B"])
        P.dma("sp", fgB[:], final_gB, writes=["fgB"])
        y1s = Rot([(P.sb("y1", [128, D], F32), "y1%d" % i) for i in range(2)])
        y2s = Rot([(P.sb("y2", [128, D], F32), "y2%d" % i) for i in range(2)])
        xls = Rot([(P.sb("xl", [128, D], F32), "xl%d" % i) for i in range(2)])
        xos = Rot([(P.sb("xo", [128, D], F32), "xo%d" % i) for i in range(2)])
        junk5 = P.sb("junk5", [128, D], F32)
        st5 = P.sb("st5", [128, 4], F32)
        last = (l == NL - 1)
        for i in range(NT):
            r0 = i * 128
            y1, y1n = y1s.next()
            y2, y2n = y2s.next()
            xl, xln = xls.next()
            xo, xon = xos.next()
            P.op("pool", lambda e, y1=y1, i=i: e.indirect_dma_start(out=y1[:], out_offset=None, in_=scr["ybuf"], in_offset=bass.IndirectOffsetOnAxis(ap=DESTI[:, i, 0:1], axis=0)),
                 reads=["DESTI", "s_ybuf"], writes=[y1n], dma=True)
            P.op("pool", lambda e, y2=y2, i=i: e.indirect_dma_start(out=y2[:], out_offset=None, in_=scr["ybuf"], in_offset=bass.IndirectOffsetOnAxis(ap=DESTI[:, i, 1:2], axis=0)),
                 reads=["DESTI", "s_ybuf"], writes=[y2n], dma=True)
            P.dma("sp", xl[:], scr["xmid"][r0:r0 + 128, :], reads=["s_xmid"], writes=[xln])
            P.op("dve", lambda e, y1=y1, i=i: e.tensor_scalar(y1[:], y1[:], GATES[:, i, 0:1], None, op0=ALU.mult), reads=["GATES"], writes=[y1n])
            P.op("dve", lambda e, y1=y1, y2=y2, i=i: e.scalar_tensor_tensor(out=y1[:], in0=y2[:], scalar=GATES[:, i, 1:2], in1=y1[:], op0=ALU.mult, op1=ALU.add), reads=["GATES", y2n], writes=[y1n])
            P.op("pool", lambda e, y1=y1: e.tensor_tensor(out=y1[:], in0=y1[:], in1=g2B[:], op=ALU.mult), reads=["g2B"], writes=[y1n])
            P.op("pool", lambda e, y1=y1, xl=xl, xo=xo: e.tensor_tensor(out=xo[:], in0=y1[:], in1=xl[:], op=ALU.add), reads=[y1n, xln], writes=[xon])
            if not last:
                P.dma("sp", x_dst[r0:r0 + 128, :], xo[:], reads=[xon], writes=["xdst"])
            else:
                P.op("act", lambda e, xo=xo: e.activation(junk5[:], xo[:], AF.Square, accum_out=st5[:, 0:1]), reads=[xon], writes=["junk5", "st5a"])
                P.op("act", lambda e: e.activation(st5[:, 1:2], st5[:, 0:1], AF.Sqrt, bias=1e-6, scale=1.0 / D), reads=["st5a"], writes=["st5b"])
                P.op("dve", lambda e: e.reciprocal(st5[:, 1:2], st5[:, 1:2]), reads=["st5b"], writes=["st5b"])
                P.op("dve", lambda e, xo=xo: e.scalar_tensor_tensor(out=xo[:], in0=xo[:], scalar=st5[:, 1:2], in1=fgB[:], op0=ALU.mult, op1=ALU.mult), reads=["st5b", "fgB"], writes=[xon])
                P.dma("sp", y_out[r0:r0 + 128, :], xo[:], reads=[xon], writes=["y"])
        P.phase_end()
        P.phase_end()
        P.phase_end()

    P.emit()
    return nc, P


def prep_inputs(inp, b, NL):
    f = lambda a: np.ascontiguousarray(np.asarray(a, dtype=np.float32))
    rep = lambda a: f(np.broadcast_to(np.asarray(a)[:, None, :], (a.shape[0], 128, a.shape[1])))
    d = {}
    d["x"] = f(inp["x"][b])
    d["c_col"] = f(np.asarray(inp["c"][b]).reshape(8, 128).T)
    d["ada_w"] = f(inp["ada_w"][:NL])
    d["ada_bB"] = rep(inp["ada_b"][:NL])
    d["n1g_col"] = f(np.asarray(inp["norm1_g"][:NL]).reshape(NL, 8, 128).transpose(0, 2, 1))
    d["w_in"] = f(inp["w_in"][:NL])
    d["conv_wT"] = f(np.asarray(inp["conv_w"][:NL]).transpose(0, 2, 1).reshape(NL, 2, 128, 31).transpose(0, 2, 1, 3))
    d["conv_bB"] = rep(inp["conv_b"][:NL])
    d["ln_gB"] = rep(inp["conv_ln_g"][:NL])
    d["ln_bB"] = rep(inp["conv_ln_b"][:NL])
    d["w_out"] = f(inp["w_out"][:NL])
    d["n2gB"] = rep(inp["norm2_g"][:NL])
    d["rw"] = f(np.concatenate([inp["router_group_w"][:NL], inp["router_expert_w"][:NL]], axis=-1))
    d["rbB"] = rep(np.concatenate([inp["router_group_b"][:NL], inp["router_expert_b"][:NL]], axis=-1))
    d["w1"] = f(np.asarray(inp["expert_w1"][:NL]).reshape(NL, 32 * 1024, 512))
    d["w3"] = f(np.asarray(inp["expert_w3"][:NL]).reshape(NL, 32 * 1024, 512))
    d["w2"] = f(np.asarray(inp["expert_w2"][:NL]).reshape(NL, 32 * 512, 1024))
    d["final_gB"] = f(np.broadcast_to(np.asarray(inp["final_g"])[None, :], (128, 1024)))
    d["mgB"] = rep(inp["moba_norm_g"][:NL])
    d["dgB"] = rep(inp["dsa_norm_g"][:NL])
    return d


_CACHE = {}


def kernel(**inputs):
    S, NL, NB_ = 8192, 2, 4
    if "nc" not in _CACHE:
        _CACHE["nc"] = build(S, NL)[0]
    nc = _CACHE["nc"]
    shared = prep_inputs(inputs, 0, NL)
    in_maps = []
    for b in range(NB_):
        d = dict(shared)
        d["x"] = np.ascontiguousarray(np.asarray(inputs["x"][b], dtype=np.float32))
        d["c_col"] = np.ascontiguousarray(np.asarray(inputs["c"][b], dtype=np.float32).reshape(8, 128).T)
        in_maps.append(d)
    res = run_bass_kernel_spmd(nc, in_maps, core_ids=list(range(NB_)))
    return np.stack([np.asarray(r["y"], dtype=np.float32) for r in res.results], axis=0)
```

```python
import types
import numpy as np
from contextlib import ExitStack
import concourse.bass as bass
import concourse.mybir as mybir
from concourse.bass_utils import run_bass_kernel_spmd

F32 = mybir.dt.float32
BF16 = mybir.dt.bfloat16
I32 = mybir.dt.int32
ALU = mybir.AluOpType
AF = mybir.ActivationFunctionType
AX = mybir.AxisListType

NDMASEM = 8
D = 1024
KC = 8
NEG = -30000.0
NCW = 3400 + 390 + 390
NIT = 10


def _freeze(fn):
    if fn.__closure__ is None:
        return fn
    cells = []
    for c in fn.__closure__:
        try:
            cells.append(types.CellType(c.cell_contents))
        except ValueError:
            cells.append(c)
    g = types.FunctionType(fn.__code__, fn.__globals__, fn.__name__, fn.__defaults__, tuple(cells))
    g.__kwdefaults__ = fn.__kwdefaults__
    return g


class Prog:
    ENGS = ("pe", "act", "dve", "pool", "sp")

    def __init__(self, nc):
        self.nc = nc
        self.ops = []
        self.state = {}
        self.es = ExitStack()
        self.ph = []
        self.pending = {e: None for e in self.ENGS}
        self.lastc = {e: None for e in self.ENGS}
        self.lastd = {}
        self.dcount = {e: 0 for e in self.ENGS}
        self.uid = 0

    def sb(self, name, shape, dt, glob=False):
        self.uid += 1
        st = self.es if (glob or not self.ph) else self.ph[-1]
        return st.enter_context(self.nc.sbuf_tensor("%s_%d" % (name, self.uid), list(shape), dt))

    def ps(self, name, shape, dt):
        self.uid += 1
        st = self.es if not self.ph else self.ph[-1]
        return st.enter_context(self.nc.psum_tensor("%s_%d" % (name, self.uid), list(shape), dt))

    def phase_begin(self):
        self.ph.append(ExitStack())

    def phase_end(self):
        fence = set()
        for e in self.ENGS:
            if self.lastc[e] is not None:
                fence.add(self.lastc[e])
        fence.update(self.lastd.values())
        for e in self.ENGS:
            self.pending[e] = set(fence) | (self.pending[e] or set())
        self.ph.pop().close()

    def op(self, eng, fn, reads=(), writes=(), dma=False):
        i = len(self.ops)
        deps = set()
        rawset = set()
        for r in reads:
            st = self.state.setdefault(r, [None, []])
            if st[0] is not None:
                deps.add(st[0])
                rawset.add(st[0])
        for w in writes:
            st = self.state.setdefault(w, [None, []])
            if st[0] is not None:
                deps.add(st[0])
            deps.update(st[1])
        for r in reads:
            self.state[r][1].append(i)
        for w in writes:
            st = self.state[w]
            st[0] = i
            st[1] = []
        if self.pending[eng]:
            deps |= self.pending[eng]
            rawset |= self.pending[eng]
            self.pending[eng] = None
        deps.discard(i)
        if dma:
            n = self.dcount[eng]
            self.dcount[eng] += 1
            self.lastd[(eng, n % NDMASEM)] = i
        else:
            self.lastc[eng] = i
        self.ops.append(dict(eng=eng, fn=_freeze(fn), deps=deps, raw=rawset, dma=dma, sig=dma))
        return i

    def dma(self, eng, out, in_, reads=(), writes=(), **kw):
        return self.op(eng, lambda e: e.dma_start(out=out, in_=in_, **kw), reads, writes, dma=True)

    def emit(self):
        nc = self.nc
        ops = self.ops
        for i, o in enumerate(ops):
            keep = set()
            for j in o["deps"]:
                pj = ops[j]
                if (not pj["dma"]) and (not o["dma"]) and pj["eng"] == o["eng"]:
                    if o["eng"] == "pe":
                        continue
                keep.add(j)
            o["deps"] = keep
        for o in ops:
            for j in o["deps"]:
                ops[j]["sig"] = True
        LIM = 30000
        DLIM = 1800
        cnt = {e: 0 for e in self.ENGS}
        dcnt = {e: 0 for e in self.ENGS}
        dsemcnt = {}
        dprev = {}
        for o in ops:
            e = o["eng"]
            if o["dma"]:
                n = dcnt[e]
                dcnt[e] += 1
                slot = n % NDMASEM
                k = dsemcnt.get((e, slot), 0)
                dsemcnt[(e, slot)] = k + 1
                o["prev"] = dprev.get((e, slot))
                o["semkey"] = ("d", e, slot, k // DLIM)
                o["semval"] = 16 * (k % DLIM + 1)
                dprev[(e, slot)] = (o["semkey"], o["semval"])
            elif o["sig"]:
                n = cnt[e]
                cnt[e] += 1
                o["semkey"] = ("c", e, n // LIM)
                o["semval"] = n % LIM + 1
        es = self.es
        sems = {}
        finals = {}
        for o in ops:
            if "semkey" in o:
                k = o["semkey"]
                if k not in sems:
                    sems[k] = es.enter_context(nc.semaphore("q_" + "_".join(str(x) for x in k)))
                finals[k] = max(finals.get(k, 0), o["semval"])
        self.nsems = len(sems)
        byeng = {e: [o for o in ops if o["eng"] == e] for e in self.ENGS}
        self.stats = {e: len(byeng[e]) for e in self.ENGS}

        def run(eng_name, engobj):
            waited = {}
            for o in byeng[eng_name]:
                need = {}
                for j in o["deps"]:
                    pj = ops[j]
                    k, v = pj["semkey"], pj["semval"]
                    if v > need.get(k, 0):
                        need[k] = v
                if o["dma"] and o["prev"] is not None:
                    k, v = o["prev"]
                    if v > need.get(k, 0):
                        need[k] = v
                for k, v in need.items():
                    if v > waited.get(k, 0):
                        engobj.wait_ge(sems[k], v)
                        waited[k] = v
                ins = o["fn"](engobj)
                if o["sig"]:
                    ins.then_inc(sems[o["semkey"]], 16 if o["dma"] else 1)
            if eng_name == "sp":
                for k, v in finals.items():
                    if v > waited.get(k, 0) and v > 0:
                        engobj.wait_ge(sems[k], v)

        with nc.Block() as block:
            @block.tensor
            def _(e):
                run("pe", e)

            @block.scalar
            def _(e):
                run("act", e)

            @block.vector
            def _(e):
                run("dve", e)

            @block.gpsimd
            def _(e):
                run("pool", e)

            @block.sync
            def _(e):
                run("sp", e)
        es.close()


_REGS = {}


def _breg(e, val):
    k = (id(e), val)
    if k not in _REGS:
        _REGS[k] = e.to_reg(val)
    return _REGS[k]


class Rot:
    def __init__(self, items):
        self.items = list(items)
        self.i = 0

    def next(self):
        it = self.items[self.i % len(self.items)]
        self.i += 1
        return it


FM_UNITS = [("qm", 0, 3, 128), ("km", 384, 3, 128), ("qd", 1664, 3, 128), ("kd", 2048, 3, 128),
            ("qi", 2816, 8, 64), ("ki", 3328, 1, 64)]
GLU_A = 1152
GLU_G = 1408


def build(S, NL, dbg=()):
    NT = S // 128
    NG = S // 512
    NB = S // 256
    nc = bass.Bass("TRN2", target_bir_lowering=False)

    def din(name, shape, dt=F32):
        return nc.dram_tensor(name, list(shape), dt, kind="ExternalInput").ap()

    def dscr(name, shape, dt):
        kind = "ExternalOutput" if name in dbg else "Internal"
        return nc.dram_tensor(name, list(shape), dt, kind=kind).ap()

    x_in = din("x", [S, D])
    c_col = din("c_col", [128, 8])
    ada_w = din("ada_w", [NL, D, 6 * D])
    ada_bB = din("ada_bB", [NL, 128, 6 * D])
    n1g_col = din("n1g_col", [NL, 128, 8])
    w_in = din("w_in", [NL, D, 3400])
    conv_wT = din("conv_wT", [NL, 128, 2, 31])
    conv_bB = din("conv_bB", [NL, 128, 256])
    ln_gB = din("ln_gB", [NL, 128, 256])
    ln_bB = din("ln_bB", [NL, 128, 256])
    w_out = din("w_out", [NL, D, D])
    n2gB = din("n2gB", [NL, 128, D])
    rw = din("rw", [NL, D, 36])
    rbBin = din("rbB", [NL, 128, 36])
    w1 = din("w1", [NL * 32 * D, 512])
    w3 = din("w3", [NL * 32 * D, 512])
    w2 = din("w2", [NL * 32 * 512, D])
    final_gB = din("final_gB", [128, D])
    mgB = din("mgB", [NL, 128, 384])
    dgB = din("dgB", [NL, 128, 384])
    y_out = nc.dram_tensor("y", [S, D], F32, kind="ExternalOutput").ap()

    scr = {}
    for nm, rows, M in [("qm", 384, 128), ("km", 384, 128), ("qd", 384, 128), ("kd", 384, 128), ("qi", 512, 64), ("ki", 64, 64)]:
        scr[nm] = dscr("s_" + nm, [rows, S], BF16)
    scr["u"] = dscr("s_u", [256, S + 32], BF16)
    scr["vm"] = dscr("s_vm", [S, 390], BF16)
    scr["vd"] = dscr("s_vd", [S, 390], BF16)
    scr["wi"] = dscr("s_wi", [S, 8], F32)
    scr["mix"] = dscr("s_mix", [S, D], BF16)
    NBLK_ = 2 * S // 512 + 32
    scr["xmid"] = dscr("s_xmid", [S, D], F32)
    scr["h2"] = dscr("s_h2", [S, D], BF16)
    scr["buf"] = dscr("s_buf", [NBLK_ * 512, D], BF16)
    scr["ybuf"] = dscr("s_ybuf", [NBLK_ * 512, D], F32)
    xbuf = [dscr("s_xbuf%d" % i, [S, D], F32) for i in range(2)]
    d_sel = [dscr("s_sel%d" % i, [128, NT, 32], F32) for i in range(2)]
    d_gates = dscr("s_gates", [128, NT, 2], F32)
    d_dest = dscr("s_dest", [128, NT, 2], I32)
    d_be = dscr("s_be", [128, NBLK_], F32)
    d_modB = dscr("s_modB", [128, 6 * D], F32)

    P = Prog(nc)
    identf = P.sb("identf", [128, 128], F32, glob=True)
    identb = P.sb("identb", [128, 128], BF16, glob=True)
    io = P.sb("io", [128, 128], F32, glob=True)
    pid = P.sb("pid", [128, 1], F32, glob=True)
    P.op("pool", lambda e: e.iota(io[:], pattern=[[1, 128]], base=0, channel_multiplier=0, allow_small_or_imprecise_dtypes=True), writes=["io"])
    P.op("pool", lambda e: e.iota(pid[:], pattern=[[0, 1]], base=0, channel_multiplier=1, allow_small_or_imprecise_dtypes=True), writes=["pid"])
    P.op("dve", lambda e: e.tensor_scalar(identf[:], io[:], pid[:, 0:1], None, op0=ALU.is_equal), reads=["io", "pid"], writes=["identf"])
    P.op("dve", lambda e: e.tensor_copy(identb[:], identf[:]), reads=["identf"], writes=["identb"])

    for l in range(NL):
        P.phase_begin()
        modB = P.sb("modB", [128, 6 * D], F32)
        P.phase_begin()
        cc = P.sb("cc", [128, 8], F32)
        sc = P.sb("sc", [128, 8], F32)
        screp = P.sb("screp", [128, 8, 128], F32)
        abB = P.sb("abB", [128, 6 * D], F32)
        P.dma("sp", cc[:], c_col, writes=["cc"])
        P.dma("sp", abB[:], ada_bB[l], writes=["abB"])
        P.op("act", lambda e: e.activation(sc[:], cc[:], AF.Silu), reads=["cc"], writes=["sc"])
        for kc in range(KC):
            P.op("dve", lambda e, kc=kc: e.tensor_scalar(screp[:, kc, :], io[:], 0.0, sc[:, kc:kc + 1], op0=ALU.mult, op1=ALU.add),
                 reads=["io", "sc"], writes=["screp"])
        awt = Rot([(P.sb("awt", [128, 8, 512], F32), "awt%d" % i) for i in range(2)])
        pm = Rot([(P.ps("pm", [128, 512], F32), "pm%d" % i) for i in range(2)])
        for fc in range(12):
            wt, wn = awt.next()
            pt, pn = pm.next()
            P.dma("sp", wt[:], ada_w[l, :, fc * 512:(fc + 1) * 512].rearrange("(kc p) n -> p kc n", p=128), writes=[wn])
            for kc in range(KC):
                P.op("pe", lambda e, kc=kc, wt=wt, pt=pt: e.matmul(pt[:], screp[:, kc, :], wt[:, kc, :], start=(kc == 0), stop=(kc == KC - 1)),
                     reads=["screp", wn], writes=[pn])
            P.op("dve", lambda e, pt=pt, fc=fc: e.tensor_tensor(out=modB[:, fc * 512:(fc + 1) * 512], in0=pt[:], in1=abB[:, fc * 512:(fc + 1) * 512], op=ALU.add),
                 reads=["abB"], writes=[pn, "modB"])
        P.dma("sp", d_modB, modB[:], reads=["modB"], writes=["s_modB"])
        P.phase_end()

        P.phase_begin()
        Wp = P.sb("Wp", [128, KC, NCW], BF16)
        sh1rep = P.sb("sh1rep", [128, KC, 128], F32)
        gmodT = P.sb("gmodT", [128, KC], F32)
        n1c = P.sb("n1c", [128, KC], F32)
        bB = P.sb("bB", [128, 3400], F32)
        bBv = P.sb("bBv", [128, 2, 6, 65], F32)
        biasT = P.sb("biasT", [128, 32], F32)
        P.dma("sp", n1c[:], n1g_col[l], writes=["n1c"])
        P.phase_begin()
        ptr = Rot([(P.ps("ptr", [128, 512], F32), "ptr%d" % i) for i in range(2)])
        for kc in range(KC):
            pt, pn = ptr.next()
            P.op("pe", lambda e, pt=pt, kc=kc: e.transpose(pt[:, 0:128], modB[:, kc * 128:(kc + 1) * 128], identf[:]), reads=["modB", "identf"], writes=[pn])
            P.op("act", lambda e, pt=pt, kc=kc: e.copy(sh1rep[:, kc, :], pt[:, 0:128]), reads=[], writes=[pn, "sh1rep"])
            pt, pn = ptr.next()
            P.op("pe", lambda e, pt=pt, kc=kc: e.transpose(pt[:, 0:128], modB[:, D + kc * 128:D + (kc + 1) * 128], identf[:]), reads=["modB", "identf"], writes=[pn])
            P.op("dve", lambda e, pt=pt, kc=kc: e.scalar_tensor_tensor(out=gmodT[:, kc:kc + 1], in0=pt[:, 0:1], scalar=1.0, in1=n1c[:, kc:kc + 1], op0=ALU.add, op1=ALU.mult),
                 reads=["n1c"], writes=[pn, "gmodT"])
        P.op("pool", lambda e: e.memset(Wp[:, :, 3400:NCW], 0.0), writes=["Wp"])
        wst = Rot([(P.sb("wst", [128, 3400], F32), "wst%d" % i) for i in range(2)])
        for kc in range(KC):
            wt, wn = wst.next()
            P.dma("sp", wt[:], w_in[l, kc * 128:(kc + 1) * 128, :], writes=[wn])
            P.op("dve", lambda e, wt=wt, kc=kc: e.tensor_scalar(Wp[:, kc, 0:3400], wt[:], gmodT[:, kc:kc + 1], None, op0=ALU.mult),
                 reads=[wn, "gmodT"], writes=["Wp"])
            for vi, c0 in ((0, 768), (1, 2432)):
                P.op("pool", lambda e, wt=wt, kc=kc, vi=vi, c0=c0: e.tensor_scalar(
                    Wp[:, kc, 3400 + vi * 390:3400 + (vi + 1) * 390].rearrange("p (h d) -> p h d", d=65)[:, :, 0:64],
                    wt[:, c0:c0 + 384].rearrange("p (h d) -> p h d", d=64), gmodT[:, kc:kc + 1], None, op0=ALU.mult),
                    reads=[wn, "gmodT"], writes=["Wp"])
        wbt = Rot([(P.sb("wbt", [128, KC, 512], F32), "wbt%d" % i) for i in range(2)])
        pbb = Rot([(P.ps("pbb", [128, 512], F32), "pbb%d" % i) for i in range(2)])
        for cc_ in range(7):
            c0 = cc_ * 512
            n = min(512, 3400 - c0)
            wt, wn = wbt.next()
            pt, pn = pbb.next()
            P.dma("sp", wt[:, :, 0:n], w_in[l, :, c0:c0 + n].rearrange("(kc p) n -> p kc n", p=128), writes=[wn])
            for kc in range(KC):
                P.op("pe", lambda e, kc=kc, wt=wt, pt=pt, n=n: e.matmul(pt[:, 0:n], sh1rep[:, kc, :], wt[:, kc, 0:n], start=(kc == 0), stop=(kc == KC - 1)),
                     reads=["sh1rep", wn], writes=[pn])
            P.op("act", lambda e, pt=pt, c0=c0, n=n: e.copy(bB[:, c0:c0 + n], pt[:, 0:n]), reads=[], writes=[pn, "bB"])
        P.op("pool", lambda e: e.memset(bBv[:], 1.0), writes=["bBv"])
        for vi, c0 in ((0, 768), (1, 2432)):
            P.op("dve", lambda e, vi=vi, c0=c0: e.tensor_copy(bBv[:, vi, :, 0:64], bB[:, c0:c0 + 384].rearrange("p (h d) -> p h d", d=64)),
                 reads=["bB"], writes=["bBv"])
        ucols = []
        for nm, c0, nu, M in FM_UNITS:
            for u in range(nu):
                ucols.append((nm, u, c0 + u * M, M))
        for u in range(2):
            ucols.append(("ca", u, GLU_A + u * 128, 128))
        for u in range(2):
            ucols.append(("cg", u, GLU_G + u * 128, 128))
        bidx = {}
        for k, (nm, u, c0, M) in enumerate(ucols):
            bidx[(nm, u)] = k
            pt, pn = ptr.next()
            P.op("pe", lambda e, pt=pt, c0=c0, M=M: e.transpose(pt[0:M, 0:128], bB[:, c0:c0 + M], identf[:]), reads=["bB", "identf"], writes=[pn])
            P.op("act", lambda e, pt=pt, k=k, M=M: e.copy(biasT[0:M, k:k + 1], pt[0:M, 0:1]), reads=[], writes=[pn, "biasT"])

        P.phase_end()
        xg = Rot([(P.sb("xg", [128, 4, D], F32), "xg%d" % i) for i in range(2)])
        xb = Rot([(P.sb("xb", [128, D], BF16), "xb%d" % i) for i in range(2)])
        xT = Rot([(P.sb("xT", [128, KC, 512], BF16), "xT%d" % i) for i in range(2)])
        junk = P.sb("junk", [128, D], F32)
        ss = Rot([(P.sb("ss", [128, 4], F32), "ss%d" % i) for i in range(2)])
        rs = Rot([(P.sb("rs", [128, 4], F32), "rs%d" % i) for i in range(2)])
        pT = Rot([(P.ps("pT", [128, KC, 128], BF16), "pT%d" % i) for i in range(2)])
        pu = Rot([(P.ps("pu", [128, 512], F32), "pu%d" % i) for i in range(4)])
        pv = Rot([(P.ps("pv", [128, 512], F32), "pv%d" % i) for i in range(2)])
        stg = Rot([(P.sb("stg", [128, 512], BF16), "stg%d" % i) for i in range(4)])
        sig = Rot([(P.sb("sig", [128, 512], F32), "sig%d" % i) for i in range(2)])
        stv = Rot([(P.sb("stv", [128, 390], BF16), "stv%d" % i) for i in range(3)])
        stw = Rot([(P.sb("stw", [128, 8], F32), "stw%d" % i) for i in range(2)])
        zt = P.sb("zt", [128, 32], BF16)
        P.op("pool", lambda e: e.memset(zt[:], 0.0), writes=["zt"])
        for h in range(2):
            P.dma("sp", scr["u"][h * 128:(h + 1) * 128, 0:32], zt[:], reads=["zt"], writes=["s_u"])

        def load_x(G):
            t, n = xg.next()
            P.dma("sp", t[:], (x_in if l == 0 else xbuf[(l - 1) % 2])[G * 512:(G + 1) * 512, :].rearrange("(j p) d -> p j d", p=128), reads=["xsrc"], writes=[n])
            return t, n

        nxt = load_x(0)
        for G in range(NG):
            xt_, xn = nxt
            if G + 1 < NG:
                nxt = load_x(G + 1)
            sst, ssn = ss.next()
            rst, rsn = rs.next()
            xTt, xTn = xT.next()
            for j in range(4):
                P.op("act", lambda e, j=j, xt_=xt_, sst=sst: e.activation(junk[:], xt_[:, j, :], AF.Square, accum_out=sst[:, j:j + 1]),
                     reads=[xn], writes=["junk", ssn])
            P.op("act", lambda e, sst=sst, rst=rst: e.activation(rst[:], sst[:], AF.Sqrt, bias=1e-6, scale=1.0 / D), reads=[ssn], writes=[rsn])
            P.op("dve", lambda e, rst=rst: e.reciprocal(rst[:], rst[:]), reads=[rsn], writes=[rsn])
            for j in range(4):
                xbt, xbn = xb.next()
                ptt, ptn = pT.next()
                P.op("dve", lambda e, j=j, xt_=xt_, xbt=xbt, rst=rst: e.tensor_scalar(xbt[:], xt_[:, j, :], rst[:, j:j + 1], None, op0=ALU.mult),
                     reads=[xn, rsn], writes=[xbn])
                for kc in range(KC):
                    P.op("pe", lambda e, kc=kc, xbt=xbt, ptt=ptt: e.transpose(ptt[:, kc, :], xbt[:, kc * 128:(kc + 1) * 128], identb[:]),
                         reads=[xbn, "identb"], writes=[ptn])
                P.op("act", lambda e, j=j, xTt=xTt, ptt=ptt: e.copy(xTt[:, :, j * 128:(j + 1) * 128], ptt[:]), reads=[], writes=[ptn, xTn])
            for nm, c0, nu, M in FM_UNITS:
                for u in range(nu):
                    put, pun = pu.next()
                    sgt, sgn = stg.next()
                    cs = c0 + u * M
                    for kc in range(KC):
                        P.op("pe", lambda e, kc=kc, put=put, cs=cs, M=M, xTt=xTt: e.matmul(put[0:M, :], Wp[:, kc, cs:cs + M], xTt[:, kc, :], start=(kc == 0), stop=(kc == KC - 1)),
                             reads=["Wp", xTn], writes=[pun])
                    k = bidx[(nm, u)]
                    P.op("act", lambda e, put=put, sgt=sgt, M=M, k=k: e.activation(sgt[0:M, :], put[0:M, :], AF.Identity, bias=biasT[0:M, k:k + 1]),
                         reads=["biasT"], writes=[pun, sgn])
                    P.dma("sp", scr[nm][u * M:(u + 1) * M, G * 512:(G + 1) * 512], sgt[0:M, :], reads=[sgn], writes=["s_" + nm])
            for u in range(2):
                pa, pan = pu.next()
                pg, pgn = pu.next()
                sgt, sgn = stg.next()
                sit, sin_ = sig.next()
                for kc in range(KC):
                    P.op("pe", lambda e, kc=kc, pa=pa, u=u, xTt=xTt: e.matmul(pa[:], Wp[:, kc, GLU_A + u * 128:GLU_A + (u + 1) * 128], xTt[:, kc, :], start=(kc == 0), stop=(kc == KC - 1)),
                         reads=["Wp", xTn], writes=[pan])
                for kc in range(KC):
                    P.op("pe", lambda e, kc=kc, pg=pg, u=u, xTt=xTt: e.matmul(pg[:], Wp[:, kc, GLU_G + u * 128:GLU_G + (u + 1) * 128], xTt[:, kc, :], start=(kc == 0), stop=(kc == KC - 1)),
                         reads=["Wp", xTn], writes=[pgn])
                kg = bidx[("cg", u)]
                ka = bidx[("ca", u)]
                P.op("act", lambda e, pg=pg, sit=sit, kg=kg: e.activation(sit[:], pg[:], AF.Sigmoid, bias=biasT[:, kg:kg + 1]),
                     reads=["biasT"], writes=[pgn, sin_])
                P.op("dve", lambda e, pa=pa, sit=sit, sgt=sgt, ka=ka: e.scalar_tensor_tensor(out=sgt[:], in0=pa[:], scalar=biasT[:, ka:ka + 1], in1=sit[:], op0=ALU.add, op1=ALU.mult),
                     reads=["biasT", sin_], writes=[pan, sgn])
                P.dma("sp", scr["u"][u * 128:(u + 1) * 128, 32 + G * 512:32 + (G + 1) * 512], sgt[:], reads=[sgn], writes=["s_u"])
            for j in range(4):
                r0 = G * 512 + j * 128
                for vi, nm in ((0, "vm"), (1, "vd")):
                    pvt, pvn = pv.next()
                    svt, svn = stv.next()
                    for kc in range(KC):
                        P.op("pe", lambda e, kc=kc, pvt=pvt, vi=vi, j=j, xTt=xTt: e.matmul(pvt[:, 0:390], xTt[:, kc, j * 128:(j + 1) * 128], Wp[:, kc, 3400 + vi * 390:3400 + (vi + 1) * 390], start=(kc == 0), stop=(kc == KC - 1)),
                             reads=["Wp", xTn], writes=[pvn])
                    P.op("dve", lambda e, pvt=pvt, svt=svt, vi=vi: e.tensor_tensor(out=svt[:], in0=pvt[:, 0:390], in1=bBv[:, vi].rearrange("p h d -> p (h d)"), op=ALU.add),
                         reads=["bBv"], writes=[pvn, svn])
                    P.dma("sp", scr[nm][r0:r0 + 128, :], svt[:], reads=[svn], writes=["s_" + nm])
                pvt, pvn = pv.next()
                swt, swn = stw.next()
                for kc in range(KC):
                    P.op("pe", lambda e, kc=kc, pvt=pvt, j=j, xTt=xTt: e.matmul(pvt[:, 0:8], xTt[:, kc, j * 128:(j + 1) * 128], Wp[:, kc, 3392:3400], start=(kc == 0), stop=(kc == KC - 1)),
                         reads=["Wp", xTn], writes=[pvn])
                P.op("dve", lambda e, pvt=pvt, swt=swt: e.tensor_tensor(out=swt[:], in0=pvt[:, 0:8], in1=bB[:, 3392:3400], op=ALU.add),
                     reads=["bB"], writes=[pvn, swn])
                P.dma("sp", scr["wi"][r0:r0 + 128, :], swt[:], reads=[swn], writes=["s_wi"])
        P.phase_end()
        P.phase_end()


        for kind in ("moba", "dsa"):
            P.phase_begin()
            qn, kn, vn = ("qm", "km", "vm") if kind == "moba" else ("qd", "kd", "vd")
            kT = P.sb("kT", [128, 3, S], BF16)
            V = P.sb("V", [128, NT, 390], BF16)
            gB = P.sb("gB", [128, 384], F32)
            for p in range(3):
                P.dma("sp", kT[:, p, :], scr[kn][p * 128:(p + 1) * 128, :], reads=["s_" + kn], writes=["kT"])
            for c8 in range(0, NT, 8):
                P.dma("sp", V[:, c8:c8 + 8, :], scr[vn][c8 * 128:(c8 + 8) * 128, :].rearrange("(c p) n -> p c n", p=128), reads=["s_" + vn], writes=["V"])
            P.dma("sp", gB[:], (mgB if kind == "moba" else dgB)[l], writes=["gB"])
            tri = P.sb("tri", [128, 128], BF16)
            P.op("dve", lambda e: e.tensor_scalar(tri[:], io[:], pid[:, 0:1], NEG, op0=ALU.is_gt, op1=ALU.mult), reads=["io", "pid"], writes=["tri"])
            nbuf = 2 if kind == "moba" else 1
            QW = 512 if kind == "moba" else 128
            qpads = [P.sb("qpad", [128, 6, QW], BF16) for _ in range(2)]
            for i in range(2):
                P.op("pool", lambda e, i=i: e.memset(qpads[i][:], 0.0), writes=["qpad%d" % i])
            qgs = Rot([(P.sb("qg", [128, 3, QW], BF16), "qg%d" % i) for i in range(2)])
            junk2 = P.sb("junk2", [128, 384], BF16)
            Sps = Rot([(P.ps("Sps", [128, 4, 128], F32), "Sps%d" % i) for i in range(3)])
            Ops = Rot([(P.ps("Ops", [128, 512], F32), "Ops%d" % i) for i in range(2)])
            if kind == "dsa":
                Xps = Rot([(P.ps("Xps", [128, 512], F32), "Xps%d" % i) for i in range(2)])
            else:
                pmisc = P.ps("pmisc", [128, 1024], BF16)
                pgate = P.ps("pgate", [128, 16, 32], F32)
            PTs = Rot([(P.sb("PT", [128, 4, 128], BF16), "PT%d" % i) for i in range(3 if kind == "moba" else 2)])
            osbs = Rot([(P.sb("osb", [128, 6, 65], F32), "osb%d" % i) for i in range(2 if kind == "moba" else 1)])
            ym = P.sb("ym", [128, 6, 64], F32)
            rden = P.sb("rden", [128, 6], F32)
            sst = P.sb("sst2", [128, 2], F32)
            mixo = Rot([(P.sb("mixo", [128, 384], BF16), "mixo%d" % i) for i in range(2 if kind == "moba" else 1)])
            if kind == "moba":
                ksum = P.sb("ksum", [128, 3, NB], F32)
                kmeanb = P.sb("kmeanb", [128, 3, 32], BF16)
                Eall = P.sb("Eall", [128, 32, 128], BF16)
                gate_sb = P.sb("gate_sb", [128, 6, 32], F32)
                mx = P.sb("mx", [128, 6, 8], F32)
                selb = P.sb("selb", [128, 6, 32], BF16)
                sbTs = Rot([(P.sb("sbT", [128, 6, 128], BF16), "sbT%d" % i) for i in range(2)])
                for (t_, n_) in sbTs.items:
                    P.op("pool", lambda e, t_=t_: e.memset(t_[:], 0.0), writes=[n_])
                for p in range(3):
                    P.op("dve", lambda e, p=p: e.tensor_reduce(out=ksum[:, p, :], in_=kT[:, p, :].rearrange("r (n k) -> r n k", k=256), axis=AX.X, op=ALU.add),
                         reads=["kT"], writes=["ksum"])
                P.op("pool", lambda e: e.memset(kmeanb[:], 0.0), writes=["kmeanb"])
                P.op("dve", lambda e: e.tensor_scalar(kmeanb[:, :, 0:NB], ksum[:], 1.0 / 256, None, op0=ALU.mult), reads=["ksum"], writes=["kmeanb"])
                P.op("pool", lambda e: e.memset(Eall[:], 0.0), writes=["Eall"])
                P.op("dve", lambda e: e.tensor_copy(Eall[0:32], identf[0:32, 0:32, None].to_broadcast([32, 32, 128])), reads=["identf"], writes=["Eall"])
                P.op("pool", lambda e: e.memset(gate_sb[:], -1e30), writes=["gate_sb"])
            else:
                kiT = P.sb("kiT", [128, S], BF16)
                P.op("pool", lambda e: e.memset(kiT[64:128, :], 0.0), writes=["kiT"])
                P.dma("sp", kiT[0:64, :], scr["ki"], reads=["s_ki"], writes=["kiT"])
                qis = Rot([(P.sb("qi", [128, 8, 128], BF16), "qi%d" % i) for i in range(2)])
                for (t_, n_) in qis.items:
                    P.op("pool", lambda e, t_=t_: e.memset(t_[:], 0.0), writes=[n_])
                wis = Rot([(P.sb("wi", [128, 4, 8], F32), "wi%d" % i) for i in range(2)])
                acc = P.sb("acc", [128, S], F32)
                dbs = Rot([(P.sb("dbias", [128, S], BF16), "dbias%d" % i) for i in range(2)])
                rls = Rot([(P.sb("rl", [128, 512], BF16), "rl%d" % i) for i in range(8)])
                dWs = Rot([(P.sb("dW", [128, 8, 128], BF16), "dW%d" % i) for i in range(1)])
                accp = P.ps("accp", [128, 512], F32)
                cntA = Rot([(P.sb("cntA", [128, 1], F32), "cntA%d" % i) for i in range(2)])
                tmpc = Rot([(P.sb("tmpc", [128, 1], F32), "tmpc%d" % i) for i in range(2)])
                triD = P.sb("triD", [128, 128], F32)
                P.op("dve", lambda e: e.tensor_scalar(triD[:], io[:], pid[:, 0:1], -1e30, op0=ALU.is_gt, op1=ALU.mult), reads=["io", "pid"], writes=["triD"])
                pw2 = P.sb("pw2", [128, NIT + 1], F32)
                for k in range(NIT + 1):
                    P.op("pool", lambda e, k=k: e.memset(pw2[:, k:k + 1], 2.0 ** -(k + 1)), writes=["pw2"])
                Wk = P.sb("Wk", [128, NIT + 1], F32)
                bs = P.sb("bs", [128, 8], F32)
                los = Rot([(P.sb("lo", [128, 1], F32), "lo%d" % i) for i in range(2)])
                mids = Rot([(P.sb("mid", [128, 1], F32), "mid%d" % i) for i in range(2)])
                cnts = Rot([(P.sb("cnt", [128, 1], F32), "cnt%d" % i) for i in range(2)])
                tts = Rot([(P.sb("tt", [128, 1], F32), "tt%d" % i) for i in range(2)])

            def load_q(G, idx):
                if not idx:
                    t, n = qgs.next()
                    P.dma("sp", t[:], scr[qn][:, G * QW:(G + 1) * QW].rearrange("(p r) t -> r p t", r=128), reads=["s_" + qn], writes=[n])
                    return [(t, n)]
                t2, n2 = None, None
                t3, n3 = wis.next()
                P.dma("sp", t3[:], scr["wi"][G * 512:(G + 1) * 512, :].rearrange("(j p) h -> p j h", p=128), reads=["s_wi"], writes=[n3])
                return [(t2, n2), (t3, n3)]

            grp = {}

            def load_att(G):
                cur = load_q(G, False)
                qg, qgn = cur[0]
                qpad = qpads[G % 2]
                qpn = "qpad%d" % (G % 2)
                P.op("act", lambda e, qpad=qpad, qg=qg: e.copy(qpad[0:64].rearrange("r (p two) t -> r p two t", two=2)[:, :, 0, :], qg[0:64, :, :]), reads=[qgn], writes=[qpn])
                P.op("dve", lambda e, qpad=qpad, qg=qg: e.tensor_copy(qpad[64:128].rearrange("r (p two) t -> r p two t", two=2)[:, :, 1, :], qg[64:128, :, :]), reads=[qgn], writes=[qpn])
                grp[G] = (qpad, qpn)

            tst = {}

            def pre(G, j):
                qt = 4 * G + j
                nch = qt + 1
                if kind == "moba":
                    if G not in grp:
                        load_att(G)
                    qpad, qpn = grp[G]
                else:
                    if j == 0:
                        cur = load_q(G, True)
                        tst["qi"] = cur
                    qi, qin = qis.next()
                    P.dma("sp", qi[0:64], scr["qi"][:, qt * 128:(qt + 1) * 128].rearrange("(h d) t -> d h t", d=64), reads=["s_qi"], writes=[qin])
                    wi, win = tst["qi"][1]
                b = qt // 2
                sbT = sbTn = dbias = dbn = None
                qt = 4 * G + j
                nch = qt + 1
                if kind == "moba":
                    b = qt // 2
                    sbT, sbTn = sbTs.next()
                    if b > 0:
                        for h in range(6):
                            P.op("pe", lambda e, h=h, j=j, qpad=qpad: e.matmul(pgate[:, h, :], qpad[:, h, j * 128:(j + 1) * 128], kmeanb[:, h // 2, :], start=True, stop=True),
                                 reads=[qpn, "kmeanb"], writes=["Xg"])
                        P.op("dve", lambda e, b=b: e.tensor_copy(gate_sb[:, :, 0:b], pgate[:, 0:6, 0:b]), reads=[], writes=["Xg", "gate_sb"])
                        for h in range(6):
                            P.op("dve", lambda e, h=h: e.max(out=mx[:, h, :], in_=gate_sb[:, h, :]), reads=["gate_sb"], writes=["mx"])
                        for h in range(6):
                            P.op("dve", lambda e, h=h: e.tensor_scalar(selb[:, h, :], gate_sb[:, h, :], mx[:, h, 2:3], NEG, op0=ALU.is_lt, op1=ALU.mult),
                                 reads=["gate_sb", "mx"], writes=["selb"])
                        for h in range(6):
                            P.op("pe", lambda e, h=h: e.transpose(pmisc[0:32, h * 128:(h + 1) * 128], selb[:, h, :], identb[:]), reads=["selb", "identb"], writes=["pmisc"])
                        P.op("act", lambda e, sbT=sbT: e.copy(sbT[0:32], pmisc[0:32, 0:768].rearrange("n (h t) -> n h t", t=128)), reads=[], writes=["pmisc", sbTn])
                else:
                    L = nch * 128
                    dbias, dbn = dbs.next()
                    dW, dWn = dWs.next()
                    for h in range(8):
                        P.op("dve", lambda e, dW=dW, wi=wi, h=h, j=j: e.tensor_scalar(dW[:, h, :], identf[:], wi[:, j, h:h + 1], None, op0=ALU.mult),
                             reads=["identf", win], writes=[dWn])
                    for cc in range((L + 511) // 512):
                        n = min(512, L - cc * 512)
                        rr = []
                        for h in range(8):
                            xp, xpn = Xps.next()
                            rl, rln = rls.next()
                            rr.append((rl, rln))
                            P.op("pe", lambda e, xp=xp, h=h, cc=cc, n=n, qi=qi, j=j: e.matmul(xp[:, 0:n], qi[:, h, :], kiT[:, cc * 512:cc * 512 + n], start=True, stop=True),
                                 reads=[qin, "kiT"], writes=[xpn])
                            if h % 3 != 2:
                                P.op("act", lambda e, xp=xp, rl=rl, n=n: e.activation(rl[:, 0:n], xp[:, 0:n], AF.Relu), reads=[], writes=[xpn, rln])
                            else:
                                P.op("dve", lambda e, xp=xp, rl=rl, n=n: e.tensor_scalar(rl[:, 0:n], xp[:, 0:n], 0.0, None, op0=ALU.max), reads=[], writes=[xpn, rln])
                        for h in range(8):
                            rl, rln = rr[h]
                            P.op("pe", lambda e, rl=rl, dW=dW, h=h, n=n: e.matmul(accp[:, 0:n], dW[:, h, :], rl[:, 0:n], start=(h == 0), stop=(h == 7)),
                                 reads=[rln, dWn], writes=["accp"])
                        if cc % 2 == 0:
                            P.op("act", lambda e, cc=cc, n=n: e.copy(acc[:, cc * 512:cc * 512 + n], accp[:, 0:n]), reads=[], writes=["accp", "acc"])
                        else:
                            P.op("dve", lambda e, cc=cc, n=n: e.tensor_copy(acc[:, cc * 512:cc * 512 + n], accp[:, 0:n]), reads=[], writes=["accp", "acc"])
                        yield
                    P.op("dve", lambda e, L=L: e.tensor_reduce(out=bs[:, 0:1], in_=acc[:, 0:L], axis=AX.X, op=ALU.max, apply_absolute_value=True), reads=["acc"], writes=["bs"])
                    P.op("dve", lambda e: e.tensor_scalar(bs[:, 1:2], bs[:, 0:1], -1.0, None, op0=ALU.mult), reads=["bs"], writes=["bs"])
                    P.op("dve", lambda e, L=L: e.tensor_tensor(out=acc[:, L - 128:L], in0=acc[:, L - 128:L], in1=triD[:], op=ALU.add), reads=["triD"], writes=["acc"])
                    lo, lon = los.next()
                    P.op("dve", lambda e, lo=lo: e.tensor_scalar(lo[:], bs[:, 1:2], -1.0, None, op0=ALU.add), reads=["bs"], writes=[lon])
                    P.op("dve", lambda e: e.scalar_tensor_tensor(out=bs[:, 2:3], in0=bs[:, 0:1], scalar=2.0, in1=bs[:, 1:2], op0=ALU.add, op1=ALU.subtract), reads=["bs"], writes=["bs"])
                    P.op("dve", lambda e: e.tensor_scalar(Wk[:], pw2[:], bs[:, 2:3], None, op0=ALU.mult), reads=["pw2", "bs"], writes=["Wk"])
                    yield
                    for k in range(NIT):
                        mid, midn = mids.next()
                        cnt, cntn = cnts.next()
                        tt, ttn = tts.next()
                        lo2, lo2n = los.next()
                        P.op("dve", lambda e, lo=lo, mid=mid, k=k: e.tensor_tensor(out=mid[:], in0=lo[:], in1=Wk[:, k:k + 1], op=ALU.add), reads=[lon, "Wk"], writes=[midn])
                        P.op("dve", lambda e, mid=mid, cnt=cnt, L=L: e.tensor_scalar(dbias[:, 0:L], acc[:, 0:L], mid[:, 0:1], 0.0, op0=ALU.is_ge, op1=ALU.add, accum_out=cnt[:]),
                             reads=["acc", midn], writes=[dbn + "lo", cntn])
                        P.op("dve", lambda e, cnt=cnt, tt=tt, k=k: e.scalar_tensor_tensor(out=tt[:], in0=cnt[:], scalar=255.5, in1=Wk[:, k:k + 1], op0=ALU.is_ge, op1=ALU.mult), reads=[cntn, "Wk"], writes=[ttn])
                        P.op("dve", lambda e, lo=lo, lo2=lo2, tt=tt: e.tensor_tensor(out=lo2[:], in0=lo[:], in1=tt[:], op=ALU.add), reads=[lon, ttn], writes=[lo2n])
                        lo, lon = lo2, lo2n
                        yield
                    P.op("dve", lambda e, lo=lo, L=L: e.tensor_scalar(dbias[:, 0:L], acc[:, 0:L], lo[:, 0:1], NEG, op0=ALU.is_lt, op1=ALU.mult), reads=["acc", lon], writes=[dbn, dbn + "lo", dbn + "hi"])

                tst[qt] = (b, sbT, sbTn, dbias, dbn)
                yield

            def att(G, j, inter=None, nsteps=1):
                qt = 4 * G + j
                nch = qt + 1
                gk = G if kind == "moba" else qt
                if gk not in grp:
                    load_att(gk)
                qpad, qpn = grp[gk]
                jo = j if kind == "moba" else 0
                b, sbT, sbTn, dbias, dbn = tst.pop(qt)
                osb, osbn = osbs.next()
                units = []
                for h in range(6):
                    for c0 in range(0, nch, 4):
                        units.append((h, list(range(c0, min(c0 + 4, nch)))))

                def emitS(h, cs):
                    sp_, spn = Sps.next()
                    for ci, c in enumerate(cs):
                        if kind == "moba":
                            blk = c // 2
                            if blk < b:
                                bias = (Eall[:, blk, :], sbT[:, h, :], ["Eall", sbTn])
                            elif c == qt:
                                bias = (tri[:], identb[:], ["tri", "identb"])
                            else:
                                bias = None
                        else:
                            bias = (dbias[:, c * 128:(c + 1) * 128], identb[:], [dbn, "identb"])
                        P.op("pe", lambda e, sp_=sp_, ci=ci, c=c, h=h, jo=jo, qpad=qpad, bias=bias: e.matmul(sp_[:, ci, :], kT[:, h // 2, c * 128:(c + 1) * 128], qpad[:, h, jo * 128:(jo + 1) * 128], start=True, stop=(bias is None)),
                             reads=["kT", qpn], writes=[spn])
                        if bias is not None:
                            P.op("pe", lambda e, sp_=sp_, ci=ci, bias=bias: e.matmul(sp_[:, ci, :], bias[0], bias[1], start=False, stop=True),
                                 reads=bias[2], writes=[spn])
                    return sp_, spn

                pend = emitS(*units[0])
                ops_ = opn = None
                sdone = 0
                for ui, (h, cs) in enumerate(units):
                    if inter is not None:
                        want = ((ui + 1) * nsteps + len(units) - 1) // len(units)
                        while sdone < want:
                            next(inter, None)
                            sdone += 1
                    sp_, spn = pend
                    if ui + 1 < len(units):
                        pend = emitS(*units[ui + 1])
                    if cs[0] == 0:
                        ops_, opn = Ops.next()
                    pt_, ptn = PTs.next()
                    ncs = len(cs)
                    P.op("act", lambda e, sp_=sp_, pt_=pt_, ncs=ncs: e.activation(pt_[:, 0:ncs, :], sp_[:, 0:ncs, :], AF.Exp, scale=0.125), reads=[], writes=[spn, ptn])
                    for ci, c in enumerate(cs):
                        P.op("pe", lambda e, ops_=ops_, pt_=pt_, ci=ci, c=c, h=h: e.matmul(ops_[:, 0:65], pt_[:, ci, :], V[:, c, h * 65:(h + 1) * 65], start=(c == 0), stop=(c == nch - 1)),
                             reads=[ptn, "V"], writes=[opn])
                    if cs[-1] == nch - 1:
                        if h % 2 == 0:
                            P.op("dve", lambda e, ops_=ops_, osb=osb, h=h: e.tensor_copy(osb[:, h, :], ops_[:, 0:65]), reads=[], writes=[opn, osbn])
                        else:
                            P.op("act", lambda e, ops_=ops_, osb=osb, h=h: e.copy(osb[:, h, :], ops_[:, 0:65]), reads=[], writes=[opn, osbn])
                P.op("dve", lambda e, osb=osb: e.reciprocal(rden[:], osb[:, :, 64]), reads=[osbn], writes=["rden"])
                P.op("dve", lambda e, osb=osb: e.tensor_tensor(out=ym[:], in0=osb[:, :, 0:64], in1=rden[:, :, None].to_broadcast([128, 6, 64]), op=ALU.mult), reads=[osbn, "rden"], writes=["ym"])
                P.op("act", lambda e: e.activation(junk2[:], ym[:].rearrange("p h d -> p (h d)"), AF.Square, accum_out=sst[:, 0:1]), reads=["ym"], writes=["junk2", "sst2"])
                P.op("act", lambda e: e.activation(sst[:, 1:2], sst[:, 0:1], AF.Sqrt, bias=1e-6, scale=1.0 / 384), reads=["sst2"], writes=["sst2b"])
                P.op("dve", lambda e: e.reciprocal(sst[:, 1:2], sst[:, 1:2]), reads=["sst2b"], writes=["sst2b"])
                mo, mon = mixo.next()
                P.op("dve", lambda e, mo=mo: e.scalar_tensor_tensor(out=mo[:], in0=ym[:].rearrange("p h d -> p (h d)"), scalar=sst[:, 1:2], in1=gB[:], op0=ALU.mult, op1=ALU.mult), reads=["ym", "sst2b", "gB"], writes=[mon])
                c0m = 0 if kind == "moba" else 640
                P.dma("sp", scr["mix"][qt * 128:(qt + 1) * 128, c0m:c0m + 384], mo[:], reads=[mon], writes=["s_mix"])

            tiles = [(G, j) for G in range(NG) for j in range(4)]
            for _ in pre(*tiles[0]):
                pass
            for ti, (G, j) in enumerate(tiles):
                if ti + 1 < len(tiles):
                    gen = pre(*tiles[ti + 1])
                    nst = (4 * tiles[ti + 1][0] + tiles[ti + 1][1] + 1 + 3) // 4
                    if kind == "moba":
                        for _ in gen:
                            pass
                        att(G, j)
                    else:
                        next(gen, None)
                        att(G, j, gen, nst + NIT + 1)
                        for _ in gen:
                            pass
                else:
                    att(G, j)
            P.phase_end()


        NBLK = 2 * S // 512 + 32
        x_src = x_in if l == 0 else xbuf[(l - 1) % 2]
        x_dst = xbuf[l % 2]
        P.phase_begin()
        SEL1 = P.sb("SEL1", [128, NT, 32], F32)
        SEL2 = P.sb("SEL2", [128, NT, 32], F32)
        GATES = P.sb("GATES", [128, NT, 2], F32)
        P.phase_begin()
        ub = P.sb("ub", [128, 2, S + 32], BF16)
        for hh in range(2):
            P.dma("sp", ub[:, hh, :], scr["u"][hh * 128:(hh + 1) * 128, :], reads=["s_u"], writes=["ub"])
        cw = P.sb("cw", [128, 2, 31], F32)
        P.dma("sp", cw[:], conv_wT[l], writes=["cw"])
        dgw = P.sb("dgw", [128, 2, 31, 128], BF16)
        for hh in range(2):
            for jj in range(31):
                P.op("dve" if jj % 2 else "pool", lambda e, hh=hh, jj=jj: e.tensor_scalar(dgw[:, hh, jj, :], identf[:], cw[:, hh, jj:jj + 1], None, op0=ALU.mult),
                     reads=["identf", "cw"], writes=["dgw"])
        woB = P.sb("woB", [128, KC, D], BF16)
        P.dma("pool", woB[:], w_out[l].rearrange("(kc p) n -> p kc n", p=128), writes=["woB"])
        cbB = P.sb("cbB", [128, 256], F32)
        lgB = P.sb("lgB", [128, 256], F32)
        lbB = P.sb("lbB", [128, 256], F32)
        g1B = P.sb("g1B", [128, D], F32)
        gm2B = P.sb("gm2B", [128, D], F32)
        sh2B = P.sb("sh2B", [128, D], F32)
        n2B = P.sb("n2B", [128, D], F32)
        wr = P.sb("wr", [128, KC, 36], F32)
        rbB = P.sb("rbB", [128, 36], F32)
        P.dma("sp", cbB[:], conv_bB[l], writes=["cbB"])
        P.dma("sp", lgB[:], ln_gB[l], writes=["lgB"])
        P.dma("sp", lbB[:], ln_bB[l], writes=["lbB"])
        P.dma("sp", g1B[:], d_modB[:, 2 * D:3 * D], reads=["s_modB"], writes=["g1B"])
        P.dma("sp", sh2B[:], d_modB[:, 3 * D:4 * D], reads=["s_modB"], writes=["sh2B"])
        P.dma("sp", gm2B[:], d_modB[:, 4 * D:5 * D], reads=["s_modB"], writes=["gm2B"])
        P.dma("sp", n2B[:], n2gB[l], writes=["n2B"])
        P.dma("sp", wr[:], rw[l].rearrange("(kc p) n -> p kc n", p=128), writes=["wr"])
        P.dma("sp", rbB[:], rbBin[l], writes=["rbB"])
        P.op("dve", lambda e: e.scalar_tensor_tensor(out=gm2B[:], in0=gm2B[:], scalar=1.0, in1=n2B[:], op0=ALU.add, op1=ALU.mult), reads=["n2B"], writes=["gm2B"])
        pc = Rot([(P.ps("pc", [128, 512], F32), "pc%d" % i) for i in range(2)])
        pT4 = Rot([(P.ps("pT4", [128, KC, 128], BF16), "pT4%d" % i) for i in range(1)])
        po = Rot([(P.ps("po", [128, 512], F32), "po%d" % i) for i in range(2)])
        pf = Rot([(P.ps("pf", [128, 4, 128], F32), "pf%d" % i) for i in range(2)])
        pl = P.ps("pl", [128, 512], F32)
        ycs = Rot([(P.sb("yc", [128, 256], F32), "yc%d" % i) for i in range(2)])
        st4 = P.sb("st4", [128, 16], F32)
        junk4 = P.sb("junk4", [128, D], F32)
        mixt = Rot([(P.sb("mixt", [128, D], BF16), "mixt%d" % i) for i in range(2)])
        mixT = P.sb("mixT", [128, KC, 128], BF16)
        xts = Rot([(P.sb("xt4", [128, D], F32), "xt4%d" % i) for i in range(2)])
        xms = Rot([(P.sb("xm", [128, D], F32), "xm%d" % i) for i in range(2)])
        h2fs = Rot([(P.sb("h2f", [128, D], F32), "h2f%d" % i) for i in range(2)])
        h2bs = Rot([(P.sb("h2b", [128, D], BF16), "h2b%d" % i) for i in range(2)])
        h2T = P.sb("h2T", [128, KC, 128], F32)
        lg = P.sb("lg", [128, 36], F32)
        rt = P.sb("rt", [128, 96], F32)
        def tile_gen(i):
            r0 = i * 128
            mt, mtn = mixt.next()
            xt_, xtn = xts.next()
            P.dma("sp", mt[:, 0:384], scr["mix"][r0:r0 + 128, 0:384], reads=["s_mix"], writes=[mtn])
            P.dma("sp", mt[:, 640:1024], scr["mix"][r0:r0 + 128, 640:1024], reads=["s_mix"], writes=[mtn])
            P.dma("sp", xt_[:], x_src[r0:r0 + 128, :], reads=["xsrc"], writes=[xtn])
            pct, pcn = pc.next()
            for hh in range(2):
                for jj in range(31):
                    P.op("pe", lambda e, pct=pct, hh=hh, jj=jj, r0=r0: e.matmul(pct[:, hh * 128:(hh + 1) * 128], ub[:, hh, r0 + 2 + jj:r0 + 2 + jj + 128], dgw[:, hh, jj, :], start=(jj == 0), stop=(jj == 30)),
                         reads=["ub", "dgw"], writes=[pcn])
            yc, ycn = ycs.next()
            P.op("dve", lambda e, pct=pct, yc=yc: e.tensor_tensor(out=yc[:], in0=pct[:, 0:256], in1=cbB[:], op=ALU.add), reads=["cbB"], writes=[pcn, ycn])
            yield
            P.op("dve", lambda e: e.tensor_reduce(out=st4[:, 0:1], in_=yc[:], axis=AX.X, op=ALU.add), reads=[ycn], writes=["st4a"])
            P.op("dve", lambda e: e.tensor_scalar(st4[:, 1:2], st4[:, 0:1], -1.0 / 256, None, op0=ALU.mult), reads=["st4a"], writes=["st4b"])
            P.op("dve", lambda e: e.tensor_scalar(yc[:], yc[:], st4[:, 1:2], None, op0=ALU.add), reads=["st4b"], writes=[ycn])
            P.op("act", lambda e: e.activation(junk4[:, 0:256], yc[:], AF.Square, accum_out=st4[:, 2:3]), reads=[ycn], writes=["junk4", "st4c"])
            P.op("act", lambda e: e.activation(st4[:, 3:4], st4[:, 2:3], AF.Sqrt, bias=1e-6, scale=1.0 / 256), reads=["st4c"], writes=["st4d"])
            P.op("dve", lambda e: e.reciprocal(st4[:, 3:4], st4[:, 3:4]), reads=["st4d"], writes=["st4d"])
            P.op("dve", lambda e: e.scalar_tensor_tensor(out=yc[:], in0=yc[:], scalar=st4[:, 3:4], in1=lgB[:], op0=ALU.mult, op1=ALU.mult), reads=["st4d", "lgB"], writes=[ycn])
            P.op("dve", lambda e: e.tensor_tensor(out=yc[:], in0=yc[:], in1=lbB[:], op=ALU.add), reads=["lbB"], writes=[ycn])
            P.op("act", lambda e, mt=mt: e.activation(mt[:, 384:640], yc[:], AF.Silu), reads=[ycn], writes=[mtn])
            ptt, ptn = pT4.next()
            for kc in range(KC):
                P.op("pe", lambda e, kc=kc, mt=mt, ptt=ptt: e.transpose(ptt[:, kc, :], mt[:, kc * 128:(kc + 1) * 128], identb[:]), reads=[mtn, "identb"], writes=[ptn])
            P.op("act", lambda e, ptt=ptt: e.copy(mixT[:], ptt[:]), reads=[], writes=[ptn, "mixT"])
            xm, xmn = xms.next()
            for hf in range(2):
                pot, pon = po.next()
                for kc in range(KC):
                    P.op("pe", lambda e, kc=kc, pot=pot, hf=hf: e.matmul(pot[:], mixT[:, kc, :], woB[:, kc, hf * 512:(hf + 1) * 512], start=(kc == 0), stop=(kc == KC - 1)),
                         reads=["mixT", "woB"], writes=[pon])
                P.op("dve", lambda e, pot=pot, hf=hf, xm=xm: e.tensor_tensor(out=xm[:, hf * 512:(hf + 1) * 512], in0=pot[:], in1=g1B[:, hf * 512:(hf + 1) * 512], op=ALU.mult),
                     reads=["g1B"], writes=[pon, xmn])
            P.op("pool", lambda e, xm=xm, xt_=xt_: e.tensor_tensor(out=xm[:], in0=xm[:], in1=xt_[:], op=ALU.add), reads=[xtn], writes=[xmn])
            P.dma("sp", scr["xmid"][r0:r0 + 128, :], xm[:], reads=[xmn], writes=["s_xmid"])
            P.op("act", lambda e, xm=xm: e.activation(junk4[:], xm[:], AF.Square, accum_out=st4[:, 4:5]), reads=[xmn], writes=["junk4", "st4e"])
            P.op("act", lambda e: e.activation(st4[:, 5:6], st4[:, 4:5], AF.Sqrt, bias=1e-6, scale=1.0 / D), reads=["st4e"], writes=["st4f"])
            P.op("dve", lambda e: e.reciprocal(st4[:, 5:6], st4[:, 5:6]), reads=["st4f"], writes=["st4f"])
            h2f, h2fn = h2fs.next()
            h2b, h2bn = h2bs.next()
            P.op("dve", lambda e, h2f=h2f, xm=xm: e.scalar_tensor_tensor(out=h2f[:], in0=xm[:], scalar=st4[:, 5:6], in1=gm2B[:], op0=ALU.mult, op1=ALU.mult), reads=[xmn, "st4f", "gm2B"], writes=[h2fn])
            P.op("pool", lambda e, h2f=h2f: e.tensor_tensor(out=h2f[:], in0=h2f[:], in1=sh2B[:], op=ALU.add), reads=["sh2B"], writes=[h2fn])
            P.op("act", lambda e, h2f=h2f, h2b=h2b: e.copy(h2b[:], h2f[:]), reads=[h2fn], writes=[h2bn])
            P.dma("sp", scr["h2"][r0:r0 + 128, :], h2b[:], reads=[h2bn], writes=["s_h2"])
            yield
            for q4 in range(2):
                pft, pfn = pf.next()
                for k4 in range(4):
                    kc = q4 * 4 + k4
                    P.op("pe", lambda e, pft=pft, k4=k4, kc=kc, h2f=h2f: e.transpose(pft[:, k4, :], h2f[:, kc * 128:(kc + 1) * 128], identf[:]), reads=[h2fn, "identf"], writes=[pfn])
                if q4 == 0:
                    P.op("act", lambda e, pft=pft, q4=q4: e.copy(h2T[:, q4 * 4:(q4 + 1) * 4, :], pft[:]), reads=[], writes=[pfn, "h2T"])
                else:
                    P.op("dve", lambda e, pft=pft, q4=q4: e.tensor_copy(h2T[:, q4 * 4:(q4 + 1) * 4, :], pft[:]), reads=[], writes=[pfn, "h2T"])
            for kc in range(KC):
                P.op("pe", lambda e, kc=kc: e.matmul(pl[:, 0:36], h2T[:, kc, :], wr[:, kc, :], start=(kc == 0), stop=(kc == KC - 1)), reads=["h2T", "wr"], writes=["pl"])
            P.op("dve", lambda e: e.tensor_tensor(out=lg[:], in0=pl[:, 0:36], in1=rbB[:], op=ALU.add), reads=["rbB"], writes=["pl", "lg"])
            V_ = lambda a, b: rt[:, a:b]
            P.op("dve", lambda e: e.tensor_reduce(out=V_(0, 1), in_=lg[:, 0:4], axis=AX.X, op=ALU.max), reads=["lg"], writes=["rt0"])
            P.op("dve", lambda e: e.tensor_scalar(V_(1, 2), V_(0, 1), -1.0, None, op0=ALU.mult), reads=["rt0"], writes=["rt1"])
            P.op("act", lambda e: e.activation(V_(40, 44), lg[:, 0:4], AF.Exp, bias=V_(1, 2), accum_out=V_(2, 3)), reads=["lg", "rt1"], writes=["rt40", "rt2"])
            P.op("dve", lambda e: e.reciprocal(V_(3, 4), V_(2, 3)), reads=["rt2"], writes=["rt3"])
            P.op("dve", lambda e: e.tensor_scalar(V_(4, 8), lg[:, 0:4], V_(0, 1), None, op0=ALU.is_equal), reads=["lg", "rt0"], writes=["rt4"])
            P.op("dve", lambda e: e.tensor_tensor(out=V_(48, 80).rearrange("p (g x) -> p g x", x=8), in0=lg[:, 4:36].rearrange("p (g x) -> p g x", x=8), in1=V_(4, 8)[:, :, None].to_broadcast([128, 4, 8]), op=ALU.mult),
                 reads=["lg", "rt4"], writes=["rt48"])
            P.op("dve", lambda e: e.tensor_reduce(out=V_(8, 16), in_=V_(48, 80).rearrange("p (g x) -> p x g", x=8), axis=AX.X, op=ALU.add), reads=["rt48"], writes=["rt8"])
            P.op("dve", lambda e: e.tensor_reduce(out=V_(16, 17), in_=V_(8, 16), axis=AX.X, op=ALU.max), reads=["rt8"], writes=["rt16"])
            P.op("dve", lambda e: e.tensor_scalar(V_(17, 18), V_(16, 17), -1.0, None, op0=ALU.mult), reads=["rt16"], writes=["rt17"])
            P.op("dve", lambda e: e.tensor_scalar(V_(18, 26), V_(8, 16), V_(16, 17), None, op0=ALU.is_equal), reads=["rt8", "rt16"], writes=["rt18"])
            P.op("dve", lambda e: e.scalar_tensor_tensor(out=V_(26, 34), in0=V_(18, 26), scalar=-1e30, in1=V_(8, 16), op0=ALU.mult, op1=ALU.add), reads=["rt18", "rt8"], writes=["rt26"])
            P.op("dve", lambda e: e.tensor_reduce(out=V_(34, 35), in_=V_(26, 34), axis=AX.X, op=ALU.max), reads=["rt26"], writes=["rt34"])
            P.op("dve", lambda e: e.tensor_scalar(V_(80, 88), V_(26, 34), V_(34, 35), None, op0=ALU.is_equal), reads=["rt26", "rt34"], writes=["rt80"])
            P.op("act", lambda e: e.activation(V_(35, 36), V_(34, 35), AF.Exp, bias=V_(17, 18)), reads=["rt34", "rt17"], writes=["rt35"])
            P.op("dve", lambda e: e.tensor_scalar(V_(36, 37), V_(35, 36), 1.0, None, op0=ALU.add), reads=["rt35"], writes=["rt36"])
            P.op("dve", lambda e: e.reciprocal(V_(36, 37), V_(36, 37)), reads=["rt36"], writes=["rt36"])
            P.op("dve", lambda e, i=i: e.tensor_tensor(out=GATES[:, i, 0:1], in0=V_(36, 37), in1=V_(3, 4), op=ALU.mult), reads=["rt36", "rt3"], writes=["GATES"])
            P.op("dve", lambda e, i=i: e.tensor_tensor(out=GATES[:, i, 1:2], in0=V_(3, 4), in1=GATES[:, i, 0:1], op=ALU.subtract), reads=["rt3"], writes=["GATES"])
            P.op("dve", lambda e, i=i: e.tensor_tensor(out=SEL1[:, i, :].rearrange("p (g x) -> p g x", x=8), in0=V_(4, 8)[:, :, None].to_broadcast([128, 4, 8]), in1=V_(18, 26)[:, None, :].to_broadcast([128, 4, 8]), op=ALU.mult),
                 reads=["rt4", "rt18"], writes=["SEL1"])
            P.op("dve", lambda e, i=i: e.tensor_tensor(out=SEL2[:, i, :].rearrange("p (g x) -> p g x", x=8), in0=V_(4, 8)[:, :, None].to_broadcast([128, 4, 8]), in1=V_(80, 88)[:, None, :].to_broadcast([128, 4, 8]), op=ALU.mult),
                 reads=["rt4", "rt80"], writes=["SEL2"])
        gens = [tile_gen(i) for i in range(NT)]
        next(gens[0])
        for i in range(NT):
            if i + 1 < NT:
                next(gens[i + 1])
            next(gens[i])
            next(gens[i], None)
        if "s_sel" in dbg:
            P.dma("sp", d_sel[0], SEL1[:], reads=["SEL1"])
            P.dma("sp", d_sel[1], SEL2[:], reads=["SEL2"])
            P.dma("sp", d_gates, GATES[:], reads=["GATES"])
        P.phase_end()

        P.phase_begin()
        W1I = P.sb("W1I", [128, NBLK, 8], I32)
        W2I = P.sb("W2I", [128, NBLK, 4], I32)
        DESTI = P.sb("DESTI", [128, NT, 2], I32)
        P.phase_begin()
        SELS = P.sb("SELS", [128, NT, 32], F32)
        CUM = P.sb("CUM", [128, NT + 1, 32], F32)
        P.op("dve", lambda e: e.tensor_tensor(out=SELS[:], in0=SEL1[:], in1=SEL2[:], op=ALU.add), reads=["SEL1", "SEL2"], writes=["SELS"])
        P.op("pool", lambda e: e.memset(CUM[:, 0, :], 0.0), writes=["CUM"])
        for i in range(NT):
            P.op("dve", lambda e, i=i: e.tensor_tensor(out=CUM[:, i + 1, :], in0=CUM[:, i, :], in1=SELS[:, i, :], op=ALU.add), reads=["SELS"], writes=["CUM"])
        onesf = P.sb("onesf", [128, 128], F32)
        UT = P.sb("UT", [128, 128], F32)
        P.op("pool", lambda e: e.memset(onesf[:], 1.0), writes=["onesf"])
        P.op("dve", lambda e: e.tensor_scalar(UT[:], io[:], pid[:, 0:1], None, op0=ALU.is_gt), reads=["io", "pid"], writes=["UT"])
        pq = Rot([(P.ps("pq", [128, 512], F32), "pq%d" % i) for i in range(2)])
        pqt, pqn = pq.next()
        P.op("pe", lambda e, pqt=pqt: e.matmul(pqt[:, 0:32], onesf[:], CUM[:, NT, :], start=True, stop=True), reads=["onesf", "CUM"], writes=[pqn])
        ms = P.sb("ms", [128, 512], F32)
        cntE = ms[:, 0:32]
        nblk = ms[:, 32:64]
        pendA = ms[:, 64:96]
        pendB = ms[:, 96:128]
        pst = ms[:, 128:160]
        thr = ms[:, 160:192]
        j32 = ms[:, 192:224]
        NM = S // 512 + 1
        P.op("dve", lambda e, pqt=pqt: e.tensor_copy(cntE, pqt[:, 0:32]), reads=[], writes=[pqn, "cntE"])
        P.op("dve", lambda e: e.tensor_scalar(thr[:, 0:NM], io[:, 0:NM], 512.0, None, op0=ALU.mult), reads=["io"], writes=["thr"])
        P.op("pool", lambda e: e.memset(nblk, 0.0), writes=["nblk"])
        for ex in range(32):
            P.op("dve", lambda e, ex=ex: e.tensor_scalar(j32[:, 0:NM], thr[:, 0:NM], cntE[:, ex:ex + 1], 0.0, op0=ALU.is_lt, op1=ALU.add, accum_out=nblk[:, ex:ex + 1]),
                 reads=["thr", "cntE"], writes=["j32", "nblk"])
        src, srcn, dst, dstn = nblk, "nblk", pendA, "pendA"
        for d_ in (1, 2, 4, 8, 16):
            P.op("dve", lambda e, src=src, dst=dst, d_=d_: e.tensor_copy(dst[:, 0:d_], src[:, 0:d_]), reads=[srcn], writes=[dstn])
            P.op("dve", lambda e, src=src, dst=dst, d_=d_: e.tensor_tensor(out=dst[:, d_:32], in0=src[:, d_:32], in1=src[:, 0:32 - d_], op=ALU.add), reads=[srcn], writes=[dstn])
            if dstn == "pendA":
                src, srcn, dst, dstn = pendA, "pendA", pendB, "pendB"
            else:
                src, srcn, dst, dstn = pendB, "pendB", pendA, "pendA"
        pend, pendn = src, srcn
        P.op("dve", lambda e, pend=pend: e.tensor_tensor(out=pst, in0=pend, in1=nblk, op=ALU.subtract), reads=[pendn, "nblk"], writes=["pst"])
        P.op("dve", lambda e: e.tensor_scalar(pst, pst, 512.0, None, op0=ALU.mult), reads=[], writes=["pst"])
        BE = P.sb("BE", [128, NBLK], F32)
        P.op("pool", lambda e: e.memset(BE[:], 0.0), writes=["BE"])
        for b in range(NBLK):
            P.op("dve", lambda e, b=b, pend=pend: e.tensor_scalar(j32, pend, float(b), 0.0, op0=ALU.is_le, op1=ALU.add, accum_out=BE[:, b:b + 1]), reads=[pendn], writes=["j32", "BE"])
        P.op("dve", lambda e: e.tensor_scalar(BE[:], BE[:], 31.0, None, op0=ALU.min), reads=[], writes=["BE"])
        iotaK = P.sb("iotaK", [128, 8], F32)
        P.op("dve", lambda e: e.tensor_scalar(iotaK[:], io[:, 0:8], 128.0, pid[:, 0:1], op0=ALU.mult, op1=ALU.add), reads=["io", "pid"], writes=["iotaK"])
        WF = P.sb("WF", [128, NBLK, 8], F32)
        BEs = P.sb("BEs", [128, NBLK], F32)
        SAME = P.sb("SAME", [128, NBLK], F32)
        P.op("pool", lambda e: e.memset(SAME[:], 0.0), writes=["SAME"])
        P.op("dve", lambda e: e.tensor_tensor(out=SAME[:, 1:NBLK], in0=BE[:, 1:NBLK], in1=BE[:, 0:NBLK - 1], op=ALU.is_equal), reads=["BE"], writes=["SAME"])
        P.op("dve", lambda e: e.tensor_scalar(BEs[:], BE[:], 1024.0, float(l * 32 * 1024), op0=ALU.mult, op1=ALU.add), reads=["BE"], writes=["BEs"])
        P.op("dve", lambda e: e.scalar_tensor_tensor(out=BEs[:], in0=SAME[:], scalar=200000.0, in1=BEs[:], op0=ALU.mult, op1=ALU.add), reads=["SAME"], writes=["BEs"])
        P.op("dve", lambda e: e.tensor_tensor(out=WF[:], in0=BEs[:, :, None].to_broadcast([128, NBLK, 8]), in1=iotaK[:, None, :].to_broadcast([128, NBLK, 8]), op=ALU.add), reads=["BEs", "iotaK"], writes=["WF"])
        P.op("dve", lambda e: e.tensor_copy(W1I[:], WF[:]), reads=["WF"], writes=["W1I"])
        P.op("dve", lambda e: e.tensor_scalar(BEs[:], BE[:], 512.0, float(l * 32 * 512), op0=ALU.mult, op1=ALU.add), reads=["BE", "WF"], writes=["BEs"])
        P.op("dve", lambda e: e.scalar_tensor_tensor(out=BEs[:], in0=SAME[:], scalar=200000.0, in1=BEs[:], op0=ALU.mult, op1=ALU.add), reads=["SAME"], writes=["BEs"])
        P.op("dve", lambda e: e.tensor_tensor(out=WF[:, :, 0:4], in0=BEs[:, :, None].to_broadcast([128, NBLK, 4]), in1=iotaK[:, None, 0:4].to_broadcast([128, NBLK, 4]), op=ALU.add), reads=["BEs", "iotaK", "W1I"], writes=["WF"])
        P.op("dve", lambda e: e.tensor_copy(W2I[:], WF[:, :, 0:4]), reads=["WF"], writes=["W2I"])
        DEST = P.sb("DEST", [128, NT, 2], F32)
        P.op("pool", lambda e: e.memset(DEST[:], 0.0), writes=["DEST"])
        tq = Rot([(P.sb("tq", [128, 32], F32), "tq%d" % i) for i in range(2)])
        for i in range(NT):
            pqt, pqn = pq.next()
            tqt, tqn = tq.next()
            P.op("pe", lambda e, pqt=pqt, i=i: e.matmul(pqt[:, 0:32], UT[:], SELS[:, i, :], start=True, stop=False), reads=["UT", "SELS"], writes=[pqn])
            P.op("pe", lambda e, pqt=pqt, i=i: e.matmul(pqt[:, 0:32], onesf[:], CUM[:, i, :], start=False, stop=True), reads=["onesf", "CUM"], writes=[pqn])
            P.op("dve", lambda e, pqt=pqt, tqt=tqt: e.tensor_tensor(out=tqt[:], in0=pqt[:, 0:32], in1=pst, op=ALU.add), reads=["pst"], writes=[pqn, tqn])
            for sl, SEL, seln in ((0, SEL1, "SEL1"), (1, SEL2, "SEL2")):
                P.op("dve", lambda e, tqt=tqt, i=i, sl=sl, SEL=SEL: e.scalar_tensor_tensor(out=j32, in0=tqt[:], scalar=1.0, in1=SEL[:, i, :], op0=ALU.mult, op1=ALU.mult, accum_out=DEST[:, i, sl:sl + 1]),
                     reads=[tqn, seln], writes=["j32", "DEST"])
        P.op("dve", lambda e: e.tensor_copy(DESTI[:], DEST[:]), reads=["DEST"], writes=["DESTI"])
        if "s_dest" in dbg:
            P.dma("sp", d_dest, DESTI[:], reads=["DESTI"])
            P.dma("sp", d_be, BE[:], reads=["BE"])
        zb = P.sb("zb", [128, 4, D], BF16)
        P.op("pool", lambda e: e.memset(zb[:], 0.0), writes=["zb"])
        for b in range(NBLK):
            P.dma("sp", scr["buf"][b * 512:(b + 1) * 512, :].rearrange("(s p) d -> p s d", p=128), zb[:], reads=["zb"], writes=["s_buf"])
        h2l = Rot([(P.sb("h2l", [128, D], BF16), "h2l%d" % i) for i in range(3)])
        for i in range(NT):
            ht, htn = h2l.next()
            P.dma("sp", ht[:], scr["h2"][i * 128:(i + 1) * 128, :], reads=["s_h2"], writes=[htn])
            for sl in range(2):
                P.op("pool", lambda e, ht=ht, i=i, sl=sl: e.indirect_dma_start(out=scr["buf"], out_offset=bass.IndirectOffsetOnAxis(ap=DESTI[:, i, sl:sl + 1], axis=0), in_=ht[:], in_offset=None),
                     reads=[htn, "DESTI"], writes=["s_buf"], dma=True)
        P.phase_end()
        P.phase_begin()
        w1v = w1
        w3v = w3
        w2v = w2
        w1f = Rot([(P.sb("w1f", [128, KC, 512], F32), "w1f%d" % i) for i in range(1)])
        w3f = Rot([(P.sb("w3f", [128, KC, 512], F32), "w3f%d" % i) for i in range(1)])
        w2f = Rot([(P.sb("w2f", [128, 4, D], F32), "w2f%d" % i) for i in range(1)])
        w1b = Rot([(P.sb("w1b", [128, KC, 512], BF16), "w1b%d" % i) for i in range(2)])
        w3b = Rot([(P.sb("w3b", [128, KC, 512], BF16), "w3b%d" % i) for i in range(2)])
        w2b = Rot([(P.sb("w2b", [128, 4, D], BF16), "w2b%d" % i) for i in range(2)])
        hbs = Rot([(P.sb("hb", [128, 4, D], BF16), "hb%d" % i) for i in range(2)])
        hTs = Rot([(P.sb("hT", [128, KC, 512], BF16), "hT%d" % i) for i in range(2)])
        sgs = Rot([(P.sb("sg", [128, 512], F32), "sg%d" % i) for i in range(2)])
        aTs = Rot([(P.sb("aT", [128, 4, 512], BF16), "aT%d" % i) for i in range(2)])
        ybs = Rot([(P.sb("yb", [128, 512], F32), "yb%d" % i) for i in range(4)])
        pT5 = Rot([(P.ps("pT5", [128, KC, 128], BF16), "pT5%d" % i) for i in range(2)])
        ph1 = Rot([(P.ps("ph1", [128, 512], F32), "ph1%d" % i) for i in range(1)])
        ph3 = Rot([(P.ps("ph3", [128, 512], F32), "ph3%d" % i) for i in range(1)])
        py = Rot([(P.ps("py", [128, 512], F32), "py%d" % i) for i in range(2)])
        for b in range(NBLK):
            a1, a1n = w1f.next()
            a3, a3n = w3f.next()
            a2, a2n = w2f.next()
            for kc in range(KC):
                P.op("pool", lambda e, a1=a1, b=b, kc=kc: e.indirect_dma_start(out=a1[:, kc, :], out_offset=None, in_=w1v, in_offset=bass.IndirectOffsetOnAxis(ap=W1I[:, b, kc:kc + 1], axis=0), bounds_check=_breg(e, NL * 32 * 1024 - 1), oob_is_err=False),
                     reads=["W1I"], writes=[a1n], dma=True)
                P.op("pool", lambda e, a3=a3, b=b, kc=kc: e.indirect_dma_start(out=a3[:, kc, :], out_offset=None, in_=w3v, in_offset=bass.IndirectOffsetOnAxis(ap=W1I[:, b, kc:kc + 1], axis=0), bounds_check=_breg(e, NL * 32 * 1024 - 1), oob_is_err=False),
                     reads=["W1I"], writes=[a3n], dma=True)
            for dc in range(4):
                P.op("pool", lambda e, a2=a2, b=b, dc=dc: e.indirect_dma_start(out=a2[:, dc, :], out_offset=None, in_=w2v, in_offset=bass.IndirectOffsetOnAxis(ap=W2I[:, b, dc:dc + 1], axis=0), bounds_check=_breg(e, NL * 32 * 512 - 1), oob_is_err=False),
                     reads=["W2I"], writes=[a2n], dma=True)
            hb, hbn = hbs.next()
            P.dma("sp", hb[:], scr["buf"][b * 512:(b + 1) * 512, :].rearrange("(s p) d -> p s d", p=128), reads=["s_buf"], writes=[hbn])
            b1, b1n = w1b.next()
            b3, b3n = w3b.next()
            b2, b2n = w2b.next()
            P.op("act", lambda e, a1=a1, b1=b1: e.copy(b1[:], a1[:]), reads=[a1n], writes=[b1n])
            P.op("dve", lambda e, a3=a3, b3=b3: e.tensor_copy(b3[:], a3[:]), reads=[a3n], writes=[b3n])
            P.op("dve", lambda e, a2=a2, b2=b2: e.tensor_copy(b2[:], a2[:]), reads=[a2n], writes=[b2n])
            hT, hTn = hTs.next()
            for sub in range(4):
                ptt, ptn = pT5.next()
                for kc in range(KC):
                    P.op("pe", lambda e, ptt=ptt, hb=hb, sub=sub, kc=kc: e.transpose(ptt[:, kc, :], hb[:, sub, kc * 128:(kc + 1) * 128], identb[:]), reads=[hbn, "identb"], writes=[ptn])
                if sub % 2 == 0:
                    P.op("act", lambda e, ptt=ptt, hT=hT, sub=sub: e.copy(hT[:, :, sub * 128:(sub + 1) * 128], ptt[:]), reads=[], writes=[ptn, hTn])
                else:
                    P.op("dve", lambda e, ptt=ptt, hT=hT, sub=sub: e.tensor_copy(hT[:, :, sub * 128:(sub + 1) * 128], ptt[:]), reads=[], writes=[ptn, hTn])
            aT, aTn = aTs.next()
            for dc in range(4):
                p1, p1n = ph1.next()
                p3, p3n = ph3.next()
                sg, sgn = sgs.next()
                for kc in range(KC):
                    P.op("pe", lambda e, p1=p1, b1=b1, hT=hT, kc=kc, dc=dc: e.matmul(p1[:], b1[:, kc, dc * 128:(dc + 1) * 128], hT[:, kc, :], start=(kc == 0), stop=(kc == KC - 1)), reads=[b1n, hTn], writes=[p1n])
                for kc in range(KC):
                    P.op("pe", lambda e, p3=p3, b3=b3, hT=hT, kc=kc, dc=dc: e.matmul(p3[:], b3[:, kc, dc * 128:(dc + 1) * 128], hT[:, kc, :], start=(kc == 0), stop=(kc == KC - 1)), reads=[b3n, hTn], writes=[p3n])
                P.op("act", lambda e, p1=p1, sg=sg: e.activation(sg[:], p1[:], AF.Silu), reads=[], writes=[p1n, sgn])
                P.op("dve", lambda e, p3=p3, sg=sg, aT=aT, dc=dc: e.tensor_tensor(out=aT[:, dc, :], in0=p3[:], in1=sg[:], op=ALU.mult), reads=[sgn], writes=[p3n, aTn])
            for sub in range(4):
                for hf in range(2):
                    pyt, pyn = py.next()
                    yb, ybn = ybs.next()
                    for dc in range(4):
                        P.op("pe", lambda e, pyt=pyt, aT=aT, b2=b2, dc=dc, sub=sub, hf=hf: e.matmul(pyt[:], aT[:, dc, sub * 128:(sub + 1) * 128], b2[:, dc, hf * 512:(hf + 1) * 512], start=(dc == 0), stop=(dc == 3)), reads=[aTn, b2n], writes=[pyn])
                    if hf == 0:
                        P.op("act", lambda e, pyt=pyt, yb=yb: e.copy(yb[:], pyt[:]), reads=[], writes=[pyn, ybn])
                    else:
                        P.op("dve", lambda e, pyt=pyt, yb=yb: e.tensor_copy(yb[:], pyt[:]), reads=[], writes=[pyn, ybn])
                    P.dma("sp", scr["ybuf"][b * 512 + sub * 128:b * 512 + (sub + 1) * 128, hf * 512:(hf + 1) * 512], yb[:], reads=[ybn], writes=["s_ybuf"])
        P.phase_end()
        P.phase_begin()
        g2B = P.sb("g2B", [128, D], F32)
        fgB = P.sb("fgB", [128, D], F32)
        P.dma("sp", g2B[:], d_modB[:, 5 * D:6 * D], reads=["s_modB"], writes=["g2B"])
        P.dma("sp", fgB[:], final_gB, writes=["fgB"])
        y1s = Rot([(P.sb("y1", [128, D], F32), "y1%d" % i) for i in range(2)])
        y2s = Rot([(P.sb("y2", [128, D], F32), "y2%d" % i) for i in range(2)])
        xls = Rot([(P.sb("xl", [128, D], F32), "xl%d" % i) for i in range(2)])
        xos = Rot([(P.sb("xo", [128, D], F32), "xo%d" % i) for i in range(2)])
        junk5 = P.sb("junk5", [128, D], F32)
        st5 = P.sb("st5", [128, 4], F32)
        last = (l == NL - 1)
        for i in range(NT):
            r0 = i * 128
            y1, y1n = y1s.next()
            y2, y2n = y2s.next()
            xl, xln = xls.next()
            xo, xon = xos.next()
            P.op("pool", lambda e, y1=y1, i=i: e.indirect_dma_start(out=y1[:], out_offset=None, in_=scr["ybuf"], in_offset=bass.IndirectOffsetOnAxis(ap=DESTI[:, i, 0:1], axis=0)),
                 reads=["DESTI", "s_ybuf"], writes=[y1n], dma=True)
            P.op("pool", lambda e, y2=y2, i=i: e.indirect_dma_start(out=y2[:], out_offset=None, in_=scr["ybuf"], in_offset=bass.IndirectOffsetOnAxis(ap=DESTI[:, i, 1:2], axis=0)),
                 reads=["DESTI", "s_ybuf"], writes=[y2n], dma=True)
            P.dma("sp", xl[:], scr["xmid"][r0:r0 + 128, :], reads=["s_xmid"], writes=[xln])
            P.op("dve", lambda e, y1=y1, i=i: e.tensor_scalar(y1[:], y1[:], GATES[:, i, 0:1], None, op0=ALU.mult), reads=["GATES"], writes=[y1n])
            P.op("dve", lambda e, y1=y1, y2=y2, i=i: e.scalar_tensor_tensor(out=y1[:], in0=y2[:], scalar=GATES[:, i, 1:2], in1=y1[:], op0=ALU.mult, op1=ALU.add), reads=["GATES", y2n], writes=[y1n])
            P.op("pool", lambda e, y1=y1: e.tensor_tensor(out=y1[:], in0=y1[:], in1=g2B[:], op=ALU.mult), reads=["g2B"], writes=[y1n])
            P.op("pool", lambda e, y1=y1, xl=xl, xo=xo: e.tensor_tensor(out=xo[:], in0=y1[:], in1=xl[:], op=ALU.add), reads=[y1n, xln], writes=[xon])
            if not last:
                P.dma("sp", x_dst[r0:r0 + 128, :], xo[:], reads=[xon], writes=["xdst"])
            else:
                P.op("act", lambda e, xo=xo: e.activation(junk5[:], xo[:], AF.Square, accum_out=st5[:, 0:1]), reads=[xon], writes=["junk5", "st5a"])
                P.op("act", lambda e: e.activation(st5[:, 1:2], st5[:, 0:1], AF.Sqrt, bias=1e-6, scale=1.0 / D), reads=["st5a"], writes=["st5b"])
                P.op("dve", lambda e: e.reciprocal(st5[:, 1:2], st5[:, 1:2]), reads=["st5b"], writes=["st5b"])
                P.op("dve", lambda e, xo=xo: e.scalar_tensor_tensor(out=xo[:], in0=xo[:], scalar=st5[:, 1:2], in1=fgB[:], op0=ALU.mult, op1=ALU.mult), reads=["st5b", "fgB"], writes=[xon])
                P.dma("sp", y_out[r0:r0 + 128, :], xo[:], reads=[xon], writes=["y"])
        P.phase_end()
        P.phase_end()
        P.phase_end()

    P.emit()
    return nc, P


def prep_inputs(inp, b, NL):
    f = lambda a: np.ascontiguousarray(np.asarray(a, dtype=np.float32))
    rep = lambda a: f(np.broadcast_to(np.asarray(a)[:, None, :], (a.shape[0], 128, a.shape[1])))
    d = {}
    d["x"] = f(inp["x"][b])
    d["c_col"] = f(np.asarray(inp["c"][b]).reshape(8, 128).T)
    d["ada_w"] = f(inp["ada_w"][:NL])
    d["ada_bB"] = rep(inp["ada_b"][:NL])
    d["n1g_col"] = f(np.asarray(inp["norm1_g"][:NL]).reshape(NL, 8, 128).transpose(0, 2, 1))
    d["w_in"] = f(inp["w_in"][:NL])
    d["conv_wT"] = f(np.asarray(inp["conv_w"][:NL]).transpose(0, 2, 1).reshape(NL, 2, 128, 31).transpose(0, 2, 1, 3))
    d["conv_bB"] = rep(inp["conv_b"][:NL])
    d["ln_gB"] = rep(inp["conv_ln_g"][:NL])
    d["ln_bB"] = rep(inp["conv_ln_b"][:NL])
    d["w_out"] = f(inp["w_out"][:NL])
    d["n2gB"] = rep(inp["norm2_g"][:NL])
    d["rw"] = f(np.concatenate([inp["router_group_w"][:NL], inp["router_expert_w"][:NL]], axis=-1))
    d["rbB"] = rep(np.concatenate([inp["router_group_b"][:NL], inp["router_expert_b"][:NL]], axis=-1))
    d["w1"] = f(np.asarray(inp["expert_w1"][:NL]).reshape(NL * 32 * 1024, 512))
    d["w3"] = f(np.asarray(inp["expert_w3"][:NL]).reshape(NL * 32 * 1024, 512))
    d["w2"] = f(np.asarray(inp["expert_w2"][:NL]).reshape(NL * 32 * 512, 1024))
    d["final_gB"] = f(np.broadcast_to(np.asarray(inp["final_g"])[None, :], (128, 1024)))
    d["mgB"] = rep(inp["moba_norm_g"][:NL])
    d["dgB"] = rep(inp["dsa_norm_g"][:NL])
    return d


_CACHE = {}


def kernel(**inputs):
    S, NL, NB_ = 8192, 2, 4
    if "nc" not in _CACHE:
        _CACHE["nc"] = build(S, NL)[0]
    nc = _CACHE["nc"]
    shared = prep_inputs(inputs, 0, NL)
    in_maps = []
    for b in range(NB_):
        d = dict(shared)
        d["x"] = np.ascontiguousarray(np.asarray(inputs["x"][b], dtype=np.float32))
        d["c_col"] = np.ascontiguousarray(np.asarray(inputs["c"][b], dtype=np.float32).reshape(8, 128).T)
        in_maps.append(d)
    res = run_bass_kernel_spmd(nc, in_maps, core_ids=list(range(NB_)))
    return np.stack([np.asarray(r["y"], dtype=np.float32) for r in res.results], axis=0)
```

```python
import types
import numpy as np
from contextlib import ExitStack
import concourse.bass as bass
import concourse.mybir as mybir
from concourse.bass_utils import run_bass_kernel_spmd

F32 = mybir.dt.float32
BF16 = mybir.dt.bfloat16
I32 = mybir.dt.int32
ALU = mybir.AluOpType
AF = mybir.ActivationFunctionType
AX = mybir.AxisListType

NDMASEM = 8
D = 1024
KC = 8
NEG = -30000.0
NCW = 3400 + 390 + 390
NIT = 10


def _freeze(fn):
    if fn.__closure__ is None:
        return fn
    cells = []
    for c in fn.__closure__:
        try:
            cells.append(types.CellType(c.cell_contents))
        except ValueError:
            cells.append(c)
    g = types.FunctionType(fn.__code__, fn.__globals__, fn.__name__, fn.__defaults__, tuple(cells))
    g.__kwdefaults__ = fn.__kwdefaults__
    return g


class Prog:
    ENGS = ("pe", "act", "dve", "pool", "sp")

    def __init__(self, nc):
        self.nc = nc
        self.ops = []
        self.state = {}
        self.es = ExitStack()
        self.ph = []
        self.pending = {e: None for e in self.ENGS}
        self.lastc = {e: None for e in self.ENGS}
        self.lastd = {}
        self.dcount = {e: 0 for e in self.ENGS}
        self.uid = 0

    def sb(self, name, shape, dt, glob=False):
        self.uid += 1
        st = self.es if (glob or not self.ph) else self.ph[-1]
        return st.enter_context(self.nc.sbuf_tensor("%s_%d" % (name, self.uid), list(shape), dt))

    def ps(self, name, shape, dt):
        self.uid += 1
        st = self.es if not self.ph else self.ph[-1]
        return st.enter_context(self.nc.psum_tensor("%s_%d" % (name, self.uid), list(shape), dt))

    def phase_begin(self):
        self.ph.append(ExitStack())

    def phase_end(self):
        fence = set()
        for e in self.ENGS:
            if self.lastc[e] is not None:
                fence.add(self.lastc[e])
        fence.update(self.lastd.values())
        for e in self.ENGS:
            self.pending[e] = set(fence) | (self.pending[e] or set())
        self.ph.pop().close()

    def op(self, eng, fn, reads=(), writes=(), dma=False):
        i = len(self.ops)
        deps = set()
        rawset = set()
        for r in reads:
            st = self.state.setdefault(r, [None, []])
            if st[0] is not None:
                deps.add(st[0])
                rawset.add(st[0])
        for w in writes:
            st = self.state.setdefault(w, [None, []])
            if st[0] is not None:
                deps.add(st[0])
            deps.update(st[1])
        for r in reads:
            self.state[r][1].append(i)
        for w in writes:
            st = self.state[w]
            st[0] = i
            st[1] = []
        if self.pending[eng]:
            deps |= self.pending[eng]
            rawset |= self.pending[eng]
            self.pending[eng] = None
        deps.discard(i)
        if dma:
            n = self.dcount[eng]
            self.dcount[eng] += 1
            self.lastd[(eng, n % NDMASEM)] = i
        else:
            self.lastc[eng] = i
        self.ops.append(dict(eng=eng, fn=_freeze(fn), deps=deps, raw=rawset, dma=dma, sig=dma))
        return i

    def dma(self, eng, out, in_, reads=(), writes=(), **kw):
        return self.op(eng, lambda e: e.dma_start(out=out, in_=in_, **kw), reads, writes, dma=True)

    def emit(self):
        nc = self.nc
        ops = self.ops
        for i, o in enumerate(ops):
            keep = set()
            for j in o["deps"]:
                pj = ops[j]
                if (not pj["dma"]) and (not o["dma"]) and pj["eng"] == o["eng"]:
                    if o["eng"] == "pe":
                        continue
                keep.add(j)
            o["deps"] = keep
        for o in ops:
            for j in o["deps"]:
                ops[j]["sig"] = True
        LIM = 30000
        DLIM = 1800
        cnt = {e: 0 for e in self.ENGS}
        dcnt = {e: 0 for e in self.ENGS}
        dsemcnt = {}
        dprev = {}
        for o in ops:
            e = o["eng"]
            if o["dma"]:
                n = dcnt[e]
                dcnt[e] += 1
                slot = n % NDMASEM
                k = dsemcnt.get((e, slot), 0)
                dsemcnt[(e, slot)] = k + 1
                o["prev"] = dprev.get((e, slot))
                o["semkey"] = ("d", e, slot, k // DLIM)
                o["semval"] = 16 * (k % DLIM + 1)
                dprev[(e, slot)] = (o["semkey"], o["semval"])
            elif o["sig"]:
                n = cnt[e]
                cnt[e] += 1
                o["semkey"] = ("c", e, n // LIM)
                o["semval"] = n % LIM + 1
        es = self.es
        sems = {}
        finals = {}
        for o in ops:
            if "semkey" in o:
                k = o["semkey"]
                if k not in sems:
                    sems[k] = es.enter_context(nc.semaphore("q_" + "_".join(str(x) for x in k)))
                finals[k] = max(finals.get(k, 0), o["semval"])
        self.nsems = len(sems)
        byeng = {e: [o for o in ops if o["eng"] == e] for e in self.ENGS}
        self.stats = {e: len(byeng[e]) for e in self.ENGS}

        def run(eng_name, engobj):
            waited = {}
            for o in byeng[eng_name]:
                need = {}
                for j in o["deps"]:
                    pj = ops[j]
                    k, v = pj["semkey"], pj["semval"]
                    if v > need.get(k, 0):
                        need[k] = v
                if o["dma"] and o["prev"] is not None:
                    k, v = o["prev"]
                    if v > need.get(k, 0):
                        need[k] = v
                for k, v in need.items():
                    if v > waited.get(k, 0):
                        engobj.wait_ge(sems[k], v)
                        waited[k] = v
                ins = o["fn"](engobj)
                if o["sig"]:
                    ins.then_inc(sems[o["semkey"]], 16 if o["dma"] else 1)
            if eng_name == "sp":
                for k, v in finals.items():
                    if v > waited.get(k, 0) and v > 0:
                        engobj.wait_ge(sems[k], v)

        with nc.Block() as block:
            @block.tensor
            def _(e):
                run("pe", e)

            @block.scalar
            def _(e):
                run("act", e)

            @block.vector
            def _(e):
                run("dve", e)

            @block.gpsimd
            def _(e):
                run("pool", e)

            @block.sync
            def _(e):
                run("sp", e)
        es.close()


_REGS = {}


def _breg(e, val):
    k = (id(e), val)
    if k not in _REGS:
        _REGS[k] = e.to_reg(val)
    return _REGS[k]


class Rot:
    def __init__(self, items):
        self.items = list(items)
        self.i = 0

    def next(self):
        it = self.items[self.i % len(self.items)]
        self.i += 1
        return it


FM_UNITS = [("qm", 0, 3, 128), ("km", 384, 3, 128), ("qd", 1664, 3, 128), ("kd", 2048, 3, 128),
            ("qi", 2816, 8, 64), ("ki", 3328, 1, 64)]
GLU_A = 1152
GLU_G = 1408


def build(S, NL, dbg=()):
    NT = S // 128
    NG = S // 512
    NB = S // 256
    nc = bass.Bass("TRN2", target_bir_lowering=False)

    def din(name, shape, dt=F32):
        return nc.dram_tensor(name, list(shape), dt, kind="ExternalInput").ap()

    def dscr(name, shape, dt):
        kind = "ExternalOutput" if name in dbg else "Internal"
        return nc.dram_tensor(name, list(shape), dt, kind=kind).ap()

    x_in = din("x", [S, D])
    c_col = din("c_col", [128, 8])
    ada_w = din("ada_w", [NL, D, 6 * D])
    ada_bB = din("ada_bB", [NL, 128, 6 * D])
    n1g_col = din("n1g_col", [NL, 128, 8])
    w_in = din("w_in", [NL, D, 3400])
    conv_wT = din("conv_wT", [NL, 128, 2, 31])
    conv_bB = din("conv_bB", [NL, 128, 256])
    ln_gB = din("ln_gB", [NL, 128, 256])
    ln_bB = din("ln_bB", [NL, 128, 256])
    w_out = din("w_out", [NL, D, D])
    n2gB = din("n2gB", [NL, 128, D])
    rw = din("rw", [NL, D, 36])
    rbBin = din("rbB", [NL, 128, 36])
    w1 = din("w1", [NL * 32 * D, 512])
    w3 = din("w3", [NL * 32 * D, 512])
    w2 = din("w2", [NL * 32 * 512, D])
    final_gB = din("final_gB", [128, D])
    mgB = din("mgB", [NL, 128, 384])
    dgB = din("dgB", [NL, 128, 384])
    y_out = nc.dram_tensor("y", [S, D], F32, kind="ExternalOutput").ap()

    scr = {}
    for nm, rows, M in [("qm", 384, 128), ("km", 384, 128), ("qd", 384, 128), ("kd", 384, 128), ("qi", 512, 64), ("ki", 64, 64)]:
        scr[nm] = dscr("s_" + nm, [rows, S], BF16)
    scr["u"] = dscr("s_u", [256, S + 32], BF16)
    scr["vm"] = dscr("s_vm", [S, 390], BF16)
    scr["vd"] = dscr("s_vd", [S, 390], BF16)
    scr["wi"] = dscr("s_wi", [S, 8], F32)
    scr["mix"] = dscr("s_mix", [S, D], BF16)
    NBLK_ = 2 * S // 512 + 32
    scr["xmid"] = dscr("s_xmid", [S, D], F32)
    scr["h2"] = dscr("s_h2", [S, D], BF16)
    scr["buf"] = dscr("s_buf", [NBLK_ * 512, D], BF16)
    scr["ybuf"] = dscr("s_ybuf", [NBLK_ * 512, D], F32)
    xbuf = [dscr("s_xbuf%d" % i, [S, D], F32) for i in range(2)]
    d_sel = [dscr("s_sel%d" % i, [128, NT, 32], F32) for i in range(2)]
    d_gates = dscr("s_gates", [128, NT, 2], F32)
    d_dest = dscr("s_dest", [128, NT, 2], I32)
    d_be = dscr("s_be", [128, NBLK_], F32)
    d_modB = dscr("s_modB", [128, 6 * D], F32)

    P = Prog(nc)
    identf = P.sb("identf", [128, 128], F32, glob=True)
    identb = P.sb("identb", [128, 128], BF16, glob=True)
    io = P.sb("io", [128, 128], F32, glob=True)
    pid = P.sb("pid", [128, 1], F32, glob=True)
    P.op("pool", lambda e: e.iota(io[:], pattern=[[1, 128]], base=0, channel_multiplier=0, allow_small_or_imprecise_dtypes=True), writes=["io"])
    P.op("pool", lambda e: e.iota(pid[:], pattern=[[0, 1]], base=0, channel_multiplier=1, allow_small_or_imprecise_dtypes=True), writes=["pid"])
    P.op("dve", lambda e: e.tensor_scalar(identf[:], io[:], pid[:, 0:1], None, op0=ALU.is_equal), reads=["io", "pid"], writes=["identf"])
    P.op("dve", lambda e: e.tensor_copy(identb[:], identf[:]), reads=["identf"], writes=["identb"])

    for l in range(NL):
        P.phase_begin()
        modB = P.sb("modB", [128, 6 * D], F32)
        P.phase_begin()
        cc = P.sb("cc", [128, 8], F32)
        sc = P.sb("sc", [128, 8], F32)
        screp = P.sb("screp", [128, 8, 128], F32)
        abB = P.sb("abB", [128, 6 * D], F32)
        P.dma("sp", cc[:], c_col, writes=["cc"])
        P.dma("sp", abB[:], ada_bB[l], writes=["abB"])
        P.op("act", lambda e: e.activation(sc[:], cc[:], AF.Silu), reads=["cc"], writes=["sc"])
        for kc in range(KC):
            P.op("dve", lambda e, kc=kc: e.tensor_scalar(screp[:, kc, :], io[:], 0.0, sc[:, kc:kc + 1], op0=ALU.mult, op1=ALU.add),
                 reads=["io", "sc"], writes=["screp"])
        awt = Rot([(P.sb("awt", [128, 8, 512], F32), "awt%d" % i) for i in range(2)])
        pm = Rot([(P.ps("pm", [128, 512], F32), "pm%d" % i) for i in range(2)])
        for fc in range(12):
            wt, wn = awt.next()
            pt, pn = pm.next()
            P.dma("sp", wt[:], ada_w[l, :, fc * 512:(fc + 1) * 512].rearrange("(kc p) n -> p kc n", p=128), writes=[wn])
            for kc in range(KC):
                P.op("pe", lambda e, kc=kc, wt=wt, pt=pt: e.matmul(pt[:], screp[:, kc, :], wt[:, kc, :], start=(kc == 0), stop=(kc == KC - 1)),
                     reads=["screp", wn], writes=[pn])
            P.op("dve", lambda e, pt=pt, fc=fc: e.tensor_tensor(out=modB[:, fc * 512:(fc + 1) * 512], in0=pt[:], in1=abB[:, fc * 512:(fc + 1) * 512], op=ALU.add),
                 reads=["abB"], writes=[pn, "modB"])
        P.dma("sp", d_modB, modB[:], reads=["modB"], writes=["s_modB"])
        P.phase_end()

        P.phase_begin()
        Wp = P.sb("Wp", [128, KC, NCW], BF16)
        sh1rep = P.sb("sh1rep", [128, KC, 128], F32)
        gmodT = P.sb("gmodT", [128, KC], F32)
        n1c = P.sb("n1c", [128, KC], F32)
        bB = P.sb("bB", [128, 3400], F32)
        bBv = P.sb("bBv", [128, 2, 6, 65], F32)
        biasT = P.sb("biasT", [128, 32], F32)
        P.dma("sp", n1c[:], n1g_col[l], writes=["n1c"])
        P.phase_begin()
        ptr = Rot([(P.ps("ptr", [128, 512], F32), "ptr%d" % i) for i in range(2)])
        for kc in range(KC):
            pt, pn = ptr.next()
            P.op("pe", lambda e, pt=pt, kc=kc: e.transpose(pt[:, 0:128], modB[:, kc * 128:(kc + 1) * 128], identf[:]), reads=["modB", "identf"], writes=[pn])
            P.op("act", lambda e, pt=pt, kc=kc: e.copy(sh1rep[:, kc, :], pt[:, 0:128]), reads=[], writes=[pn, "sh1rep"])
            pt, pn = ptr.next()
            P.op("pe", lambda e, pt=pt, kc=kc: e.transpose(pt[:, 0:128], modB[:, D + kc * 128:D + (kc + 1) * 128], identf[:]), reads=["modB", "identf"], writes=[pn])
            P.op("dve", lambda e, pt=pt, kc=kc: e.scalar_tensor_tensor(out=gmodT[:, kc:kc + 1], in0=pt[:, 0:1], scalar=1.0, in1=n1c[:, kc:kc + 1], op0=ALU.add, op1=ALU.mult),
                 reads=["n1c"], writes=[pn, "gmodT"])
        P.op("pool", lambda e: e.memset(Wp[:, :, 3400:NCW], 0.0), writes=["Wp"])
        wst = Rot([(P.sb("wst", [128, 3400], F32), "wst%d" % i) for i in range(2)])
        for kc in range(KC):
            wt, wn = wst.next()
            P.dma("sp", wt[:], w_in[l, kc * 128:(kc + 1) * 128, :], writes=[wn])
            P.op("dve", lambda e, wt=wt, kc=kc: e.tensor_scalar(Wp[:, kc, 0:3400], wt[:], gmodT[:, kc:kc + 1], None, op0=ALU.mult),
                 reads=[wn, "gmodT"], writes=["Wp"])
            for vi, c0 in ((0, 768), (1, 2432)):
                P.op("pool", lambda e, wt=wt, kc=kc, vi=vi, c0=c0: e.tensor_scalar(
                    Wp[:, kc, 3400 + vi * 390:3400 + (vi + 1) * 390].rearrange("p (h d) -> p h d", d=65)[:, :, 0:64],
                    wt[:, c0:c0 + 384].rearrange("p (h d) -> p h d", d=64), gmodT[:, kc:kc + 1], None, op0=ALU.mult),
                    reads=[wn, "gmodT"], writes=["Wp"])
        wbt = Rot([(P.sb("wbt", [128, KC, 512], F32), "wbt%d" % i) for i in range(2)])
        pbb = Rot([(P.ps("pbb", [128, 512], F32), "pbb%d" % i) for i in range(2)])
        for cc_ in range(7):
            c0 = cc_ * 512
            n = min(512, 3400 - c0)
            wt, wn = wbt.next()
            pt, pn = pbb.next()
            P.dma("sp", wt[:, :, 0:n], w_in[l, :, c0:c0 + n].rearrange("(kc p) n -> p kc n", p=128), writes=[wn])
            for kc in range(KC):
                P.op("pe", lambda e, kc=kc, wt=wt, pt=pt, n=n: e.matmul(pt[:, 0:n], sh1rep[:, kc, :], wt[:, kc, 0:n], start=(kc == 0), stop=(kc == KC - 1)),
                     reads=["sh1rep", wn], writes=[pn])
            P.op("act", lambda e, pt=pt, c0=c0, n=n: e.copy(bB[:, c0:c0 + n], pt[:, 0:n]), reads=[], writes=[pn, "bB"])
        P.op("pool", lambda e: e.memset(bBv[:], 1.0), writes=["bBv"])
        for vi, c0 in ((0, 768), (1, 2432)):
            P.op("dve", lambda e, vi=vi, c0=c0: e.tensor_copy(bBv[:, vi, :, 0:64], bB[:, c0:c0 + 384].rearrange("p (h d) -> p h d", d=64)),
                 reads=["bB"], writes=["bBv"])
        ucols = []
        for nm, c0, nu, M in FM_UNITS:
            for u in range(nu):
                ucols.append((nm, u, c0 + u * M, M))
        for u in range(2):
            ucols.append(("ca", u, GLU_A + u * 128, 128))
        for u in range(2):
            ucols.append(("cg", u, GLU_G + u * 128, 128))
        bidx = {}
        for k, (nm, u, c0, M) in enumerate(ucols):
            bidx[(nm, u)] = k
            pt, pn = ptr.next()
            P.op("pe", lambda e, pt=pt, c0=c0, M=M: e.transpose(pt[0:M, 0:128], bB[:, c0:c0 + M], identf[:]), reads=["bB", "identf"], writes=[pn])
            P.op("act", lambda e, pt=pt, k=k, M=M: e.copy(biasT[0:M, k:k + 1], pt[0:M, 0:1]), reads=[], writes=[pn, "biasT"])

        P.phase_end()
        xg = Rot([(P.sb("xg", [128, 4, D], F32), "xg%d" % i) for i in range(2)])
        xb = Rot([(P.sb("xb", [128, D], BF16), "xb%d" % i) for i in range(2)])
        xT = Rot([(P.sb("xT", [128, KC, 512], BF16), "xT%d" % i) for i in range(2)])
        junk = P.sb("junk", [128, D], F32)
        ss = Rot([(P.sb("ss", [128, 4], F32), "ss%d" % i) for i in range(2)])
        rs = Rot([(P.sb("rs", [128, 4], F32), "rs%d" % i) for i in range(2)])
        pT = Rot([(P.ps("pT", [128, KC, 128], BF16), "pT%d" % i) for i in range(2)])
        pu = Rot([(P.ps("pu", [128, 512], F32), "pu%d" % i) for i in range(4)])
        pv = Rot([(P.ps("pv", [128, 512], F32), "pv%d" % i) for i in range(2)])
        stg = Rot([(P.sb("stg", [128, 512], BF16), "stg%d" % i) for i in range(4)])
        sig = Rot([(P.sb("sig", [128, 512], F32), "sig%d" % i) for i in range(2)])
        stv = Rot([(P.sb("stv", [128, 390], BF16), "stv%d" % i) for i in range(3)])
        stw = Rot([(P.sb("stw", [128, 8], F32), "stw%d" % i) for i in range(2)])
        zt = P.sb("zt", [128, 32], BF16)
        P.op("pool", lambda e: e.memset(zt[:], 0.0), writes=["zt"])
        for h in range(2):
            P.dma("sp", scr["u"][h * 128:(h + 1) * 128, 0:32], zt[:], reads=["zt"], writes=["s_u"])

        def load_x(G):
            t, n = xg.next()
            P.dma("sp", t[:], (x_in if l == 0 else xbuf[(l - 1) % 2])[G * 512:(G + 1) * 512, :].rearrange("(j p) d -> p j d", p=128), reads=["xsrc"], writes=[n])
            return t, n

        nxt = load_x(0)
        for G in range(NG):
            xt_, xn = nxt
            if G + 1 < NG:
                nxt = load_x(G + 1)
            sst, ssn = ss.next()
            rst, rsn = rs.next()
            xTt, xTn = xT.next()
            for j in range(4):
                P.op("act", lambda e, j=j, xt_=xt_, sst=sst: e.activation(junk[:], xt_[:, j, :], AF.Square, accum_out=sst[:, j:j + 1]),
                     reads=[xn], writes=["junk", ssn])
            P.op("act", lambda e, sst=sst, rst=rst: e.activation(rst[:], sst[:], AF.Sqrt, bias=1e-6, scale=1.0 / D), reads=[ssn], writes=[rsn])
            P.op("dve", lambda e, rst=rst: e.reciprocal(rst[:], rst[:]), reads=[rsn], writes=[rsn])
            for j in range(4):
                xbt, xbn = xb.next()
                ptt, ptn = pT.next()
                P.op("dve", lambda e, j=j, xt_=xt_, xbt=xbt, rst=rst: e.tensor_scalar(xbt[:], xt_[:, j, :], rst[:, j:j + 1], None, op0=ALU.mult),
                     reads=[xn, rsn], writes=[xbn])
                for kc in range(KC):
                    P.op("pe", lambda e, kc=kc, xbt=xbt, ptt=ptt: e.transpose(ptt[:, kc, :], xbt[:, kc * 128:(kc + 1) * 128], identb[:]),
                         reads=[xbn, "identb"], writes=[ptn])
                P.op("act", lambda e, j=j, xTt=xTt, ptt=ptt: e.copy(xTt[:, :, j * 128:(j + 1) * 128], ptt[:]), reads=[], writes=[ptn, xTn])
            for nm, c0, nu, M in FM_UNITS:
                for u in range(nu):
                    put, pun = pu.next()
                    sgt, sgn = stg.next()
                    cs = c0 + u * M
                    for kc in range(KC):
                        P.op("pe", lambda e, kc=kc, put=put, cs=cs, M=M, xTt=xTt: e.matmul(put[0:M, :], Wp[:, kc, cs:cs + M], xTt[:, kc, :], start=(kc == 0), stop=(kc == KC - 1)),
                             reads=["Wp", xTn], writes=[pun])
                    k = bidx[(nm, u)]
                    P.op("act", lambda e, put=put, sgt=sgt, M=M, k=k: e.activation(sgt[0:M, :], put[0:M, :], AF.Identity, bias=biasT[0:M, k:k + 1]),
                         reads=["biasT"], writes=[pun, sgn])
                    P.dma("sp", scr[nm][u * M:(u + 1) * M, G * 512:(G + 1) * 512], sgt[0:M, :], reads=[sgn], writes=["s_" + nm])
            for u in range(2):
                pa, pan = pu.next()
                pg, pgn = pu.next()
                sgt, sgn = stg.next()
                sit, sin_ = sig.next()
                for kc in range(KC):
                    P.op("pe", lambda e, kc=kc, pa=pa, u=u, xTt=xTt: e.matmul(pa[:], Wp[:, kc, GLU_A + u * 128:GLU_A + (u + 1) * 128], xTt[:, kc, :], start=(kc == 0), stop=(kc == KC - 1)),
                         reads=["Wp", xTn], writes=[pan])
                for kc in range(KC):
                    P.op("pe", lambda e, kc=kc, pg=pg, u=u, xTt=xTt: e.matmul(pg[:], Wp[:, kc, GLU_G + u * 128:GLU_G + (u + 1) * 128], xTt[:, kc, :], start=(kc == 0), stop=(kc == KC - 1)),
                         reads=["Wp", xTn], writes=[pgn])
                kg = bidx[("cg", u)]
                ka = bidx[("ca", u)]
                P.op("act", lambda e, pg=pg, sit=sit, kg=kg: e.activation(sit[:], pg[:], AF.Sigmoid, bias=biasT[:, kg:kg + 1]),
                     reads=["biasT"], writes=[pgn, sin_])
                P.op("dve", lambda e, pa=pa, sit=sit, sgt=sgt, ka=ka: e.scalar_tensor_tensor(out=sgt[:], in0=pa[:], scalar=biasT[:, ka:ka + 1], in1=sit[:], op0=ALU.add, op1=ALU.mult),
                     reads=["biasT", sin_], writes=[pan, sgn])
                P.dma("sp", scr["u"][u * 128:(u + 1) * 128, 32 + G * 512:32 + (G + 1) * 512], sgt[:], reads=[sgn], writes=["s_u"])
            for j in range(4):
                r0 = G * 512 + j * 128
                for vi, nm in ((0, "vm"), (1, "vd")):
                    pvt, pvn = pv.next()
                    svt, svn = stv.next()
                    for kc in range(KC):
                        P.op("pe", lambda e, kc=kc, pvt=pvt, vi=vi, j=j, xTt=xTt: e.matmul(pvt[:, 0:390], xTt[:, kc, j * 128:(j + 1) * 128], Wp[:, kc, 3400 + vi * 390:3400 + (vi + 1) * 390], start=(kc == 0), stop=(kc == KC - 1)),
                             reads=["Wp", xTn], writes=[pvn])
                    P.op("dve", lambda e, pvt=pvt, svt=svt, vi=vi: e.tensor_tensor(out=svt[:], in0=pvt[:, 0:390], in1=bBv[:, vi].rearrange("p h d -> p (h d)"), op=ALU.add),
                         reads=["bBv"], writes=[pvn, svn])
                    P.dma("sp", scr[nm][r0:r0 + 128, :], svt[:], reads=[svn], writes=["s_" + nm])
                pvt, pvn = pv.next()
                swt, swn = stw.next()
                for kc in range(KC):
                    P.op("pe", lambda e, kc=kc, pvt=pvt, j=j, xTt=xTt: e.matmul(pvt[:, 0:8], xTt[:, kc, j * 128:(j + 1) * 128], Wp[:, kc, 3392:3400], start=(kc == 0), stop=(kc == KC - 1)),
                         reads=["Wp", xTn], writes=[pvn])
                P.op("dve", lambda e, pvt=pvt, swt=swt: e.tensor_tensor(out=swt[:], in0=pvt[:, 0:8], in1=bB[:, 3392:3400], op=ALU.add),
                     reads=["bB"], writes=[pvn, swn])
                P.dma("sp", scr["wi"][r0:r0 + 128, :], swt[:], reads=[swn], writes=["s_wi"])
        P.phase_end()
        P.phase_end()


        for kind in ("moba", "dsa"):
            P.phase_begin()
            qn, kn, vn = ("qm", "km", "vm") if kind == "moba" else ("qd", "kd", "vd")
            kT = P.sb("kT", [128, 3, S], BF16)
            V = P.sb("V", [128, NT, 390], BF16)
            gB = P.sb("gB", [128, 384], F32)
            for p in range(3):
                P.dma("sp", kT[:, p, :], scr[kn][p * 128:(p + 1) * 128, :], reads=["s_" + kn], writes=["kT"])
            for c8 in range(0, NT, 8):
                P.dma("sp", V[:, c8:c8 + 8, :], scr[vn][c8 * 128:(c8 + 8) * 128, :].rearrange("(c p) n -> p c n", p=128), reads=["s_" + vn], writes=["V"])
            P.dma("sp", gB[:], (mgB if kind == "moba" else dgB)[l], writes=["gB"])
            tri = P.sb("tri", [128, 128], BF16)
            P.op("dve", lambda e: e.tensor_scalar(tri[:], io[:], pid[:, 0:1], NEG, op0=ALU.is_gt, op1=ALU.mult), reads=["io", "pid"], writes=["tri"])
            nbuf = 2 if kind == "moba" else 1
            QW = 512 if kind == "moba" else 128
            qpads = [P.sb("qpad", [128, 6, QW], BF16) for _ in range(2)]
            for i in range(2):
                P.op("pool", lambda e, i=i: e.memset(qpads[i][:], 0.0), writes=["qpad%d" % i])
            qgs = Rot([(P.sb("qg", [128, 3, QW], BF16), "qg%d" % i) for i in range(2)])
            junk2 = P.sb("junk2", [128, 384], BF16)
            Sps = Rot([(P.ps("Sps", [128, 4, 128], F32), "Sps%d" % i) for i in range(3 if kind == "moba" else 2)])
            Ops = Rot([(P.ps("Ops", [128, 512], F32), "Ops%d" % i) for i in range(2)])
            if kind == "dsa":
                Xps = Rot([(P.ps("Xps", [128, 512], F32), "Xps%d" % i) for i in range(3)])
            else:
                pmisc = P.ps("pmisc", [128, 1024], BF16)
                pgate = P.ps("pgate", [128, 16, 32], F32)
            PTs = Rot([(P.sb("PT", [128, 4, 128], BF16), "PT%d" % i) for i in range(3 if kind == "moba" else 2)])
            osbs = Rot([(P.sb("osb", [128, 6, 65], F32), "osb%d" % i) for i in range(2 if kind == "moba" else 1)])
            ym = P.sb("ym", [128, 6, 64], F32)
            rden = P.sb("rden", [128, 6], F32)
            sst = P.sb("sst2", [128, 2], F32)
            mixo = Rot([(P.sb("mixo", [128, 384], BF16), "mixo%d" % i) for i in range(2 if kind == "moba" else 1)])
            if kind == "moba":
                ksum = P.sb("ksum", [128, 3, NB], F32)
                kmeanb = P.sb("kmeanb", [128, 3, 32], BF16)
                Eall = P.sb("Eall", [128, 32, 128], BF16)
                gate_sb = P.sb("gate_sb", [128, 6, 32], F32)
                mx = P.sb("mx", [128, 6, 8], F32)
                selb = P.sb("selb", [128, 6, 32], BF16)
                sbTs = Rot([(P.sb("sbT", [128, 6, 128], BF16), "sbT%d" % i) for i in range(2)])
                for (t_, n_) in sbTs.items:
                    P.op("pool", lambda e, t_=t_: e.memset(t_[:], 0.0), writes=[n_])
                for p in range(3):
                    P.op("dve", lambda e, p=p: e.tensor_reduce(out=ksum[:, p, :], in_=kT[:, p, :].rearrange("r (n k) -> r n k", k=256), axis=AX.X, op=ALU.add),
                         reads=["kT"], writes=["ksum"])
                P.op("pool", lambda e: e.memset(kmeanb[:], 0.0), writes=["kmeanb"])
                P.op("dve", lambda e: e.tensor_scalar(kmeanb[:, :, 0:NB], ksum[:], 1.0 / 256, None, op0=ALU.mult), reads=["ksum"], writes=["kmeanb"])
                P.op("pool", lambda e: e.memset(Eall[:], 0.0), writes=["Eall"])
                P.op("dve", lambda e: e.tensor_copy(Eall[0:32], identf[0:32, 0:32, None].to_broadcast([32, 32, 128])), reads=["identf"], writes=["Eall"])
                P.op("pool", lambda e: e.memset(gate_sb[:], -1e30), writes=["gate_sb"])
            else:
                kiT = P.sb("kiT", [128, S], BF16)
                P.op("pool", lambda e: e.memset(kiT[64:128, :], 0.0), writes=["kiT"])
                P.dma("sp", kiT[0:64, :], scr["ki"], reads=["s_ki"], writes=["kiT"])
                qis = Rot([(P.sb("qi", [128, 8, 128], BF16), "qi%d" % i) for i in range(2)])
                for (t_, n_) in qis.items:
                    P.op("pool", lambda e, t_=t_: e.memset(t_[:], 0.0), writes=[n_])
                wis = Rot([(P.sb("wi", [128, 4, 8], F32), "wi%d" % i) for i in range(2)])
                acc = P.sb("acc", [128, S], F32)
                dbs = Rot([(P.sb("dbias", [128, S], BF16), "dbias%d" % i) for i in range(2)])
                rls = Rot([(P.sb("rl", [128, 512], BF16), "rl%d" % i) for i in range(8)])
                dWs = Rot([(P.sb("dW", [128, 8, 128], BF16), "dW%d" % i) for i in range(1)])
                accp = P.ps("accp", [128, 512], F32)
                cntA = Rot([(P.sb("cntA", [128, 1], F32), "cntA%d" % i) for i in range(2)])
                tmpc = Rot([(P.sb("tmpc", [128, 1], F32), "tmpc%d" % i) for i in range(2)])
                triD = P.sb("triD", [128, 128], F32)
                P.op("dve", lambda e: e.tensor_scalar(triD[:], io[:], pid[:, 0:1], -1e30, op0=ALU.is_gt, op1=ALU.mult), reads=["io", "pid"], writes=["triD"])
                pw2 = P.sb("pw2", [128, NIT + 1], F32)
                for k in range(NIT + 1):
                    P.op("pool", lambda e, k=k: e.memset(pw2[:, k:k + 1], 2.0 ** -(k + 1)), writes=["pw2"])
                Wk = P.sb("Wk", [128, NIT + 1], F32)
                bs = P.sb("bs", [128, 8], F32)
                los = Rot([(P.sb("lo", [128, 1], F32), "lo%d" % i) for i in range(2)])
                mids = Rot([(P.sb("mid", [128, 1], F32), "mid%d" % i) for i in range(2)])
                cnts = Rot([(P.sb("cnt", [128, 1], F32), "cnt%d" % i) for i in range(2)])
                tts = Rot([(P.sb("tt", [128, 1], F32), "tt%d" % i) for i in range(2)])

            def load_q(G, idx):
                if not idx:
                    t, n = qgs.next()
                    P.dma("sp", t[:], scr[qn][:, G * QW:(G + 1) * QW].rearrange("(p r) t -> r p t", r=128), reads=["s_" + qn], writes=[n])
                    return [(t, n)]
                t2, n2 = None, None
                t3, n3 = wis.next()
                P.dma("sp", t3[:], scr["wi"][G * 512:(G + 1) * 512, :].rearrange("(j p) h -> p j h", p=128), reads=["s_wi"], writes=[n3])
                return [(t2, n2), (t3, n3)]

            grp = {}

            def load_att(G):
                cur = load_q(G, False)
                qg, qgn = cur[0]
                qpad = qpads[G % 2]
                qpn = "qpad%d" % (G % 2)
                P.op("act", lambda e, qpad=qpad, qg=qg: e.copy(qpad[0:64].rearrange("r (p two) t -> r p two t", two=2)[:, :, 0, :], qg[0:64, :, :]), reads=[qgn], writes=[qpn])
                P.op("dve", lambda e, qpad=qpad, qg=qg: e.tensor_copy(qpad[64:128].rearrange("r (p two) t -> r p two t", two=2)[:, :, 1, :], qg[64:128, :, :]), reads=[qgn], writes=[qpn])
                grp[G] = (qpad, qpn)

            tst = {}

            def pre(G, j):
                qt = 4 * G + j
                nch = qt + 1
                if kind == "moba":
                    if G not in grp:
                        load_att(G)
                    qpad, qpn = grp[G]
                else:
                    if j == 0:
                        cur = load_q(G, True)
                        tst["qi"] = cur
                    qi, qin = qis.next()
                    P.dma("sp", qi[0:64], scr["qi"][:, qt * 128:(qt + 1) * 128].rearrange("(h d) t -> d h t", d=64), reads=["s_qi"], writes=[qin])
                    wi, win = tst["qi"][1]
                b = qt // 2
                sbT = sbTn = dbias = dbn = None
                qt = 4 * G + j
                nch = qt + 1
                if kind == "moba":
                    b = qt // 2
                    sbT, sbTn = sbTs.next()
                    if b > 0:
                        for h in range(6):
                            P.op("pe", lambda e, h=h, j=j, qpad=qpad: e.matmul(pgate[:, h, :], qpad[:, h, j * 128:(j + 1) * 128], kmeanb[:, h // 2, :], start=True, stop=True),
                                 reads=[qpn, "kmeanb"], writes=["Xg"])
                        P.op("dve", lambda e, b=b: e.tensor_copy(gate_sb[:, :, 0:b], pgate[:, 0:6, 0:b]), reads=[], writes=["Xg", "gate_sb"])
                        for h in range(6):
                            P.op("dve", lambda e, h=h: e.max(out=mx[:, h, :], in_=gate_sb[:, h, :]), reads=["gate_sb"], writes=["mx"])
                        for h in range(6):
                            P.op("dve", lambda e, h=h: e.tensor_scalar(selb[:, h, :], gate_sb[:, h, :], mx[:, h, 2:3], NEG, op0=ALU.is_lt, op1=ALU.mult),
                                 reads=["gate_sb", "mx"], writes=["selb"])
                        for h in range(6):
                            P.op("pe", lambda e, h=h: e.transpose(pmisc[0:32, h * 128:(h + 1) * 128], selb[:, h, :], identb[:]), reads=["selb", "identb"], writes=["pmisc"])
                        P.op("act", lambda e, sbT=sbT: e.copy(sbT[0:32], pmisc[0:32, 0:768].rearrange("n (h t) -> n h t", t=128)), reads=[], writes=["pmisc", sbTn])
                else:
                    L = nch * 128
                    dbias, dbn = dbs.next()
                    dW, dWn = dWs.next()
                    for h in range(8):
                        P.op("dve", lambda e, dW=dW, wi=wi, h=h, j=j: e.tensor_scalar(dW[:, h, :], identf[:], wi[:, j, h:h + 1], None, op0=ALU.mult),
                             reads=["identf", win], writes=[dWn])
                    for cc in range((L + 511) // 512):
                        n = min(512, L - cc * 512)
                        rr = []
                        for h in range(8):
                            xp, xpn = Xps.next()
                            rl, rln = rls.next()
                            rr.append((rl, rln))
                            P.op("pe", lambda e, xp=xp, h=h, cc=cc, n=n, qi=qi, j=j: e.matmul(xp[:, 0:n], qi[:, h, :], kiT[:, cc * 512:cc * 512 + n], start=True, stop=True),
                                 reads=[qin, "kiT"], writes=[xpn])
                            if h % 3 != 2:
                                P.op("act", lambda e, xp=xp, rl=rl, n=n: e.activation(rl[:, 0:n], xp[:, 0:n], AF.Relu), reads=[], writes=[xpn, rln])
                            else:
                                P.op("dve", lambda e, xp=xp, rl=rl, n=n: e.tensor_scalar(rl[:, 0:n], xp[:, 0:n], 0.0, None, op0=ALU.max), reads=[], writes=[xpn, rln])
                        for h in range(8):
                            rl, rln = rr[h]
                            P.op("pe", lambda e, rl=rl, dW=dW, h=h, n=n: e.matmul(accp[:, 0:n], dW[:, h, :], rl[:, 0:n], start=(h == 0), stop=(h == 7)),
                                 reads=[rln, dWn], writes=["accp"])
                        if cc % 2 == 0:
                            P.op("act", lambda e, cc=cc, n=n: e.copy(acc[:, cc * 512:cc * 512 + n], accp[:, 0:n]), reads=[], writes=["accp", "acc"])
                        else:
                            P.op("dve", lambda e, cc=cc, n=n: e.tensor_copy(acc[:, cc * 512:cc * 512 + n], accp[:, 0:n]), reads=[], writes=["accp", "acc"])
                        yield
                    P.op("dve", lambda e, L=L: e.tensor_reduce(out=bs[:, 0:1], in_=acc[:, 0:L], axis=AX.X, op=ALU.max, apply_absolute_value=True), reads=["acc"], writes=["bs"])
                    P.op("dve", lambda e: e.tensor_scalar(bs[:, 1:2], bs[:, 0:1], -1.0, None, op0=ALU.mult), reads=["bs"], writes=["bs"])
                    P.op("dve", lambda e, L=L: e.tensor_tensor(out=acc[:, L - 128:L], in0=acc[:, L - 128:L], in1=triD[:], op=ALU.add), reads=["triD"], writes=["acc"])
                    lo, lon = los.next()
                    P.op("dve", lambda e, lo=lo: e.tensor_scalar(lo[:], bs[:, 1:2], -1.0, None, op0=ALU.add), reads=["bs"], writes=[lon])
                    P.op("dve", lambda e: e.scalar_tensor_tensor(out=bs[:, 2:3], in0=bs[:, 0:1], scalar=2.0, in1=bs[:, 1:2], op0=ALU.add, op1=ALU.subtract), reads=["bs"], writes=["bs"])
                    P.op("dve", lambda e: e.tensor_scalar(Wk[:], pw2[:], bs[:, 2:3], None, op0=ALU.mult), reads=["pw2", "bs"], writes=["Wk"])
                    yield
                    for k in range(NIT):
                        mid, midn = mids.next()
                        cnt, cntn = cnts.next()
                        tt, ttn = tts.next()
                        lo2, lo2n = los.next()
                        P.op("dve", lambda e, lo=lo, mid=mid, k=k: e.tensor_tensor(out=mid[:], in0=lo[:], in1=Wk[:, k:k + 1], op=ALU.add), reads=[lon, "Wk"], writes=[midn])
                        P.op("dve", lambda e, mid=mid, cnt=cnt, L=L: e.tensor_scalar(dbias[:, 0:L], acc[:, 0:L], mid[:, 0:1], 0.0, op0=ALU.is_ge, op1=ALU.add, accum_out=cnt[:]),
                             reads=["acc", midn], writes=[dbn + "lo", cntn])
                        P.op("dve", lambda e, cnt=cnt, tt=tt, k=k: e.scalar_tensor_tensor(out=tt[:], in0=cnt[:], scalar=255.5, in1=Wk[:, k:k + 1], op0=ALU.is_ge, op1=ALU.mult), reads=[cntn, "Wk"], writes=[ttn])
                        P.op("dve", lambda e, lo=lo, lo2=lo2, tt=tt: e.tensor_tensor(out=lo2[:], in0=lo[:], in1=tt[:], op=ALU.add), reads=[lon, ttn], writes=[lo2n])
                        lo, lon = lo2, lo2n
                        yield
                    P.op("dve", lambda e, lo=lo, L=L: e.tensor_scalar(dbias[:, 0:L], acc[:, 0:L], lo[:, 0:1], NEG, op0=ALU.is_lt, op1=ALU.mult), reads=["acc", lon], writes=[dbn, dbn + "lo", dbn + "hi"])

                tst[qt] = (b, sbT, sbTn, dbias, dbn)
                yield

            def att(G, j, inter=None, nsteps=1):
                qt = 4 * G + j
                nch = qt + 1
                gk = G if kind == "moba" else qt
                if gk not in grp:
                    load_att(gk)
                qpad, qpn = grp[gk]
                jo = j if kind == "moba" else 0
                b, sbT, sbTn, dbias, dbn = tst.pop(qt)
                osb, osbn = osbs.next()
                units = []
                for h in range(6):
                    for c0 in range(0, nch, 4):
                        units.append((h, list(range(c0, min(c0 + 4, nch)))))

                def emitS(h, cs):
                    sp_, spn = Sps.next()
                    for ci, c in enumerate(cs):
                        if kind == "moba":
                            blk = c // 2
                            if blk < b:
                                bias = (Eall[:, blk, :], sbT[:, h, :], ["Eall", sbTn])
                            elif c == qt:
                                bias = (tri[:], identb[:], ["tri", "identb"])
                            else:
                                bias = None
                        else:
                            bias = (dbias[:, c * 128:(c + 1) * 128], identb[:], [dbn, "identb"])
                        P.op("pe", lambda e, sp_=sp_, ci=ci, c=c, h=h, jo=jo, qpad=qpad, bias=bias: e.matmul(sp_[:, ci, :], kT[:, h // 2, c * 128:(c + 1) * 128], qpad[:, h, jo * 128:(jo + 1) * 128], start=True, stop=(bias is None)),
                             reads=["kT", qpn], writes=[spn])
                        if bias is not None:
                            P.op("pe", lambda e, sp_=sp_, ci=ci, bias=bias: e.matmul(sp_[:, ci, :], bias[0], bias[1], start=False, stop=True),
                                 reads=bias[2], writes=[spn])
                    return sp_, spn

                pend = emitS(*units[0])
                ops_ = opn = None
                sdone = 0
                for ui, (h, cs) in enumerate(units):
                    if inter is not None:
                        want = ((ui + 1) * nsteps + len(units) - 1) // len(units)
                        while sdone < want:
                            next(inter, None)
                            sdone += 1
                    sp_, spn = pend
                    if ui + 1 < len(units):
                        pend = emitS(*units[ui + 1])
                    if cs[0] == 0:
                        ops_, opn = Ops.next()
                    pt_, ptn = PTs.next()
                    ncs = len(cs)
                    P.op("act", lambda e, sp_=sp_, pt_=pt_, ncs=ncs: e.activation(pt_[:, 0:ncs, :], sp_[:, 0:ncs, :], AF.Exp, scale=0.125), reads=[], writes=[spn, ptn])
                    for ci, c in enumerate(cs):
                        P.op("pe", lambda e, ops_=ops_, pt_=pt_, ci=ci, c=c, h=h: e.matmul(ops_[:, 0:65], pt_[:, ci, :], V[:, c, h * 65:(h + 1) * 65], start=(c == 0), stop=(c == nch - 1)),
                             reads=[ptn, "V"], writes=[opn])
                    if cs[-1] == nch - 1:
                        if h % 2 == 0:
                            P.op("dve", lambda e, ops_=ops_, osb=osb, h=h: e.tensor_copy(osb[:, h, :], ops_[:, 0:65]), reads=[], writes=[opn, osbn])
                        else:
                            P.op("act", lambda e, ops_=ops_, osb=osb, h=h: e.copy(osb[:, h, :], ops_[:, 0:65]), reads=[], writes=[opn, osbn])
                P.op("dve", lambda e, osb=osb: e.reciprocal(rden[:], osb[:, :, 64]), reads=[osbn], writes=["rden"])
                P.op("dve", lambda e, osb=osb: e.tensor_tensor(out=ym[:], in0=osb[:, :, 0:64], in1=rden[:, :, None].to_broadcast([128, 6, 64]), op=ALU.mult), reads=[osbn, "rden"], writes=["ym"])
                P.op("act", lambda e: e.activation(junk2[:], ym[:].rearrange("p h d -> p (h d)"), AF.Square, accum_out=sst[:, 0:1]), reads=["ym"], writes=["junk2", "sst2"])
                P.op("act", lambda e: e.activation(sst[:, 1:2], sst[:, 0:1], AF.Sqrt, bias=1e-6, scale=1.0 / 384), reads=["sst2"], writes=["sst2b"])
                P.op("dve", lambda e: e.reciprocal(sst[:, 1:2], sst[:, 1:2]), reads=["sst2b"], writes=["sst2b"])
                mo, mon = mixo.next()
                P.op("dve", lambda e, mo=mo: e.scalar_tensor_tensor(out=mo[:], in0=ym[:].rearrange("p h d -> p (h d)"), scalar=sst[:, 1:2], in1=gB[:], op0=ALU.mult, op1=ALU.mult), reads=["ym", "sst2b", "gB"], writes=[mon])
                c0m = 0 if kind == "moba" else 640
                P.dma("sp", scr["mix"][qt * 128:(qt + 1) * 128, c0m:c0m + 384], mo[:], reads=[mon], writes=["s_mix"])

            tiles = [(G, j) for G in range(NG) for j in range(4)]
            for _ in pre(*tiles[0]):
                pass
            for ti, (G, j) in enumerate(tiles):
                if ti + 1 < len(tiles):
                    gen = pre(*tiles[ti + 1])
                    nst = (4 * tiles[ti + 1][0] + tiles[ti + 1][1] + 1 + 3) // 4
                    if kind == "moba":
                        for _ in gen:
                            pass
                        att(G, j)
                    else:
                        next(gen, None)
                        att(G, j, gen, nst + NIT + 1)
                        for _ in gen:
                            pass
                else:
                    att(G, j)
            P.phase_end()


        NBLK = 2 * S // 512 + 32
        x_src = x_in if l == 0 else xbuf[(l - 1) % 2]
        x_dst = xbuf[l % 2]
        P.phase_begin()
        SEL1 = P.sb("SEL1", [128, NT, 32], F32)
        SEL2 = P.sb("SEL2", [128, NT, 32], F32)
        GATES = P.sb("GATES", [128, NT, 2], F32)
        P.phase_begin()
        ub = P.sb("ub", [128, 2, S + 32], BF16)
        for hh in range(2):
            P.dma("sp", ub[:, hh, :], scr["u"][hh * 128:(hh + 1) * 128, :], reads=["s_u"], writes=["ub"])
        cw = P.sb("cw", [128, 2, 31], F32)
        P.dma("sp", cw[:], conv_wT[l], writes=["cw"])
        dgw = P.sb("dgw", [128, 2, 31, 128], BF16)
        for hh in range(2):
            for jj in range(31):
                P.op("dve" if jj % 2 else "pool", lambda e, hh=hh, jj=jj: e.tensor_scalar(dgw[:, hh, jj, :], identf[:], cw[:, hh, jj:jj + 1], None, op0=ALU.mult),
                     reads=["identf", "cw"], writes=["dgw"])
        woB = P.sb("woB", [128, KC, D], BF16)
        P.dma("pool", woB[:], w_out[l].rearrange("(kc p) n -> p kc n", p=128), writes=["woB"])
        cbB = P.sb("cbB", [128, 256], F32)
        lgB = P.sb("lgB", [128, 256], F32)
        lbB = P.sb("lbB", [128, 256], F32)
        g1B = P.sb("g1B", [128, D], F32)
        gm2B = P.sb("gm2B", [128, D], F32)
        sh2B = P.sb("sh2B", [128, D], F32)
        n2B = P.sb("n2B", [128, D], F32)
        wr = P.sb("wr", [128, KC, 36], F32)
        rbB = P.sb("rbB", [128, 36], F32)
        P.dma("sp", cbB[:], conv_bB[l], writes=["cbB"])
        P.dma("sp", lgB[:], ln_gB[l], writes=["lgB"])
        P.dma("sp", lbB[:], ln_bB[l], writes=["lbB"])
        P.dma("sp", g1B[:], d_modB[:, 2 * D:3 * D], reads=["s_modB"], writes=["g1B"])
        P.dma("sp", sh2B[:], d_modB[:, 3 * D:4 * D], reads=["s_modB"], writes=["sh2B"])
        P.dma("sp", gm2B[:], d_modB[:, 4 * D:5 * D], reads=["s_modB"], writes=["gm2B"])
        P.dma("sp", n2B[:], n2gB[l], writes=["n2B"])
        P.dma("sp", wr[:], rw[l].rearrange("(kc p) n -> p kc n", p=128), writes=["wr"])
        P.dma("sp", rbB[:], rbBin[l], writes=["rbB"])
        P.op("dve", lambda e: e.scalar_tensor_tensor(out=gm2B[:], in0=gm2B[:], scalar=1.0, in1=n2B[:], op0=ALU.add, op1=ALU.mult), reads=["n2B"], writes=["gm2B"])
        pc = Rot([(P.ps("pc", [128, 512], F32), "pc%d" % i) for i in range(2)])
        pT4 = Rot([(P.ps("pT4", [128, KC, 128], BF16), "pT4%d" % i) for i in range(1)])
        po = Rot([(P.ps("po", [128, 512], F32), "po%d" % i) for i in range(2)])
        pf = Rot([(P.ps("pf", [128, 4, 128], F32), "pf%d" % i) for i in range(2)])
        pl = P.ps("pl", [128, 512], F32)
        ycs = Rot([(P.sb("yc", [128, 256], F32), "yc%d" % i) for i in range(2)])
        st4 = P.sb("st4", [128, 16], F32)
        junk4 = P.sb("junk4", [128, D], F32)
        mixt = Rot([(P.sb("mixt", [128, D], BF16), "mixt%d" % i) for i in range(2)])
        mixT = P.sb("mixT", [128, KC, 128], BF16)
        xts = Rot([(P.sb("xt4", [128, D], F32), "xt4%d" % i) for i in range(2)])
        xms = Rot([(P.sb("xm", [128, D], F32), "xm%d" % i) for i in range(2)])
        h2fs = Rot([(P.sb("h2f", [128, D], F32), "h2f%d" % i) for i in range(2)])
        h2bs = Rot([(P.sb("h2b", [128, D], BF16), "h2b%d" % i) for i in range(2)])
        h2T = P.sb("h2T", [128, KC, 128], F32)
        lg = P.sb("lg", [128, 36], F32)
        rt = P.sb("rt", [128, 96], F32)
        def tile_gen(i):
            r0 = i * 128
            mt, mtn = mixt.next()
            xt_, xtn = xts.next()
            P.dma("sp", mt[:, 0:384], scr["mix"][r0:r0 + 128, 0:384], reads=["s_mix"], writes=[mtn])
            P.dma("sp", mt[:, 640:1024], scr["mix"][r0:r0 + 128, 640:1024], reads=["s_mix"], writes=[mtn])
            P.dma("sp", xt_[:], x_src[r0:r0 + 128, :], reads=["xsrc"], writes=[xtn])
            pct, pcn = pc.next()
            for hh in range(2):
                for jj in range(31):
                    P.op("pe", lambda e, pct=pct, hh=hh, jj=jj, r0=r0: e.matmul(pct[:, hh * 128:(hh + 1) * 128], ub[:, hh, r0 + 2 + jj:r0 + 2 + jj + 128], dgw[:, hh, jj, :], start=(jj == 0), stop=(jj == 30)),
                         reads=["ub", "dgw"], writes=[pcn])
            yc, ycn = ycs.next()
            P.op("dve", lambda e, pct=pct, yc=yc: e.tensor_tensor(out=yc[:], in0=pct[:, 0:256], in1=cbB[:], op=ALU.add), reads=["cbB"], writes=[pcn, ycn])
            yield
            P.op("dve", lambda e: e.tensor_reduce(out=st4[:, 0:1], in_=yc[:], axis=AX.X, op=ALU.add), reads=[ycn], writes=["st4a"])
            P.op("dve", lambda e: e.tensor_scalar(st4[:, 1:2], st4[:, 0:1], -1.0 / 256, None, op0=ALU.mult), reads=["st4a"], writes=["st4b"])
            P.op("dve", lambda e: e.tensor_scalar(yc[:], yc[:], st4[:, 1:2], None, op0=ALU.add), reads=["st4b"], writes=[ycn])
            P.op("act", lambda e: e.activation(junk4[:, 0:256], yc[:], AF.Square, accum_out=st4[:, 2:3]), reads=[ycn], writes=["junk4", "st4c"])
            P.op("act", lambda e: e.activation(st4[:, 3:4], st4[:, 2:3], AF.Sqrt, bias=1e-6, scale=1.0 / 256), reads=["st4c"], writes=["st4d"])
            P.op("dve", lambda e: e.reciprocal(st4[:, 3:4], st4[:, 3:4]), reads=["st4d"], writes=["st4d"])
            P.op("dve", lambda e: e.scalar_tensor_tensor(out=yc[:], in0=yc[:], scalar=st4[:, 3:4], in1=lgB[:], op0=ALU.mult, op1=ALU.mult), reads=["st4d", "lgB"], writes=[ycn])
            P.op("dve", lambda e: e.tensor_tensor(out=yc[:], in0=yc[:], in1=lbB[:], op=ALU.add), reads=["lbB"], writes=[ycn])
            P.op("act", lambda e, mt=mt: e.activation(mt[:, 384:640], yc[:], AF.Silu), reads=[ycn], writes=[mtn])
            ptt, ptn = pT4.next()
            for kc in range(KC):
                P.op("pe", lambda e, kc=kc, mt=mt, ptt=ptt: e.transpose(ptt[:, kc, :], mt[:, kc * 128:(kc + 1) * 128], identb[:]), reads=[mtn, "identb"], writes=[ptn])
            P.op("act", lambda e, ptt=ptt: e.copy(mixT[:], ptt[:]), reads=[], writes=[ptn, "mixT"])
            xm, xmn = xms.next()
            for hf in range(2):
                pot, pon = po.next()
                for kc in range(KC):
                    P.op("pe", lambda e, kc=kc, pot=pot, hf=hf: e.matmul(pot[:], mixT[:, kc, :], woB[:, kc, hf * 512:(hf + 1) * 512], start=(kc == 0), stop=(kc == KC - 1)),
                         reads=["mixT", "woB"], writes=[pon])
                P.op("dve", lambda e, pot=pot, hf=hf, xm=xm: e.tensor_tensor(out=xm[:, hf * 512:(hf + 1) * 512], in0=pot[:], in1=g1B[:, hf * 512:(hf + 1) * 512], op=ALU.mult),
                     reads=["g1B"], writes=[pon, xmn])
            P.op("pool", lambda e, xm=xm, xt_=xt_: e.tensor_tensor(out=xm[:], in0=xm[:], in1=xt_[:], op=ALU.add), reads=[xtn], writes=[xmn])
            P.dma("sp", scr["xmid"][r0:r0 + 128, :], xm[:], reads=[xmn], writes=["s_xmid"])
            P.op("act", lambda e, xm=xm: e.activation(junk4[:], xm[:], AF.Square, accum_out=st4[:, 4:5]), reads=[xmn], writes=["junk4", "st4e"])
            P.op("act", lambda e: e.activation(st4[:, 5:6], st4[:, 4:5], AF.Sqrt, bias=1e-6, scale=1.0 / D), reads=["st4e"], writes=["st4f"])
            P.op("dve", lambda e: e.reciprocal(st4[:, 5:6], st4[:, 5:6]), reads=["st4f"], writes=["st4f"])
            h2f, h2fn = h2fs.next()
            h2b, h2bn = h2bs.next()
            P.op("dve", lambda e, h2f=h2f, xm=xm: e.scalar_tensor_tensor(out=h2f[:], in0=xm[:], scalar=st4[:, 5:6], in1=gm2B[:], op0=ALU.mult, op1=ALU.mult), reads=[xmn, "st4f", "gm2B"], writes=[h2fn])
            P.op("pool", lambda e, h2f=h2f: e.tensor_tensor(out=h2f[:], in0=h2f[:], in1=sh2B[:], op=ALU.add), reads=["sh2B"], writes=[h2fn])
            P.op("act", lambda e, h2f=h2f, h2b=h2b: e.copy(h2b[:], h2f[:]), reads=[h2fn], writes=[h2bn])
            P.dma("sp", scr["h2"][r0:r0 + 128, :], h2b[:], reads=[h2bn], writes=["s_h2"])
            yield
            for q4 in range(2):
                pft, pfn = pf.next()
                for k4 in range(4):
                    kc = q4 * 4 + k4
                    P.op("pe", lambda e, pft=pft, k4=k4, kc=kc, h2f=h2f: e.transpose(pft[:, k4, :], h2f[:, kc * 128:(kc + 1) * 128], identf[:]), reads=[h2fn, "identf"], writes=[pfn])
                if q4 == 0:
                    P.op("act", lambda e, pft=pft, q4=q4: e.copy(h2T[:, q4 * 4:(q4 + 1) * 4, :], pft[:]), reads=[], writes=[pfn, "h2T"])
                else:
                    P.op("dve", lambda e, pft=pft, q4=q4: e.tensor_copy(h2T[:, q4 * 4:(q4 + 1) * 4, :], pft[:]), reads=[], writes=[pfn, "h2T"])
            for kc in range(KC):
                P.op("pe", lambda e, kc=kc: e.matmul(pl[:, 0:36], h2T[:, kc, :], wr[:, kc, :], start=(kc == 0), stop=(kc == KC - 1)), reads=["h2T", "wr"], writes=["pl"])
            P.op("dve", lambda e: e.tensor_tensor(out=lg[:], in0=pl[:, 0:36], in1=rbB[:], op=ALU.add), reads=["rbB"], writes=["pl", "lg"])
            V_ = lambda a, b: rt[:, a:b]
            P.op("dve", lambda e: e.tensor_reduce(out=V_(0, 1), in_=lg[:, 0:4], axis=AX.X, op=ALU.max), reads=["lg"], writes=["rt0"])
            P.op("dve", lambda e: e.tensor_scalar(V_(1, 2), V_(0, 1), -1.0, None, op0=ALU.mult), reads=["rt0"], writes=["rt1"])
            P.op("act", lambda e: e.activation(V_(40, 44), lg[:, 0:4], AF.Exp, bias=V_(1, 2), accum_out=V_(2, 3)), reads=["lg", "rt1"], writes=["rt40", "rt2"])
            P.op("dve", lambda e: e.reciprocal(V_(3, 4), V_(2, 3)), reads=["rt2"], writes=["rt3"])
            P.op("dve", lambda e: e.tensor_scalar(V_(4, 8), lg[:, 0:4], V_(0, 1), None, op0=ALU.is_equal), reads=["lg", "rt0"], writes=["rt4"])
            P.op("dve", lambda e: e.tensor_tensor(out=V_(48, 80).rearrange("p (g x) -> p g x", x=8), in0=lg[:, 4:36].rearrange("p (g x) -> p g x", x=8), in1=V_(4, 8)[:, :, None].to_broadcast([128, 4, 8]), op=ALU.mult),
                 reads=["lg", "rt4"], writes=["rt48"])
            P.op("dve", lambda e: e.tensor_reduce(out=V_(8, 16), in_=V_(48, 80).rearrange("p (g x) -> p x g", x=8), axis=AX.X, op=ALU.add), reads=["rt48"], writes=["rt8"])
            P.op("dve", lambda e: e.tensor_reduce(out=V_(16, 17), in_=V_(8, 16), axis=AX.X, op=ALU.max), reads=["rt8"], writes=["rt16"])
            P.op("dve", lambda e: e.tensor_scalar(V_(17, 18), V_(16, 17), -1.0, None, op0=ALU.mult), reads=["rt16"], writes=["rt17"])
            P.op("dve", lambda e: e.tensor_scalar(V_(18, 26), V_(8, 16), V_(16, 17), None, op0=ALU.is_equal), reads=["rt8", "rt16"], writes=["rt18"])
            P.op("dve", lambda e: e.scalar_tensor_tensor(out=V_(26, 34), in0=V_(18, 26), scalar=-1e30, in1=V_(8, 16), op0=ALU.mult, op1=ALU.add), reads=["rt18", "rt8"], writes=["rt26"])
            P.op("dve", lambda e: e.tensor_reduce(out=V_(34, 35), in_=V_(26, 34), axis=AX.X, op=ALU.max), reads=["rt26"], writes=["rt34"])
            P.op("dve", lambda e: e.tensor_scalar(V_(80, 88), V_(26, 34), V_(34, 35), None, op0=ALU.is_equal), reads=["rt26", "rt34"], writes=["rt80"])
            P.op("act", lambda e: e.activation(V_(35, 36), V_(34, 35), AF.Exp, bias=V_(17, 18)), reads=["rt34", "rt17"], writes=["rt35"])
            P.op("dve", lambda e: e.tensor_scalar(V_(36, 37), V_(35, 36), 1.0, None, op0=ALU.add), reads=["rt35"], writes=["rt36"])
            P.op("dve", lambda e: e.reciprocal(V_(36, 37), V_(36, 37)), reads=["rt36"], writes=["rt36"])
            P.op("dve", lambda e, i=i: e.tensor_tensor(out=GATES[:, i, 0:1], in0=V_(36, 37), in1=V_(3, 4), op=ALU.mult), reads=["rt36", "rt3"], writes=["GATES"])
            P.op("dve", lambda e, i=i: e.tensor_tensor(out=GATES[:, i, 1:2], in0=V_(3, 4), in1=GATES[:, i, 0:1], op=ALU.subtract), reads=["rt3"], writes=["GATES"])
            P.op("dve", lambda e, i=i: e.tensor_tensor(out=SEL1[:, i, :].rearrange("p (g x) -> p g x", x=8), in0=V_(4, 8)[:, :, None].to_broadcast([128, 4, 8]), in1=V_(18, 26)[:, None, :].to_broadcast([128, 4, 8]), op=ALU.mult),
                 reads=["rt4", "rt18"], writes=["SEL1"])
            P.op("dve", lambda e, i=i: e.tensor_tensor(out=SEL2[:, i, :].rearrange("p (g x) -> p g x", x=8), in0=V_(4, 8)[:, :, None].to_broadcast([128, 4, 8]), in1=V_(80, 88)[:, None, :].to_broadcast([128, 4, 8]), op=ALU.mult),
                 reads=["rt4", "rt80"], writes=["SEL2"])
        gens = [tile_gen(i) for i in range(NT)]
        next(gens[0])
        for i in range(NT):
            if i + 1 < NT:
                next(gens[i + 1])
            next(gens[i])
            next(gens[i], None)
        if "s_sel" in dbg:
            P.dma("sp", d_sel[0], SEL1[:], reads=["SEL1"])
            P.dma("sp", d_sel[1], SEL2[:], reads=["SEL2"])
            P.dma("sp", d_gates, GATES[:], reads=["GATES"])
        P.phase_end()

        P.phase_begin()
        W1I = P.sb("W1I", [128, NBLK, 8], I32)
        W2I = P.sb("W2I", [128, NBLK, 4], I32)
        DESTI = P.sb("DESTI", [128, NT, 2], I32)
        P.phase_begin()
        SELS = P.sb("SELS", [128, NT, 32], F32)
        CUM = P.sb("CUM", [128, NT + 1, 32], F32)
        P.op("dve", lambda e: e.tensor_tensor(out=SELS[:], in0=SEL1[:], in1=SEL2[:], op=ALU.add), reads=["SEL1", "SEL2"], writes=["SELS"])
        P.op("pool", lambda e: e.memset(CUM[:, 0, :], 0.0), writes=["CUM"])
        for i in range(NT):
            P.op("dve", lambda e, i=i: e.tensor_tensor(out=CUM[:, i + 1, :], in0=CUM[:, i, :], in1=SELS[:, i, :], op=ALU.add), reads=["SELS"], writes=["CUM"])
        onesf = P.sb("onesf", [128, 128], F32)
        UT = P.sb("UT", [128, 128], F32)
        P.op("pool", lambda e: e.memset(onesf[:], 1.0), writes=["onesf"])
        P.op("dve", lambda e: e.tensor_scalar(UT[:], io[:], pid[:, 0:1], None, op0=ALU.is_gt), reads=["io", "pid"], writes=["UT"])
        pq = Rot([(P.ps("pq", [128, 512], F32), "pq%d" % i) for i in range(2)])
        pqt, pqn = pq.next()
        P.op("pe", lambda e, pqt=pqt: e.matmul(pqt[:, 0:32], onesf[:], CUM[:, NT, :], start=True, stop=True), reads=["onesf", "CUM"], writes=[pqn])
        ms = P.sb("ms", [128, 512], F32)
        cntE = ms[:, 0:32]
        nblk = ms[:, 32:64]
        pendA = ms[:, 64:96]
        pendB = ms[:, 96:128]
        pst = ms[:, 128:160]
        thr = ms[:, 160:192]
        j32 = ms[:, 192:224]
        NM = S // 512 + 1
        P.op("dve", lambda e, pqt=pqt: e.tensor_copy(cntE, pqt[:, 0:32]), reads=[], writes=[pqn, "cntE"])
        P.op("dve", lambda e: e.tensor_scalar(thr[:, 0:NM], io[:, 0:NM], 512.0, None, op0=ALU.mult), reads=["io"], writes=["thr"])
        P.op("pool", lambda e: e.memset(nblk, 0.0), writes=["nblk"])
        for ex in range(32):
            P.op("dve", lambda e, ex=ex: e.tensor_scalar(j32[:, 0:NM], thr[:, 0:NM], cntE[:, ex:ex + 1], 0.0, op0=ALU.is_lt, op1=ALU.add, accum_out=nblk[:, ex:ex + 1]),
                 reads=["thr", "cntE"], writes=["j32", "nblk"])
        src, srcn, dst, dstn = nblk, "nblk", pendA, "pendA"
        for d_ in (1, 2, 4, 8, 16):
            P.op("dve", lambda e, src=src, dst=dst, d_=d_: e.tensor_copy(dst[:, 0:d_], src[:, 0:d_]), reads=[srcn], writes=[dstn])
            P.op("dve", lambda e, src=src, dst=dst, d_=d_: e.tensor_tensor(out=dst[:, d_:32], in0=src[:, d_:32], in1=src[:, 0:32 - d_], op=ALU.add), reads=[srcn], writes=[dstn])
            if dstn == "pendA":
                src, srcn, dst, dstn = pendA, "pendA", pendB, "pendB"
            else:
                src, srcn, dst, dstn = pendB, "pendB", pendA, "pendA"
        pend, pendn = src, srcn
        P.op("dve", lambda e, pend=pend: e.tensor_tensor(out=pst, in0=pend, in1=nblk, op=ALU.subtract), reads=[pendn, "nblk"], writes=["pst"])
        P.op("dve", lambda e: e.tensor_scalar(pst, pst, 512.0, None, op0=ALU.mult), reads=[], writes=["pst"])
        BE = P.sb("BE", [128, NBLK], F32)
        P.op("pool", lambda e: e.memset(BE[:], 0.0), writes=["BE"])
        for b in range(NBLK):
            P.op("dve", lambda e, b=b, pend=pend: e.tensor_scalar(j32, pend, float(b), 0.0, op0=ALU.is_le, op1=ALU.add, accum_out=BE[:, b:b + 1]), reads=[pendn], writes=["j32", "BE"])
        P.op("dve", lambda e: e.tensor_scalar(BE[:], BE[:], 31.0, None, op0=ALU.min), reads=[], writes=["BE"])
        iotaK = P.sb("iotaK", [128, 8], F32)
        P.op("dve", lambda e: e.tensor_scalar(iotaK[:], io[:, 0:8], 128.0, pid[:, 0:1], op0=ALU.mult, op1=ALU.add), reads=["io", "pid"], writes=["iotaK"])
        WF = P.sb("WF", [128, NBLK, 8], F32)
        BEs = P.sb("BEs", [128, NBLK], F32)
        SAME = P.sb("SAME", [128, NBLK], F32)
        P.op("pool", lambda e: e.memset(SAME[:], 0.0), writes=["SAME"])
        P.op("dve", lambda e: e.tensor_tensor(out=SAME[:, 1:NBLK], in0=BE[:, 1:NBLK], in1=BE[:, 0:NBLK - 1], op=ALU.is_equal), reads=["BE"], writes=["SAME"])
        P.op("dve", lambda e: e.tensor_scalar(BEs[:], BE[:], 1024.0, float(l * 32 * 1024), op0=ALU.mult, op1=ALU.add), reads=["BE"], writes=["BEs"])
        P.op("dve", lambda e: e.scalar_tensor_tensor(out=BEs[:], in0=SAME[:], scalar=200000.0, in1=BEs[:], op0=ALU.mult, op1=ALU.add), reads=["SAME"], writes=["BEs"])
        P.op("dve", lambda e: e.tensor_tensor(out=WF[:], in0=BEs[:, :, None].to_broadcast([128, NBLK, 8]), in1=iotaK[:, None, :].to_broadcast([128, NBLK, 8]), op=ALU.add), reads=["BEs", "iotaK"], writes=["WF"])
        P.op("dve", lambda e: e.tensor_copy(W1I[:], WF[:]), reads=["WF"], writes=["W1I"])
        P.op("dve", lambda e: e.tensor_scalar(BEs[:], BE[:], 512.0, float(l * 32 * 512), op0=ALU.mult, op1=ALU.add), reads=["BE", "WF"], writes=["BEs"])
        P.op("dve", lambda e: e.scalar_tensor_tensor(out=BEs[:], in0=SAME[:], scalar=200000.0, in1=BEs[:], op0=ALU.mult, op1=ALU.add), reads=["SAME"], writes=["BEs"])
        P.op("dve", lambda e: e.tensor_tensor(out=WF[:, :, 0:4], in0=BEs[:, :, None].to_broadcast([128, NBLK, 4]), in1=iotaK[:, None, 0:4].to_broadcast([128, NBLK, 4]), op=ALU.add), reads=["BEs", "iotaK", "W1I"], writes=["WF"])
        P.op("dve", lambda e: e.tensor_copy(W2I[:], WF[:, :, 0:4]), reads=["WF"], writes=["W2I"])
        DEST = P.sb("DEST", [128, NT, 2], F32)
        P.op("pool", lambda e: e.memset(DEST[:], 0.0), writes=["DEST"])
        tq = Rot([(P.sb("tq", [128, 32], F32), "tq%d" % i) for i in range(2)])
        for i in range(NT):
            pqt, pqn = pq.next()
            tqt, tqn = tq.next()
            P.op("pe", lambda e, pqt=pqt, i=i: e.matmul(pqt[:, 0:32], UT[:], SELS[:, i, :], start=True, stop=False), reads=["UT", "SELS"], writes=[pqn])
            P.op("pe", lambda e, pqt=pqt, i=i: e.matmul(pqt[:, 0:32], onesf[:], CUM[:, i, :], start=False, stop=True), reads=["onesf", "CUM"], writes=[pqn])
            P.op("dve", lambda e, pqt=pqt, tqt=tqt: e.tensor_tensor(out=tqt[:], in0=pqt[:, 0:32], in1=pst, op=ALU.add), reads=["pst"], writes=[pqn, tqn])
            for sl, SEL, seln in ((0, SEL1, "SEL1"), (1, SEL2, "SEL2")):
                P.op("dve", lambda e, tqt=tqt, i=i, sl=sl, SEL=SEL: e.scalar_tensor_tensor(out=j32, in0=tqt[:], scalar=1.0, in1=SEL[:, i, :], op0=ALU.mult, op1=ALU.mult, accum_out=DEST[:, i, sl:sl + 1]),
                     reads=[tqn, seln], writes=["j32", "DEST"])
        P.op("dve", lambda e: e.tensor_copy(DESTI[:], DEST[:]), reads=["DEST"], writes=["DESTI"])
        if "s_dest" in dbg:
            P.dma("sp", d_dest, DESTI[:], reads=["DESTI"])
            P.dma("sp", d_be, BE[:], reads=["BE"])
        zb = P.sb("zb", [128, 4, D], BF16)
        P.op("pool", lambda e: e.memset(zb[:], 0.0), writes=["zb"])
        for b in range(NBLK):
            P.dma("sp", scr["buf"][b * 512:(b + 1) * 512, :].rearrange("(s p) d -> p s d", p=128), zb[:], reads=["zb"], writes=["s_buf"])
        h2l = Rot([(P.sb("h2l", [128, D], BF16), "h2l%d" % i) for i in range(3)])
        for i in range(NT):
            ht, htn = h2l.next()
            P.dma("sp", ht[:], scr["h2"][i * 128:(i + 1) * 128, :], reads=["s_h2"], writes=[htn])
            for sl in range(2):
                P.op("pool", lambda e, ht=ht, i=i, sl=sl: e.indirect_dma_start(out=scr["buf"], out_offset=bass.IndirectOffsetOnAxis(ap=DESTI[:, i, sl:sl + 1], axis=0), in_=ht[:], in_offset=None),
                     reads=[htn, "DESTI"], writes=["s_buf"], dma=True)
        P.phase_end()
        P.phase_begin()
        w1v = w1
        w3v = w3
        w2v = w2
        w1f = Rot([(P.sb("w1f", [128, KC, 512], F32), "w1f%d" % i) for i in range(1)])
        w3f = Rot([(P.sb("w3f", [128, KC, 512], F32), "w3f%d" % i) for i in range(1)])
        w2f = Rot([(P.sb("w2f", [128, 4, D], F32), "w2f%d" % i) for i in range(1)])
        w1b = Rot([(P.sb("w1b", [128, KC, 512], BF16), "w1b%d" % i) for i in range(2)])
        w3b = Rot([(P.sb("w3b", [128, KC, 512], BF16), "w3b%d" % i) for i in range(2)])
        w2b = Rot([(P.sb("w2b", [128, 4, D], BF16), "w2b%d" % i) for i in range(2)])
        hbs = Rot([(P.sb("hb", [128, 4, D], BF16), "hb%d" % i) for i in range(2)])
        hTs = Rot([(P.sb("hT", [128, KC, 512], BF16), "hT%d" % i) for i in range(2)])
        sgs = Rot([(P.sb("sg", [128, 512], F32), "sg%d" % i) for i in range(2)])
        aTs = Rot([(P.sb("aT", [128, 4, 512], BF16), "aT%d" % i) for i in range(2)])
        ybs = Rot([(P.sb("yb", [128, 512], F32), "yb%d" % i) for i in range(4)])
        pT5 = Rot([(P.ps("pT5", [128, KC, 128], BF16), "pT5%d" % i) for i in range(2)])
        ph1 = Rot([(P.ps("ph1", [128, 512], F32), "ph1%d" % i) for i in range(1)])
        ph3 = Rot([(P.ps("ph3", [128, 512], F32), "ph3%d" % i) for i in range(1)])
        py = Rot([(P.ps("py", [128, 512], F32), "py%d" % i) for i in range(2)])
        for b in range(NBLK):
            a1, a1n = w1f.next()
            a3, a3n = w3f.next()
            a2, a2n = w2f.next()
            for kc in range(KC):
                P.op("pool", lambda e, a1=a1, b=b, kc=kc: e.indirect_dma_start(out=a1[:, kc, :], out_offset=None, in_=w1v, in_offset=bass.IndirectOffsetOnAxis(ap=W1I[:, b, kc:kc + 1], axis=0), bounds_check=_breg(e, NL * 32 * 1024 - 1), oob_is_err=False),
                     reads=["W1I"], writes=[a1n], dma=True)
                P.op("pool", lambda e, a3=a3, b=b, kc=kc: e.indirect_dma_start(out=a3[:, kc, :], out_offset=None, in_=w3v, in_offset=bass.IndirectOffsetOnAxis(ap=W1I[:, b, kc:kc + 1], axis=0), bounds_check=_breg(e, NL * 32 * 1024 - 1), oob_is_err=False),
                     reads=["W1I"], writes=[a3n], dma=True)
            for dc in range(4):
                P.op("pool", lambda e, a2=a2, b=b, dc=dc: e.indirect_dma_start(out=a2[:, dc, :], out_offset=None, in_=w2v, in_offset=bass.IndirectOffsetOnAxis(ap=W2I[:, b, dc:dc + 1], axis=0), bounds_check=_breg(e, NL * 32 * 512 - 1), oob_is_err=False),
                     reads=["W2I"], writes=[a2n], dma=True)
            hb, hbn = hbs.next()
            P.dma("sp", hb[:], scr["buf"][b * 512:(b + 1) * 512, :].rearrange("(s p) d -> p s d", p=128), reads=["s_buf"], writes=[hbn])
            b1, b1n = w1b.next()
            b3, b3n = w3b.next()
            b2, b2n = w2b.next()
            P.op("act", lambda e, a1=a1, b1=b1: e.copy(b1[:], a1[:]), reads=[a1n], writes=[b1n])
            P.op("dve", lambda e, a3=a3, b3=b3: e.tensor_copy(b3[:], a3[:]), reads=[a3n], writes=[b3n])
            P.op("dve", lambda e, a2=a2, b2=b2: e.tensor_copy(b2[:], a2[:]), reads=[a2n], writes=[b2n])
            hT, hTn = hTs.next()
            for sub in range(4):
                ptt, ptn = pT5.next()
                for kc in range(KC):
                    P.op("pe", lambda e, ptt=ptt, hb=hb, sub=sub, kc=kc: e.transpose(ptt[:, kc, :], hb[:, sub, kc * 128:(kc + 1) * 128], identb[:]), reads=[hbn, "identb"], writes=[ptn])
                if sub % 2 == 0:
                    P.op("act", lambda e, ptt=ptt, hT=hT, sub=sub: e.copy(hT[:, :, sub * 128:(sub + 1) * 128], ptt[:]), reads=[], writes=[ptn, hTn])
                else:
                    P.op("dve", lambda e, ptt=ptt, hT=hT, sub=sub: e.tensor_copy(hT[:, :, sub * 128:(sub + 1) * 128], ptt[:]), reads=[], writes=[ptn, hTn])
            aT, aTn = aTs.next()
            for dc in range(4):
                p1, p1n = ph1.next()
                p3, p3n = ph3.next()
                sg, sgn = sgs.next()
                for kc in range(KC):
                    P.op("pe", lambda e, p1=p1, b1=b1, hT=hT, kc=kc, dc=dc: e.matmul(p1[:], b1[:, kc, dc * 128:(dc + 1) * 128], hT[:, kc, :], start=(kc == 0), stop=(kc == KC - 1)), reads=[b1n, hTn], writes=[p1n])
                for kc in range(KC):
                    P.op("pe", lambda e, p3=p3, b3=b3, hT=hT, kc=kc, dc=dc: e.matmul(p3[:], b3[:, kc, dc * 128:(dc + 1) * 128], hT[:, kc, :], start=(kc == 0), stop=(kc == KC - 1)), reads=[b3n, hTn], writes=[p3n])
                P.op("act", lambda e, p1=p1, sg=sg: e.activation(sg[:], p1[:], AF.Silu), reads=[], writes=[p1n, sgn])
                P.op("dve", lambda e, p3=p3, sg=sg, aT=aT, dc=dc: e.tensor_tensor(out=aT[:, dc, :], in0=p3[:], in1=sg[:], op=ALU.mult), reads=[sgn], writes=[p3n, aTn])
            for sub in range(4):
                for hf in range(2):
                    pyt, pyn = py.next()
                    yb, ybn = ybs.next()
                    for dc in range(4):
                        P.op("pe", lambda e, pyt=pyt, aT=aT, b2=b2, dc=dc, sub=sub, hf=hf: e.matmul(pyt[:], aT[:, dc, sub * 128:(sub + 1) * 128], b2[:, dc, hf * 512:(hf + 1) * 512], start=(dc == 0), stop=(dc == 3)), reads=[aTn, b2n], writes=[pyn])
                    if hf == 0:
                        P.op("act", lambda e, pyt=pyt, yb=yb: e.copy(yb[:], pyt[:]), reads=[], writes=[pyn, ybn])
                    else:
                        P.op("dve", lambda e, pyt=pyt, yb=yb: e.tensor_copy(yb[:], pyt[:]), reads=[], writes=[pyn, ybn])
                    P.dma("sp", scr["ybuf"][b * 512 + sub * 128:b * 512 + (sub + 1) * 128, hf * 512:(hf + 1) * 512], yb[:], reads=[ybn], writes=["s_ybuf"])
        P.phase_end()
        P.phase_begin()
        g2B = P.sb("g2B", [128, D], F32)
        fgB = P.sb("fgB", [128, D], F32)
        P.dma("sp", g2B[:], d_modB[:, 5 * D:6 * D], reads=["s_modB"], writes=["g2B"])
        P.dma("sp", fgB[:], final_gB, writes=["fgB"])
        y1s = Rot([(P.sb("y1", [128, D], F32), "y1%d" % i) for i in range(2)])
        y2s = Rot([(P.sb("y2", [128, D], F32), "y2%d" % i) for i in range(2)])
        xls = Rot([(P.sb("xl", [128, D], F32), "xl%d" % i) for i in range(2)])
        xos = Rot([(P.sb("xo", [128, D], F32), "xo%d" % i) for i in range(2)])
        junk5 = P.sb("junk5", [128, D], F32)
        st5 = P.sb("st5", [128, 4], F32)
        last = (l == NL - 1)
        for i in range(NT):
            r0 = i * 128
            y1, y1n = y1s.next()
            y2, y2n = y2s.next()
            xl, xln = xls.next()
            xo, xon = xos.next()
            P.op("pool", lambda e, y1=y1, i=i: e.indirect_dma_start(out=y1[:], out_offset=None, in_=scr["ybuf"], in_offset=bass.IndirectOffsetOnAxis(ap=DESTI[:, i, 0:1], axis=0)),
                 reads=["DESTI", "s_ybuf"], writes=[y1n], dma=True)
            P.op("pool", lambda e, y2=y2, i=i: e.indirect_dma_start(out=y2[:], out_offset=None, in_=scr["ybuf"], in_offset=bass.IndirectOffsetOnAxis(ap=DESTI[:, i, 1:2], axis=0)),
                 reads=["DESTI", "s_ybuf"], writes=[y2n], dma=True)
            P.dma("sp", xl[:], scr["xmid"][r0:r0 + 128, :], reads=["s_xmid"], writes=[xln])
            P.op("dve", lambda e, y1=y1, i=i: e.tensor_scalar(y1[:], y1[:], GATES[:, i, 0:1], None, op0=ALU.mult), reads=["GATES"], writes=[y1n])
            P.op("dve", lambda e, y1=y1, y2=y2, i=i: e.scalar_tensor_tensor(out=y1[:], in0=y2[:], scalar=GATES[:, i, 1:2], in1=y1[:], op0=ALU.mult, op1=ALU.add), reads=["GATES", y2n], writes=[y1n])
            P.op("pool", lambda e, y1=y1: e.tensor_tensor(out=y1[:], in0=y1[:], in1=g2B[:], op=ALU.mult), reads=["g2B"], writes=[y1n])
            P.op("pool", lambda e, y1=y1, xl=xl, xo=xo: e.tensor_tensor(out=xo[:], in0=y1[:], in1=xl[:], op=ALU.add), reads=[y1n, xln], writes=[xon])
            if not last:
                P.dma("sp", x_dst[r0:r0 + 128, :], xo[:], reads=[xon], writes=["xdst"])
            else:
                P.op("act", lambda e, xo=xo: e.activation(junk5[:], xo[:], AF.Square, accum_out=st5[:, 0:1]), reads=[xon], writes=["junk5", "st5a"])
                P.op("act", lambda e: e.activation(st5[:, 1:2], st5[:, 0:1], AF.Sqrt, bias=1e-6, scale=1.0 / D), reads=["st5a"], writes=["st5b"])
                P.op("dve", lambda e: e.reciprocal(st5[:, 1:2], st5[:, 1:2]), reads=["st5b"], writes=["st5b"])
                P.op("dve", lambda e, xo=xo: e.scalar_tensor_tensor(out=xo[:], in0=xo[:], scalar=st5[:, 1:2], in1=fgB[:], op0=ALU.mult, op1=ALU.mult), reads=["st5b", "fgB"], writes=[xon])
                P.dma("sp", y_out[r0:r0 + 128, :], xo[:], reads=[xon], writes=["y"])
        P.phase_end()
        P.phase_end()
        P.phase_end()

    P.emit()
    return nc, P


def prep_inputs(inp, b, NL):
    f = lambda a: np.ascontiguousarray(np.asarray(a, dtype=np.float32))
    rep = lambda a: f(np.broadcast_to(np.asarray(a)[:, None, :], (a.shape[0], 128, a.shape[1])))
    d = {}
    d["x"] = f(inp["x"][b])
    d["c_col"] = f(np.asarray(inp["c"][b]).reshape(8, 128).T)
    d["ada_w"] = f(inp["ada_w"][:NL])
    d["ada_bB"] = rep(inp["ada_b"][:NL])
    d["n1g_col"] = f(np.asarray(inp["norm1_g"][:NL]).reshape(NL, 8, 128).transpose(0, 2, 1))
    d["w_in"] = f(inp["w_in"][:NL])
    d["conv_wT"] = f(np.asarray(inp["conv_w"][:NL]).transpose(0, 2, 1).reshape(NL, 2, 128, 31).transpose(0, 2, 1, 3))
    d["conv_bB"] = rep(inp["conv_b"][:NL])
    d["ln_gB"] = rep(inp["conv_ln_g"][:NL])
    d["ln_bB"] = rep(inp["conv_ln_b"][:NL])
    d["w_out"] = f(inp["w_out"][:NL])
    d["n2gB"] = rep(inp["norm2_g"][:NL])
    d["rw"] = f(np.concatenate([inp["router_group_w"][:NL], inp["router_expert_w"][:NL]], axis=-1))
    d["rbB"] = rep(np.concatenate([inp["router_group_b"][:NL], inp["router_expert_b"][:NL]], axis=-1))
    d["w1"] = f(np.asarray(inp["expert_w1"][:NL]).reshape(NL * 32 * 1024, 512))
    d["w3"] = f(np.asarray(inp["expert_w3"][:NL]).reshape(NL * 32 * 1024, 512))
    d["w2"] = f(np.asarray(inp["expert_w2"][:NL]).reshape(NL * 32 * 512, 1024))
    d["final_gB"] = f(np.broadcast_to(np.asarray(inp["final_g"])[None, :], (128, 1024)))
    d["mgB"] = rep(inp["moba_norm_g"][:NL])
    d["dgB"] = rep(inp["dsa_norm_g"][:NL])
    return d


_CACHE = {}


def kernel(**inputs):
    S, NL, NB_ = 8192, 2, 4
    if "nc" not in _CACHE:
        _CACHE["nc"] = build(S, NL)[0]
    nc = _CACHE["nc"]
    shared = prep_inputs(inputs, 0, NL)
    in_maps = []
    for b in range(NB_):
        d = dict(shared)
        d["x"] = np.ascontiguousarray(np.asarray(inputs["x"][b], dtype=np.float32))
        d["c_col"] = np.ascontiguousarray(np.asarray(inputs["c"][b], dtype=np.float32).reshape(8, 128).T)
        in_maps.append(d)
    res = run_bass_kernel_spmd(nc, in_maps, core_ids=list(range(NB_)))
    return np.stack([np.asarray(r["y"], dtype=np.float32) for r in res.results], axis=0)
```

```python
import types
import numpy as np
from contextlib import ExitStack
import concourse.bass as bass
import concourse.mybir as mybir
from concourse.bass_utils import run_bass_kernel_spmd

F32 = mybir.dt.float32
BF16 = mybir.dt.bfloat16
I32 = mybir.dt.int32
ALU = mybir.AluOpType
AF = mybir.ActivationFunctionType
AX = mybir.AxisListType

NDMASEM = 8
D = 1024
KC = 8
NEG = -30000.0
NCW = 3400 + 390 + 390
NIT = 10


def _freeze(fn):
    if fn.__closure__ is None:
        return fn
    cells = []
    for c in fn.__closure__:
        try:
            cells.append(types.CellType(c.cell_contents))
        except ValueError:
            cells.append(c)
    g = types.FunctionType(fn.__code__, fn.__globals__, fn.__name__, fn.__defaults__, tuple(cells))
    g.__kwdefaults__ = fn.__kwdefaults__
    return g


class Prog:
    ENGS = ("pe", "act", "dve", "pool", "sp")

    def __init__(self, nc):
        self.nc = nc
        self.ops = []
        self.state = {}
        self.es = ExitStack()
        self.ph = []
        self.pending = {e: None for e in self.ENGS}
        self.lastc = {e: None for e in self.ENGS}
        self.lastd = {}
        self.dcount = {e: 0 for e in self.ENGS}
        self.uid = 0

    def sb(self, name, shape, dt, glob=False):
        self.uid += 1
        st = self.es if (glob or not self.ph) else self.ph[-1]
        return st.enter_context(self.nc.sbuf_tensor("%s_%d" % (name, self.uid), list(shape), dt))

    def ps(self, name, shape, dt):
        self.uid += 1
        st = self.es if not self.ph else self.ph[-1]
        return st.enter_context(self.nc.psum_tensor("%s_%d" % (name, self.uid), list(shape), dt))

    def phase_begin(self):
        self.ph.append(ExitStack())

    def phase_end(self):
        fence = set()
        for e in self.ENGS:
            if self.lastc[e] is not None:
                fence.add(self.lastc[e])
        fence.update(self.lastd.values())
        for e in self.ENGS:
            self.pending[e] = set(fence) | (self.pending[e] or set())
        self.ph.pop().close()

    def op(self, eng, fn, reads=(), writes=(), dma=False):
        i = len(self.ops)
        deps = set()
        rawset = set()
        for r in reads:
            st = self.state.setdefault(r, [None, []])
            if st[0] is not None:
                deps.add(st[0])
                rawset.add(st[0])
        for w in writes:
            st = self.state.setdefault(w, [None, []])
            if st[0] is not None:
                deps.add(st[0])
            deps.update(st[1])
        for r in reads:
            self.state[r][1].append(i)
        for w in writes:
            st = self.state[w]
            st[0] = i
            st[1] = []
        if self.pending[eng]:
            deps |= self.pending[eng]
            rawset |= self.pending[eng]
            self.pending[eng] = None
        deps.discard(i)
        if dma:
            n = self.dcount[eng]
            self.dcount[eng] += 1
            self.lastd[(eng, n % NDMASEM)] = i
        else:
            self.lastc[eng] = i
        self.ops.append(dict(eng=eng, fn=_freeze(fn), deps=deps, raw=rawset, dma=dma, sig=dma))
        return i

    def dma(self, eng, out, in_, reads=(), writes=(), **kw):
        return self.op(eng, lambda e: e.dma_start(out=out, in_=in_, **kw), reads, writes, dma=True)

    def emit(self):
        nc = self.nc
        ops = self.ops
        for i, o in enumerate(ops):
            keep = set()
            for j in o["deps"]:
                pj = ops[j]
                if (not pj["dma"]) and (not o["dma"]) and pj["eng"] == o["eng"]:
                    if o["eng"] == "pe":
                        continue
                keep.add(j)
            o["deps"] = keep
        for o in ops:
            for j in o["deps"]:
                ops[j]["sig"] = True
        LIM = 30000
        DLIM = 1800
        cnt = {e: 0 for e in self.ENGS}
        dcnt = {e: 0 for e in self.ENGS}
        dsemcnt = {}
        dprev = {}
        for o in ops:
            e = o["eng"]
            if o["dma"]:
                n = dcnt[e]
                dcnt[e] += 1
                slot = n % NDMASEM
                k = dsemcnt.get((e, slot), 0)
                dsemcnt[(e, slot)] = k + 1
                o["prev"] = dprev.get((e, slot))
                o["semkey"] = ("d", e, slot, k // DLIM)
                o["semval"] = 16 * (k % DLIM + 1)
                dprev[(e, slot)] = (o["semkey"], o["semval"])
            elif o["sig"]:
                n = cnt[e]
                cnt[e] += 1
                o["semkey"] = ("c", e, n // LIM)
                o["semval"] = n % LIM + 1
        es = self.es
        sems = {}
        finals = {}
        for o in ops:
            if "semkey" in o:
                k = o["semkey"]
                if k not in sems:
                    sems[k] = es.enter_context(nc.semaphore("q_" + "_".join(str(x) for x in k)))
                finals[k] = max(finals.get(k, 0), o["semval"])
        self.nsems = len(sems)
        byeng = {e: [o for o in ops if o["eng"] == e] for e in self.ENGS}
        self.stats = {e: len(byeng[e]) for e in self.ENGS}

        def run(eng_name, engobj):
            waited = {}
            for o in byeng[eng_name]:
                need = {}
                for j in o["deps"]:
                    pj = ops[j]
                    k, v = pj["semkey"], pj["semval"]
                    if v > need.get(k, 0):
                        need[k] = v
                if o["dma"] and o["prev"] is not None:
                    k, v = o["prev"]
                    if v > need.get(k, 0):
                        need[k] = v
                for k, v in need.items():
                    if v > waited.get(k, 0):
                        engobj.wait_ge(sems[k], v)
                        waited[k] = v
                ins = o["fn"](engobj)
                if o["sig"]:
                    ins.then_inc(sems[o["semkey"]], 16 if o["dma"] else 1)
            if eng_name == "sp":
                for k, v in finals.items():
                    if v > waited.get(k, 0) and v > 0:
                        engobj.wait_ge(sems[k], v)

        with nc.Block() as block:
            @block.tensor
            def _(e):
                run("pe", e)

            @block.scalar
            def _(e):
                run("act", e)

            @block.vector
            def _(e):
                run("dve", e)

            @block.gpsimd
            def _(e):
                run("pool", e)

            @block.sync
            def _(e):
                run("sp", e)
        es.close()


_REGS = {}


def _breg(e, val):
    k = (id(e), val)
    if k not in _REGS:
        _REGS[k] = e.to_reg(val)
    return _REGS[k]


class Rot:
    def __init__(self, items):
        self.items = list(items)
        self.i = 0

    def next(self):
        it = self.items[self.i % len(self.items)]
        self.i += 1
        return it


FM_UNITS = [("qm", 0, 3, 128), ("km", 384, 3, 128), ("qd", 1664, 3, 128), ("kd", 2048, 3, 128),
            ("qi", 2816, 8, 64), ("ki", 3328, 1, 64)]
GLU_A = 1152
GLU_G = 1408


def build(S, NL, dbg=()):
    NT = S // 128
    NG = S // 512
    NB = S // 256
    nc = bass.Bass("TRN2", target_bir_lowering=False)

    def din(name, shape, dt=F32):
        return nc.dram_tensor(name, list(shape), dt, kind="ExternalInput").ap()

    def dscr(name, shape, dt):
        kind = "ExternalOutput" if name in dbg else "Internal"
        return nc.dram_tensor(name, list(shape), dt, kind=kind).ap()

    x_in = din("x", [S, D])
    c_col = din("c_col", [128, 8])
    ada_w = din("ada_w", [NL, D, 6 * D])
    ada_bB = din("ada_bB", [NL, 128, 6 * D])
    n1g_col = din("n1g_col", [NL, 128, 8])
    w_in = din("w_in", [NL, D, 3400])
    conv_wT = din("conv_wT", [NL, 128, 2, 31])
    conv_bB = din("conv_bB", [NL, 128, 256])
    ln_gB = din("ln_gB", [NL, 128, 256])
    ln_bB = din("ln_bB", [NL, 128, 256])
    w_out = din("w_out", [NL, D, D])
    n2gB = din("n2gB", [NL, 128, D])
    rw = din("rw", [NL, D, 36])
    rbBin = din("rbB", [NL, 128, 36])
    w1 = din("w1", [NL * 32 * D, 512])
    w3 = din("w3", [NL * 32 * D, 512])
    w2 = din("w2", [NL * 32 * 512, D])
    final_gB = din("final_gB", [128, D])
    mgB = din("mgB", [NL, 128, 384])
    dgB = din("dgB", [NL, 128, 384])
    y_out = nc.dram_tensor("y", [S, D], F32, kind="ExternalOutput").ap()

    scr = {}
    for nm, rows, M in [("qm", 384, 128), ("km", 384, 128), ("qd", 384, 128), ("kd", 384, 128), ("qi", 512, 64), ("ki", 64, 64)]:
        scr[nm] = dscr("s_" + nm, [rows, S], BF16)
    scr["u"] = dscr("s_u", [256, S + 32], BF16)
    scr["vm"] = dscr("s_vm", [S, 390], BF16)
    scr["vd"] = dscr("s_vd", [S, 390], BF16)
    scr["wi"] = dscr("s_wi", [S, 8], F32)
    scr["mix"] = dscr("s_mix", [S, D], BF16)
    NBLK_ = 2 * S // 512 + 32
    scr["xmid"] = dscr("s_xmid", [S, D], F32)
    scr["h2"] = dscr("s_h2", [S, D], BF16)
    scr["buf"] = dscr("s_buf", [NBLK_ * 512, D], BF16)
    scr["ybuf"] = dscr("s_ybuf", [NBLK_ * 512, D], F32)
    xbuf = [dscr("s_xbuf%d" % i, [S, D], F32) for i in range(2)]
    d_sel = [dscr("s_sel%d" % i, [128, NT, 32], F32) for i in range(2)]
    d_gates = dscr("s_gates", [128, NT, 2], F32)
    d_dest = dscr("s_dest", [128, NT, 2], I32)
    d_be = dscr("s_be", [128, NBLK_], F32)
    d_modB = dscr("s_modB", [128, 6 * D], F32)

    P = Prog(nc)
    identf = P.sb("identf", [128, 128], F32, glob=True)
    identb = P.sb("identb", [128, 128], BF16, glob=True)
    io = P.sb("io", [128, 128], F32, glob=True)
    pid = P.sb("pid", [128, 1], F32, glob=True)
    P.op("pool", lambda e: e.iota(io[:], pattern=[[1, 128]], base=0, channel_multiplier=0, allow_small_or_imprecise_dtypes=True), writes=["io"])
    P.op("pool", lambda e: e.iota(pid[:], pattern=[[0, 1]], base=0, channel_multiplier=1, allow_small_or_imprecise_dtypes=True), writes=["pid"])
    P.op("dve", lambda e: e.tensor_scalar(identf[:], io[:], pid[:, 0:1], None, op0=ALU.is_equal), reads=["io", "pid"], writes=["identf"])
    P.op("dve", lambda e: e.tensor_copy(identb[:], identf[:]), reads=["identf"], writes=["identb"])

    for l in range(NL):
        P.phase_begin()
        modB = P.sb("modB", [128, 6 * D], F32)
        P.phase_begin()
        cc = P.sb("cc", [128, 8], F32)
        sc = P.sb("sc", [128, 8], F32)
        screp = P.sb("screp", [128, 8, 128], F32)
        abB = P.sb("abB", [128, 6 * D], F32)
        P.dma("sp", cc[:], c_col, writes=["cc"])
        P.dma("sp", abB[:], ada_bB[l], writes=["abB"])
        P.op("act", lambda e: e.activation(sc[:], cc[:], AF.Silu), reads=["cc"], writes=["sc"])
        for kc in range(KC):
            P.op("dve", lambda e, kc=kc: e.tensor_scalar(screp[:, kc, :], io[:], 0.0, sc[:, kc:kc + 1], op0=ALU.mult, op1=ALU.add),
                 reads=["io", "sc"], writes=["screp"])
        awt = Rot([(P.sb("awt", [128, 8, 512], F32), "awt%d" % i) for i in range(2)])
        pm = Rot([(P.ps("pm", [128, 512], F32), "pm%d" % i) for i in range(2)])
        for fc in range(12):
            wt, wn = awt.next()
            pt, pn = pm.next()
            P.dma("sp", wt[:], ada_w[l, :, fc * 512:(fc + 1) * 512].rearrange("(kc p) n -> p kc n", p=128), writes=[wn])
            for kc in range(KC):
                P.op("pe", lambda e, kc=kc, wt=wt, pt=pt: e.matmul(pt[:], screp[:, kc, :], wt[:, kc, :], start=(kc == 0), stop=(kc == KC - 1)),
                     reads=["screp", wn], writes=[pn])
            P.op("dve", lambda e, pt=pt, fc=fc: e.tensor_tensor(out=modB[:, fc * 512:(fc + 1) * 512], in0=pt[:], in1=abB[:, fc * 512:(fc + 1) * 512], op=ALU.add),
                 reads=["abB"], writes=[pn, "modB"])
        P.dma("sp", d_modB, modB[:], reads=["modB"], writes=["s_modB"])
        P.phase_end()

        P.phase_begin()
        Wp = P.sb("Wp", [128, KC, NCW], BF16)
        sh1rep = P.sb("sh1rep", [128, KC, 128], F32)
        gmodT = P.sb("gmodT", [128, KC], F32)
        n1c = P.sb("n1c", [128, KC], F32)
        bB = P.sb("bB", [128, 3400], F32)
        bBv = P.sb("bBv", [128, 2, 6, 65], F32)
        biasT = P.sb("biasT", [128, 32], F32)
        P.dma("sp", n1c[:], n1g_col[l], writes=["n1c"])
        P.phase_begin()
        ptr = Rot([(P.ps("ptr", [128, 512], F32), "ptr%d" % i) for i in range(2)])
        for kc in range(KC):
            pt, pn = ptr.next()
            P.op("pe", lambda e, pt=pt, kc=kc: e.transpose(pt[:, 0:128], modB[:, kc * 128:(kc + 1) * 128], identf[:]), reads=["modB", "identf"], writes=[pn])
            P.op("act", lambda e, pt=pt, kc=kc: e.copy(sh1rep[:, kc, :], pt[:, 0:128]), reads=[], writes=[pn, "sh1rep"])
            pt, pn = ptr.next()
            P.op("pe", lambda e, pt=pt, kc=kc: e.transpose(pt[:, 0:128], modB[:, D + kc * 128:D + (kc + 1) * 128], identf[:]), reads=["modB", "identf"], writes=[pn])
            P.op("dve", lambda e, pt=pt, kc=kc: e.scalar_tensor_tensor(out=gmodT[:, kc:kc + 1], in0=pt[:, 0:1], scalar=1.0, in1=n1c[:, kc:kc + 1], op0=ALU.add, op1=ALU.mult),
                 reads=["n1c"], writes=[pn, "gmodT"])
        P.op("pool", lambda e: e.memset(Wp[:, :, 3400:NCW], 0.0), writes=["Wp"])
        wst = Rot([(P.sb("wst", [128, 3400], F32), "wst%d" % i) for i in range(2)])
        for kc in range(KC):
            wt, wn = wst.next()
            P.dma("sp", wt[:], w_in[l, kc * 128:(kc + 1) * 128, :], writes=[wn])
            P.op("dve", lambda e, wt=wt, kc=kc: e.tensor_scalar(Wp[:, kc, 0:3400], wt[:], gmodT[:, kc:kc + 1], None, op0=ALU.mult),
                 reads=[wn, "gmodT"], writes=["Wp"])
            for vi, c0 in ((0, 768), (1, 2432)):
                P.op("pool", lambda e, wt=wt, kc=kc, vi=vi, c0=c0: e.tensor_scalar(
                    Wp[:, kc, 3400 + vi * 390:3400 + (vi + 1) * 390].rearrange("p (h d) -> p h d", d=65)[:, :, 0:64],
                    wt[:, c0:c0 + 384].rearrange("p (h d) -> p h d", d=64), gmodT[:, kc:kc + 1], None, op0=ALU.mult),
                    reads=[wn, "gmodT"], writes=["Wp"])
        wbt = Rot([(P.sb("wbt", [128, KC, 512], F32), "wbt%d" % i) for i in range(2)])
        pbb = Rot([(P.ps("pbb", [128, 512], F32), "pbb%d" % i) for i in range(2)])
        for cc_ in range(7):
            c0 = cc_ * 512
            n = min(512, 3400 - c0)
            wt, wn = wbt.next()
            pt, pn = pbb.next()
            P.dma("sp", wt[:, :, 0:n], w_in[l, :, c0:c0 + n].rearrange("(kc p) n -> p kc n", p=128), writes=[wn])
            for kc in range(KC):
                P.op("pe", lambda e, kc=kc, wt=wt, pt=pt, n=n: e.matmul(pt[:, 0:n], sh1rep[:, kc, :], wt[:, kc, 0:n], start=(kc == 0), stop=(kc == KC - 1)),
                     reads=["sh1rep", wn], writes=[pn])
            P.op("act", lambda e, pt=pt, c0=c0, n=n: e.copy(bB[:, c0:c0 + n], pt[:, 0:n]), reads=[], writes=[pn, "bB"])
        P.op("pool", lambda e: e.memset(bBv[:], 1.0), writes=["bBv"])
        for vi, c0 in ((0, 768), (1, 2432)):
            P.op("dve", lambda e, vi=vi, c0=c0: e.tensor_copy(bBv[:, vi, :, 0:64], bB[:, c0:c0 + 384].rearrange("p (h d) -> p h d", d=64)),
                 reads=["bB"], writes=["bBv"])
        ucols = []
        for nm, c0, nu, M in FM_UNITS:
            for u in range(nu):
                ucols.append((nm, u, c0 + u * M, M))
        for u in range(2):
            ucols.append(("ca", u, GLU_A + u * 128, 128))
        for u in range(2):
            ucols.append(("cg", u, GLU_G + u * 128, 128))
        bidx = {}
        for k, (nm, u, c0, M) in enumerate(ucols):
            bidx[(nm, u)] = k
            pt, pn = ptr.next()
            P.op("pe", lambda e, pt=pt, c0=c0, M=M: e.transpose(pt[0:M, 0:128], bB[:, c0:c0 + M], identf[:]), reads=["bB", "identf"], writes=[pn])
            P.op("act", lambda e, pt=pt, k=k, M=M: e.copy(biasT[0:M, k:k + 1], pt[0:M, 0:1]), reads=[], writes=[pn, "biasT"])

        P.phase_end()
        xg = Rot([(P.sb("xg", [128, 4, D], F32), "xg%d" % i) for i in range(2)])
        xb = Rot([(P.sb("xb", [128, D], BF16), "xb%d" % i) for i in range(2)])
        xT = Rot([(P.sb("xT", [128, KC, 512], BF16), "xT%d" % i) for i in range(2)])
        junk = P.sb("junk", [128, D], F32)
        ss = Rot([(P.sb("ss", [128, 4], F32), "ss%d" % i) for i in range(2)])
        rs = Rot([(P.sb("rs", [128, 4], F32), "rs%d" % i) for i in range(2)])
        pT = Rot([(P.ps("pT", [128, KC, 128], BF16), "pT%d" % i) for i in range(2)])
        pu = Rot([(P.ps("pu", [128, 512], F32), "pu%d" % i) for i in range(4)])
        pv = Rot([(P.ps("pv", [128, 512], F32), "pv%d" % i) for i in range(2)])
        stg = Rot([(P.sb("stg", [128, 512], BF16), "stg%d" % i) for i in range(4)])
        sig = Rot([(P.sb("sig", [128, 512], F32), "sig%d" % i) for i in range(2)])
        stv = Rot([(P.sb("stv", [128, 390], BF16), "stv%d" % i) for i in range(3)])
        stw = Rot([(P.sb("stw", [128, 8], F32), "stw%d" % i) for i in range(2)])
        zt = P.sb("zt", [128, 32], BF16)
        P.op("pool", lambda e: e.memset(zt[:], 0.0), writes=["zt"])
        for h in range(2):
            P.dma("sp", scr["u"][h * 128:(h + 1) * 128, 0:32], zt[:], reads=["zt"], writes=["s_u"])

        def load_x(G):
            t, n = xg.next()
            P.dma("sp", t[:], (x_in if l == 0 else xbuf[(l - 1) % 2])[G * 512:(G + 1) * 512, :].rearrange("(j p) d -> p j d", p=128), reads=["xsrc"], writes=[n])
            return t, n

        nxt = load_x(0)
        for G in range(NG):
            xt_, xn = nxt
            if G + 1 < NG:
                nxt = load_x(G + 1)
            sst, ssn = ss.next()
            rst, rsn = rs.next()
            xTt, xTn = xT.next()
            for j in range(4):
                P.op("act", lambda e, j=j, xt_=xt_, sst=sst: e.activation(junk[:], xt_[:, j, :], AF.Square, accum_out=sst[:, j:j + 1]),
                     reads=[xn], writes=["junk", ssn])
            P.op("act", lambda e, sst=sst, rst=rst: e.activation(rst[:], sst[:], AF.Sqrt, bias=1e-6, scale=1.0 / D), reads=[ssn], writes=[rsn])
            P.op("dve", lambda e, rst=rst: e.reciprocal(rst[:], rst[:]), reads=[rsn], writes=[rsn])
            for j in range(4):
                xbt, xbn = xb.next()
                ptt, ptn = pT.next()
                P.op("dve", lambda e, j=j, xt_=xt_, xbt=xbt, rst=rst: e.tensor_scalar(xbt[:], xt_[:, j, :], rst[:, j:j + 1], None, op0=ALU.mult),
                     reads=[xn, rsn], writes=[xbn])
                for kc in range(KC):
                    P.op("pe", lambda e, kc=kc, xbt=xbt, ptt=ptt: e.transpose(ptt[:, kc, :], xbt[:, kc * 128:(kc + 1) * 128], identb[:]),
                         reads=[xbn, "identb"], writes=[ptn])
                P.op("act", lambda e, j=j, xTt=xTt, ptt=ptt: e.copy(xTt[:, :, j * 128:(j + 1) * 128], ptt[:]), reads=[], writes=[ptn, xTn])
            for nm, c0, nu, M in FM_UNITS:
                for u in range(nu):
                    put, pun = pu.next()
                    sgt, sgn = stg.next()
                    cs = c0 + u * M
                    for kc in range(KC):
                        P.op("pe", lambda e, kc=kc, put=put, cs=cs, M=M, xTt=xTt: e.matmul(put[0:M, :], Wp[:, kc, cs:cs + M], xTt[:, kc, :], start=(kc == 0), stop=(kc == KC - 1)),
                             reads=["Wp", xTn], writes=[pun])
                    k = bidx[(nm, u)]
                    P.op("act", lambda e, put=put, sgt=sgt, M=M, k=k: e.activation(sgt[0:M, :], put[0:M, :], AF.Identity, bias=biasT[0:M, k:k + 1]),
                         reads=["biasT"], writes=[pun, sgn])
                    P.dma("sp", scr[nm][u * M:(u + 1) * M, G * 512:(G + 1) * 512], sgt[0:M, :], reads=[sgn], writes=["s_" + nm])
            for u in range(2):
                pa, pan = pu.next()
                pg, pgn = pu.next()
                sgt, sgn = stg.next()
                sit, sin_ = sig.next()
                for kc in range(KC):
                    P.op("pe", lambda e, kc=kc, pa=pa, u=u, xTt=xTt: e.matmul(pa[:], Wp[:, kc, GLU_A + u * 128:GLU_A + (u + 1) * 128], xTt[:, kc, :], start=(kc == 0), stop=(kc == KC - 1)),
                         reads=["Wp", xTn], writes=[pan])
                for kc in range(KC):
                    P.op("pe", lambda e, kc=kc, pg=pg, u=u, xTt=xTt: e.matmul(pg[:], Wp[:, kc, GLU_G + u * 128:GLU_G + (u + 1) * 128], xTt[:, kc, :], start=(kc == 0), stop=(kc == KC - 1)),
                         reads=["Wp", xTn], writes=[pgn])
                kg = bidx[("cg", u)]
                ka = bidx[("ca", u)]
                P.op("act", lambda e, pg=pg, sit=sit, kg=kg: e.activation(sit[:], pg[:], AF.Sigmoid, bias=biasT[:, kg:kg + 1]),
                     reads=["biasT"], writes=[pgn, sin_])
                P.op("dve", lambda e, pa=pa, sit=sit, sgt=sgt, ka=ka: e.scalar_tensor_tensor(out=sgt[:], in0=pa[:], scalar=biasT[:, ka:ka + 1], in1=sit[:], op0=ALU.add, op1=ALU.mult),
                     reads=["biasT", sin_], writes=[pan, sgn])
                P.dma("sp", scr["u"][u * 128:(u + 1) * 128, 32 + G * 512:32 + (G + 1) * 512], sgt[:], reads=[sgn], writes=["s_u"])
            for j in range(4):
                r0 = G * 512 + j * 128
                for vi, nm in ((0, "vm"), (1, "vd")):
                    pvt, pvn = pv.next()
                    svt, svn = stv.next()
                    for kc in range(KC):
                        P.op("pe", lambda e, kc=kc, pvt=pvt, vi=vi, j=j, xTt=xTt: e.matmul(pvt[:, 0:390], xTt[:, kc, j * 128:(j + 1) * 128], Wp[:, kc, 3400 + vi * 390:3400 + (vi + 1) * 390], start=(kc == 0), stop=(kc == KC - 1)),
                             reads=["Wp", xTn], writes=[pvn])
                    P.op("dve", lambda e, pvt=pvt, svt=svt, vi=vi: e.tensor_tensor(out=svt[:], in0=pvt[:, 0:390], in1=bBv[:, vi].rearrange("p h d -> p (h d)"), op=ALU.add),
                         reads=["bBv"], writes=[pvn, svn])
                    P.dma("sp", scr[nm][r0:r0 + 128, :], svt[:], reads=[svn], writes=["s_" + nm])
                pvt, pvn = pv.next()
                swt, swn = stw.next()
                for kc in range(KC):
                    P.op("pe", lambda e, kc=kc, pvt=pvt, j=j, xTt=xTt: e.matmul(pvt[:, 0:8], xTt[:, kc, j * 128:(j + 1) * 128], Wp[:, kc, 3392:3400], start=(kc == 0), stop=(kc == KC - 1)),
                         reads=["Wp", xTn], writes=[pvn])
                P.op("dve", lambda e, pvt=pvt, swt=swt: e.tensor_tensor(out=swt[:], in0=pvt[:, 0:8], in1=bB[:, 3392:3400], op=ALU.add),
                     reads=["bB"], writes=[pvn, swn])
                P.dma("sp", scr["wi"][r0:r0 + 128, :], swt[:], reads=[swn], writes=["s_wi"])
        P.phase_end()
        P.phase_end()


        for kind in ("moba", "dsa"):
            P.phase_begin()
            qn, kn, vn = ("qm", "km", "vm") if kind == "moba" else ("qd", "kd", "vd")
            kT = P.sb("kT", [128, 3, S], BF16)
            V = P.sb("V", [128, NT, 390], BF16)
            gB = P.sb("gB", [128, 384], F32)
            for p in range(3):
                P.dma("sp", kT[:, p, :], scr[kn][p * 128:(p + 1) * 128, :], reads=["s_" + kn], writes=["kT"])
            for c8 in range(0, NT, 8):
                P.dma("sp", V[:, c8:c8 + 8, :], scr[vn][c8 * 128:(c8 + 8) * 128, :].rearrange("(c p) n -> p c n", p=128), reads=["s_" + vn], writes=["V"])
            P.dma("sp", gB[:], (mgB if kind == "moba" else dgB)[l], writes=["gB"])
            tri = P.sb("tri", [128, 128], BF16)
            P.op("dve", lambda e: e.tensor_scalar(tri[:], io[:], pid[:, 0:1], NEG, op0=ALU.is_gt, op1=ALU.mult), reads=["io", "pid"], writes=["tri"])
            nbuf = 2 if kind == "moba" else 1
            QW = 512 if kind == "moba" else 128
            qpads = [P.sb("qpad", [128, 6, QW], BF16) for _ in range(2)]
            for i in range(2):
                P.op("pool", lambda e, i=i: e.memset(qpads[i][:], 0.0), writes=["qpad%d" % i])
            qgs = Rot([(P.sb("qg", [128, 3, QW], BF16), "qg%d" % i) for i in range(2)])
            junk2 = P.sb("junk2", [128, 384], BF16)
            Sps = Rot([(P.ps("Sps", [128, 4, 128], F32), "Sps%d" % i) for i in range(3 if kind == "moba" else 2)])
            Ops = Rot([(P.ps("Ops", [128, 512], F32), "Ops%d" % i) for i in range(2 if kind == "moba" else 1)])
            if kind == "dsa":
                Xps = Rot([(P.ps("Xps", [128, 512], F32), "Xps%d" % i) for i in range(4)])
            else:
                pmisc = P.ps("pmisc", [128, 1024], BF16)
                pgate = P.ps("pgate", [128, 16, 32], F32)
            PTs = Rot([(P.sb("PT", [128, 4, 128], BF16), "PT%d" % i) for i in range(3 if kind == "moba" else 2)])
            osbs = Rot([(P.sb("osb", [128, 6, 65], F32), "osb%d" % i) for i in range(2 if kind == "moba" else 1)])
            ym = P.sb("ym", [128, 6, 64], F32)
            rden = P.sb("rden", [128, 6], F32)
            sst = P.sb("sst2", [128, 2], F32)
            mixo = Rot([(P.sb("mixo", [128, 384], BF16), "mixo%d" % i) for i in range(2 if kind == "moba" else 1)])
            if kind == "moba":
                ksum = P.sb("ksum", [128, 3, NB], F32)
                kmeanb = P.sb("kmeanb", [128, 3, 32], BF16)
                Eall = P.sb("Eall", [128, 32, 128], BF16)
                gate_sb = P.sb("gate_sb", [128, 6, 32], F32)
                mx = P.sb("mx", [128, 6, 8], F32)
                selb = P.sb("selb", [128, 6, 32], BF16)
                sbTs = Rot([(P.sb("sbT", [128, 6, 128], BF16), "sbT%d" % i) for i in range(2)])
                for (t_, n_) in sbTs.items:
                    P.op("pool", lambda e, t_=t_: e.memset(t_[:], 0.0), writes=[n_])
                for p in range(3):
                    P.op("dve", lambda e, p=p: e.tensor_reduce(out=ksum[:, p, :], in_=kT[:, p, :].rearrange("r (n k) -> r n k", k=256), axis=AX.X, op=ALU.add),
                         reads=["kT"], writes=["ksum"])
                P.op("pool", lambda e: e.memset(kmeanb[:], 0.0), writes=["kmeanb"])
                P.op("dve", lambda e: e.tensor_scalar(kmeanb[:, :, 0:NB], ksum[:], 1.0 / 256, None, op0=ALU.mult), reads=["ksum"], writes=["kmeanb"])
                P.op("pool", lambda e: e.memset(Eall[:], 0.0), writes=["Eall"])
                P.op("dve", lambda e: e.tensor_copy(Eall[0:32], identf[0:32, 0:32, None].to_broadcast([32, 32, 128])), reads=["identf"], writes=["Eall"])
                P.op("pool", lambda e: e.memset(gate_sb[:], -1e30), writes=["gate_sb"])
            else:
                kiT = P.sb("kiT", [128, S], BF16)
                P.op("pool", lambda e: e.memset(kiT[64:128, :], 0.0), writes=["kiT"])
                P.dma("sp", kiT[0:64, :], scr["ki"], reads=["s_ki"], writes=["kiT"])
                qis = Rot([(P.sb("qi", [128, 8, 128], BF16), "qi%d" % i) for i in range(2)])
                for (t_, n_) in qis.items:
                    P.op("pool", lambda e, t_=t_: e.memset(t_[:], 0.0), writes=[n_])
                wis = Rot([(P.sb("wi", [128, 4, 8], F32), "wi%d" % i) for i in range(2)])
                acc = P.sb("acc", [128, S], F32)
                dbs = Rot([(P.sb("dbias", [128, S], BF16), "dbias%d" % i) for i in range(2)])
                rls = Rot([(P.sb("rl", [128, 512], BF16), "rl%d" % i) for i in range(8)])
                dWs = Rot([(P.sb("dW", [128, 8, 128], BF16), "dW%d" % i) for i in range(1)])
                accp = P.ps("accp", [128, 512], F32)
                cntA = Rot([(P.sb("cntA", [128, 1], F32), "cntA%d" % i) for i in range(2)])
                tmpc = Rot([(P.sb("tmpc", [128, 1], F32), "tmpc%d" % i) for i in range(2)])
                triD = P.sb("triD", [128, 128], F32)
                P.op("dve", lambda e: e.tensor_scalar(triD[:], io[:], pid[:, 0:1], -1e30, op0=ALU.is_gt, op1=ALU.mult), reads=["io", "pid"], writes=["triD"])
                pw2 = P.sb("pw2", [128, NIT + 1], F32)
                for k in range(NIT + 1):
                    P.op("pool", lambda e, k=k: e.memset(pw2[:, k:k + 1], 2.0 ** -(k + 1)), writes=["pw2"])
                Wk = P.sb("Wk", [128, NIT + 1], F32)
                bs = P.sb("bs", [128, 8], F32)
                los = Rot([(P.sb("lo", [128, 1], F32), "lo%d" % i) for i in range(2)])
                mids = Rot([(P.sb("mid", [128, 1], F32), "mid%d" % i) for i in range(2)])
                cnts = Rot([(P.sb("cnt", [128, 1], F32), "cnt%d" % i) for i in range(2)])
                tts = Rot([(P.sb("tt", [128, 1], F32), "tt%d" % i) for i in range(2)])

            def load_q(G, idx):
                if not idx:
                    t, n = qgs.next()
                    P.dma("sp", t[:], scr[qn][:, G * QW:(G + 1) * QW].rearrange("(p r) t -> r p t", r=128), reads=["s_" + qn], writes=[n])
                    return [(t, n)]
                t2, n2 = None, None
                t3, n3 = wis.next()
                P.dma("sp", t3[:], scr["wi"][G * 512:(G + 1) * 512, :].rearrange("(j p) h -> p j h", p=128), reads=["s_wi"], writes=[n3])
                return [(t2, n2), (t3, n3)]

            grp = {}

            def load_att(G):
                cur = load_q(G, False)
                qg, qgn = cur[0]
                qpad = qpads[G % 2]
                qpn = "qpad%d" % (G % 2)
                P.op("act", lambda e, qpad=qpad, qg=qg: e.copy(qpad[0:64].rearrange("r (p two) t -> r p two t", two=2)[:, :, 0, :], qg[0:64, :, :]), reads=[qgn], writes=[qpn])
                P.op("dve", lambda e, qpad=qpad, qg=qg: e.tensor_copy(qpad[64:128].rearrange("r (p two) t -> r p two t", two=2)[:, :, 1, :], qg[64:128, :, :]), reads=[qgn], writes=[qpn])
                grp[G] = (qpad, qpn)

            tst = {}

            def pre(G, j):
                qt = 4 * G + j
                nch = qt + 1
                if kind == "moba":
                    if G not in grp:
                        load_att(G)
                    qpad, qpn = grp[G]
                else:
                    if j == 0:
                        cur = load_q(G, True)
                        tst["qi"] = cur
                    qi, qin = qis.next()
                    P.dma("sp", qi[0:64], scr["qi"][:, qt * 128:(qt + 1) * 128].rearrange("(h d) t -> d h t", d=64), reads=["s_qi"], writes=[qin])
                    wi, win = tst["qi"][1]
                b = qt // 2
                sbT = sbTn = dbias = dbn = None
                qt = 4 * G + j
                nch = qt + 1
                if kind == "moba":
                    b = qt // 2
                    sbT, sbTn = sbTs.next()
                    if b > 0:
                        for h in range(6):
                            P.op("pe", lambda e, h=h, j=j, qpad=qpad: e.matmul(pgate[:, h, :], qpad[:, h, j * 128:(j + 1) * 128], kmeanb[:, h // 2, :], start=True, stop=True),
                                 reads=[qpn, "kmeanb"], writes=["Xg"])
                        P.op("dve", lambda e, b=b: e.tensor_copy(gate_sb[:, :, 0:b], pgate[:, 0:6, 0:b]), reads=[], writes=["Xg", "gate_sb"])
                        for h in range(6):
                            P.op("dve", lambda e, h=h: e.max(out=mx[:, h, :], in_=gate_sb[:, h, :]), reads=["gate_sb"], writes=["mx"])
                        for h in range(6):
                            P.op("dve", lambda e, h=h: e.tensor_scalar(selb[:, h, :], gate_sb[:, h, :], mx[:, h, 2:3], NEG, op0=ALU.is_lt, op1=ALU.mult),
                                 reads=["gate_sb", "mx"], writes=["selb"])
                        for h in range(6):
                            P.op("pe", lambda e, h=h: e.transpose(pmisc[0:32, h * 128:(h + 1) * 128], selb[:, h, :], identb[:]), reads=["selb", "identb"], writes=["pmisc"])
                        P.op("act", lambda e, sbT=sbT: e.copy(sbT[0:32], pmisc[0:32, 0:768].rearrange("n (h t) -> n h t", t=128)), reads=[], writes=["pmisc", sbTn])
                else:
                    L = nch * 128
                    dbias, dbn = dbs.next()
                    dW, dWn = dWs.next()
                    for h in range(8):
                        P.op("dve", lambda e, dW=dW, wi=wi, h=h, j=j: e.tensor_scalar(dW[:, h, :], identf[:], wi[:, j, h:h + 1], None, op0=ALU.mult),
                             reads=["identf", win], writes=[dWn])
                    for cc in range((L + 511) // 512):
                        n = min(512, L - cc * 512)
                        rr = []
                        for h in range(8):
                            xp, xpn = Xps.next()
                            rl, rln = rls.next()
                            rr.append((rl, rln))
                            P.op("pe", lambda e, xp=xp, h=h, cc=cc, n=n, qi=qi, j=j: e.matmul(xp[:, 0:n], qi[:, h, :], kiT[:, cc * 512:cc * 512 + n], start=True, stop=True),
                                 reads=[qin, "kiT"], writes=[xpn])
                            if h % 3 != 2:
                                P.op("act", lambda e, xp=xp, rl=rl, n=n: e.activation(rl[:, 0:n], xp[:, 0:n], AF.Relu), reads=[], writes=[xpn, rln])
                            else:
                                P.op("dve", lambda e, xp=xp, rl=rl, n=n: e.tensor_scalar(rl[:, 0:n], xp[:, 0:n], 0.0, None, op0=ALU.max), reads=[], writes=[xpn, rln])
                        for h in range(8):
                            rl, rln = rr[h]
                            P.op("pe", lambda e, rl=rl, dW=dW, h=h, n=n: e.matmul(accp[:, 0:n], dW[:, h, :], rl[:, 0:n], start=(h == 0), stop=(h == 7)),
                                 reads=[rln, dWn], writes=["accp"])
                        if cc % 2 == 0:
                            P.op("act", lambda e, cc=cc, n=n: e.copy(acc[:, cc * 512:cc * 512 + n], accp[:, 0:n]), reads=[], writes=["accp", "acc"])
                        else:
                            P.op("dve", lambda e, cc=cc, n=n: e.tensor_copy(acc[:, cc * 512:cc * 512 + n], accp[:, 0:n]), reads=[], writes=["accp", "acc"])
                        yield
                    P.op("dve", lambda e, L=L: e.tensor_reduce(out=bs[:, 0:1], in_=acc[:, 0:L], axis=AX.X, op=ALU.max, apply_absolute_value=True), reads=["acc"], writes=["bs"])
                    P.op("dve", lambda e: e.tensor_scalar(bs[:, 1:2], bs[:, 0:1], -1.0, None, op0=ALU.mult), reads=["bs"], writes=["bs"])
                    P.op("dve", lambda e, L=L: e.tensor_tensor(out=acc[:, L - 128:L], in0=acc[:, L - 128:L], in1=triD[:], op=ALU.add), reads=["triD"], writes=["acc"])
                    lo, lon = los.next()
                    P.op("dve", lambda e, lo=lo: e.tensor_scalar(lo[:], bs[:, 1:2], -1.0, None, op0=ALU.add), reads=["bs"], writes=[lon])
                    P.op("dve", lambda e: e.scalar_tensor_tensor(out=bs[:, 2:3], in0=bs[:, 0:1], scalar=2.0, in1=bs[:, 1:2], op0=ALU.add, op1=ALU.subtract), reads=["bs"], writes=["bs"])
                    P.op("dve", lambda e: e.tensor_scalar(Wk[:], pw2[:], bs[:, 2:3], None, op0=ALU.mult), reads=["pw2", "bs"], writes=["Wk"])
                    yield
                    for k in range(NIT):
                        mid, midn = mids.next()
                        cnt, cntn = cnts.next()
                        tt, ttn = tts.next()
                        lo2, lo2n = los.next()
                        P.op("dve", lambda e, lo=lo, mid=mid, k=k: e.tensor_tensor(out=mid[:], in0=lo[:], in1=Wk[:, k:k + 1], op=ALU.add), reads=[lon, "Wk"], writes=[midn])
                        P.op("dve", lambda e, mid=mid, cnt=cnt, L=L: e.tensor_scalar(dbias[:, 0:L], acc[:, 0:L], mid[:, 0:1], 0.0, op0=ALU.is_ge, op1=ALU.add, accum_out=cnt[:]),
                             reads=["acc", midn], writes=[dbn + "lo", cntn])
                        P.op("dve", lambda e, cnt=cnt, tt=tt, k=k: e.scalar_tensor_tensor(out=tt[:], in0=cnt[:], scalar=255.5, in1=Wk[:, k:k + 1], op0=ALU.is_ge, op1=ALU.mult), reads=[cntn, "Wk"], writes=[ttn])
                        P.op("dve", lambda e, lo=lo, lo2=lo2, tt=tt: e.tensor_tensor(out=lo2[:], in0=lo[:], in1=tt[:], op=ALU.add), reads=[lon, ttn], writes=[lo2n])
                        lo, lon = lo2, lo2n
                        yield
                    P.op("dve", lambda e, lo=lo, L=L: e.tensor_scalar(dbias[:, 0:L], acc[:, 0:L], lo[:, 0:1], NEG, op0=ALU.is_lt, op1=ALU.mult), reads=["acc", lon], writes=[dbn, dbn + "lo", dbn + "hi"])

                tst[qt] = (b, sbT, sbTn, dbias, dbn)
                yield

            def att(G, j, inter=None, nsteps=1):
                qt = 4 * G + j
                nch = qt + 1
                gk = G if kind == "moba" else qt
                if gk not in grp:
                    load_att(gk)
                qpad, qpn = grp[gk]
                jo = j if kind == "moba" else 0
                b, sbT, sbTn, dbias, dbn = tst.pop(qt)
                osb, osbn = osbs.next()
                units = []
                for h in range(6):
                    for c0 in range(0, nch, 4):
                        units.append((h, list(range(c0, min(c0 + 4, nch)))))

                def emitS(h, cs):
                    sp_, spn = Sps.next()
                    for ci, c in enumerate(cs):
                        if kind == "moba":
                            blk = c // 2
                            if blk < b:
                                bias = (Eall[:, blk, :], sbT[:, h, :], ["Eall", sbTn])
                            elif c == qt:
                                bias = (tri[:], identb[:], ["tri", "identb"])
                            else:
                                bias = None
                        else:
                            bias = (dbias[:, c * 128:(c + 1) * 128], identb[:], [dbn, "identb"])
                        P.op("pe", lambda e, sp_=sp_, ci=ci, c=c, h=h, jo=jo, qpad=qpad, bias=bias: e.matmul(sp_[:, ci, :], kT[:, h // 2, c * 128:(c + 1) * 128], qpad[:, h, jo * 128:(jo + 1) * 128], start=True, stop=(bias is None)),
                             reads=["kT", qpn], writes=[spn])
                        if bias is not None:
                            P.op("pe", lambda e, sp_=sp_, ci=ci, bias=bias: e.matmul(sp_[:, ci, :], bias[0], bias[1], start=False, stop=True),
                                 reads=bias[2], writes=[spn])
                    return sp_, spn

                pend = emitS(*units[0])
                ops_ = opn = None
                sdone = 0
                for ui, (h, cs) in enumerate(units):
                    if inter is not None:
                        want = ((ui + 1) * nsteps + len(units) - 1) // len(units)
                        while sdone < want:
                            next(inter, None)
                            sdone += 1
                    sp_, spn = pend
                    if ui + 1 < len(units):
                        pend = emitS(*units[ui + 1])
                    if cs[0] == 0:
                        ops_, opn = Ops.next()
                    pt_, ptn = PTs.next()
                    ncs = len(cs)
                    P.op("act", lambda e, sp_=sp_, pt_=pt_, ncs=ncs: e.activation(pt_[:, 0:ncs, :], sp_[:, 0:ncs, :], AF.Exp, scale=0.125), reads=[], writes=[spn, ptn])
                    for ci, c in enumerate(cs):
                        P.op("pe", lambda e, ops_=ops_, pt_=pt_, ci=ci, c=c, h=h: e.matmul(ops_[:, 0:65], pt_[:, ci, :], V[:, c, h * 65:(h + 1) * 65], start=(c == 0), stop=(c == nch - 1)),
                             reads=[ptn, "V"], writes=[opn])
                    if cs[-1] == nch - 1:
                        if h % 2 == 0:
                            P.op("dve", lambda e, ops_=ops_, osb=osb, h=h: e.tensor_copy(osb[:, h, :], ops_[:, 0:65]), reads=[], writes=[opn, osbn])
                        else:
                            P.op("act", lambda e, ops_=ops_, osb=osb, h=h: e.copy(osb[:, h, :], ops_[:, 0:65]), reads=[], writes=[opn, osbn])
                P.op("dve", lambda e, osb=osb: e.reciprocal(rden[:], osb[:, :, 64]), reads=[osbn], writes=["rden"])
                P.op("dve", lambda e, osb=osb: e.tensor_tensor(out=ym[:], in0=osb[:, :, 0:64], in1=rden[:, :, None].to_broadcast([128, 6, 64]), op=ALU.mult), reads=[osbn, "rden"], writes=["ym"])
                P.op("act", lambda e: e.activation(junk2[:], ym[:].rearrange("p h d -> p (h d)"), AF.Square, accum_out=sst[:, 0:1]), reads=["ym"], writes=["junk2", "sst2"])
                P.op("act", lambda e: e.activation(sst[:, 1:2], sst[:, 0:1], AF.Sqrt, bias=1e-6, scale=1.0 / 384), reads=["sst2"], writes=["sst2b"])
                P.op("dve", lambda e: e.reciprocal(sst[:, 1:2], sst[:, 1:2]), reads=["sst2b"], writes=["sst2b"])
                mo, mon = mixo.next()
                P.op("dve", lambda e, mo=mo: e.scalar_tensor_tensor(out=mo[:], in0=ym[:].rearrange("p h d -> p (h d)"), scalar=sst[:, 1:2], in1=gB[:], op0=ALU.mult, op1=ALU.mult), reads=["ym", "sst2b", "gB"], writes=[mon])
                c0m = 0 if kind == "moba" else 640
                P.dma("sp", scr["mix"][qt * 128:(qt + 1) * 128, c0m:c0m + 384], mo[:], reads=[mon], writes=["s_mix"])

            tiles = [(G, j) for G in range(NG) for j in range(4)]
            for _ in pre(*tiles[0]):
                pass
            for ti, (G, j) in enumerate(tiles):
                if ti + 1 < len(tiles):
                    gen = pre(*tiles[ti + 1])
                    nst = (4 * tiles[ti + 1][0] + tiles[ti + 1][1] + 1 + 3) // 4
                    if kind == "moba":
                        for _ in gen:
                            pass
                        att(G, j)
                    else:
                        next(gen, None)
                        att(G, j, gen, nst + NIT + 1)
                        for _ in gen:
                            pass
                else:
                    att(G, j)
            P.phase_end()


        NBLK = 2 * S // 512 + 32
        x_src = x_in if l == 0 else xbuf[(l - 1) % 2]
        x_dst = xbuf[l % 2]
        P.phase_begin()
        SEL1 = P.sb("SEL1", [128, NT, 32], F32)
        SEL2 = P.sb("SEL2", [128, NT, 32], F32)
        GATES = P.sb("GATES", [128, NT, 2], F32)
        P.phase_begin()
        ub = P.sb("ub", [128, 2, S + 32], BF16)
        for hh in range(2):
            P.dma("sp", ub[:, hh, :], scr["u"][hh * 128:(hh + 1) * 128, :], reads=["s_u"], writes=["ub"])
        cw = P.sb("cw", [128, 2, 31], F32)
        P.dma("sp", cw[:], conv_wT[l], writes=["cw"])
        dgw = P.sb("dgw", [128, 2, 31, 128], BF16)
        for hh in range(2):
            for jj in range(31):
                P.op("dve" if jj % 2 else "pool", lambda e, hh=hh, jj=jj: e.tensor_scalar(dgw[:, hh, jj, :], identf[:], cw[:, hh, jj:jj + 1], None, op0=ALU.mult),
                     reads=["identf", "cw"], writes=["dgw"])
        woB = P.sb("woB", [128, KC, D], BF16)
        P.dma("pool", woB[:], w_out[l].rearrange("(kc p) n -> p kc n", p=128), writes=["woB"])
        cbB = P.sb("cbB", [128, 256], F32)
        lgB = P.sb("lgB", [128, 256], F32)
        lbB = P.sb("lbB", [128, 256], F32)
        g1B = P.sb("g1B", [128, D], F32)
        gm2B = P.sb("gm2B", [128, D], F32)
        sh2B = P.sb("sh2B", [128, D], F32)
        n2B = P.sb("n2B", [128, D], F32)
        wr = P.sb("wr", [128, KC, 36], F32)
        rbB = P.sb("rbB", [128, 36], F32)
        P.dma("sp", cbB[:], conv_bB[l], writes=["cbB"])
        P.dma("sp", lgB[:], ln_gB[l], writes=["lgB"])
        P.dma("sp", lbB[:], ln_bB[l], writes=["lbB"])
        P.dma("sp", g1B[:], d_modB[:, 2 * D:3 * D], reads=["s_modB"], writes=["g1B"])
        P.dma("sp", sh2B[:], d_modB[:, 3 * D:4 * D], reads=["s_modB"], writes=["sh2B"])
        P.dma("sp", gm2B[:], d_modB[:, 4 * D:5 * D], reads=["s_modB"], writes=["gm2B"])
        P.dma("sp", n2B[:], n2gB[l], writes=["n2B"])
        P.dma("sp", wr[:], rw[l].rearrange("(kc p) n -> p kc n", p=128), writes=["wr"])
        P.dma("sp", rbB[:], rbBin[l], writes=["rbB"])
        P.op("dve", lambda e: e.scalar_tensor_tensor(out=gm2B[:], in0=gm2B[:], scalar=1.0, in1=n2B[:], op0=ALU.add, op1=ALU.mult), reads=["n2B"], writes=["gm2B"])
        pc = Rot([(P.ps("pc", [128, 512], F32), "pc%d" % i) for i in range(2)])
        pT4 = Rot([(P.ps("pT4", [128, KC, 128], BF16), "pT4%d" % i) for i in range(1)])
        po = Rot([(P.ps("po", [128, 512], F32), "po%d" % i) for i in range(2)])
        pf = Rot([(P.ps("pf", [128, 4, 128], F32), "pf%d" % i) for i in range(2)])
        pl = P.ps("pl", [128, 512], F32)
        ycs = Rot([(P.sb("yc", [128, 256], F32), "yc%d" % i) for i in range(2)])
        st4 = P.sb("st4", [128, 16], F32)
        junk4 = P.sb("junk4", [128, D], F32)
        mixt = Rot([(P.sb("mixt", [128, D], BF16), "mixt%d" % i) for i in range(2)])
        mixT = P.sb("mixT", [128, KC, 128], BF16)
        xts = Rot([(P.sb("xt4", [128, D], F32), "xt4%d" % i) for i in range(2)])
        xms = Rot([(P.sb("xm", [128, D], F32), "xm%d" % i) for i in range(2)])
        h2fs = Rot([(P.sb("h2f", [128, D], F32), "h2f%d" % i) for i in range(2)])
        h2bs = Rot([(P.sb("h2b", [128, D], BF16), "h2b%d" % i) for i in range(2)])
        h2T = P.sb("h2T", [128, KC, 128], F32)
        lg = P.sb("lg", [128, 36], F32)
        rt = P.sb("rt", [128, 96], F32)
        def tile_gen(i):
            r0 = i * 128
            mt, mtn = mixt.next()
            xt_, xtn = xts.next()
            P.dma("sp", mt[:, 0:384], scr["mix"][r0:r0 + 128, 0:384], reads=["s_mix"], writes=[mtn])
            P.dma("sp", mt[:, 640:1024], scr["mix"][r0:r0 + 128, 640:1024], reads=["s_mix"], writes=[mtn])
            P.dma("sp", xt_[:], x_src[r0:r0 + 128, :], reads=["xsrc"], writes=[xtn])
            pct, pcn = pc.next()
            for hh in range(2):
                for jj in range(31):
                    P.op("pe", lambda e, pct=pct, hh=hh, jj=jj, r0=r0: e.matmul(pct[:, hh * 128:(hh + 1) * 128], ub[:, hh, r0 + 2 + jj:r0 + 2 + jj + 128], dgw[:, hh, jj, :], start=(jj == 0), stop=(jj == 30)),
                         reads=["ub", "dgw"], writes=[pcn])
            yc, ycn = ycs.next()
            P.op("dve", lambda e, pct=pct, yc=yc: e.tensor_tensor(out=yc[:], in0=pct[:, 0:256], in1=cbB[:], op=ALU.add), reads=["cbB"], writes=[pcn, ycn])
            yield
            P.op("dve", lambda e: e.tensor_reduce(out=st4[:, 0:1], in_=yc[:], axis=AX.X, op=ALU.add), reads=[ycn], writes=["st4a"])
            P.op("dve", lambda e: e.tensor_scalar(st4[:, 1:2], st4[:, 0:1], -1.0 / 256, None, op0=ALU.mult), reads=["st4a"], writes=["st4b"])
            P.op("dve", lambda e: e.tensor_scalar(yc[:], yc[:], st4[:, 1:2], None, op0=ALU.add), reads=["st4b"], writes=[ycn])
            P.op("act", lambda e: e.activation(junk4[:, 0:256], yc[:], AF.Square, accum_out=st4[:, 2:3]), reads=[ycn], writes=["junk4", "st4c"])
            P.op("act", lambda e: e.activation(st4[:, 3:4], st4[:, 2:3], AF.Sqrt, bias=1e-6, scale=1.0 / 256), reads=["st4c"], writes=["st4d"])
            P.op("dve", lambda e: e.reciprocal(st4[:, 3:4], st4[:, 3:4]), reads=["st4d"], writes=["st4d"])
            P.op("dve", lambda e: e.scalar_tensor_tensor(out=yc[:], in0=yc[:], scalar=st4[:, 3:4], in1=lgB[:], op0=ALU.mult, op1=ALU.mult), reads=["st4d", "lgB"], writes=[ycn])
            P.op("dve", lambda e: e.tensor_tensor(out=yc[:], in0=yc[:], in1=lbB[:], op=ALU.add), reads=["lbB"], writes=[ycn])
            P.op("act", lambda e, mt=mt: e.activation(mt[:, 384:640], yc[:], AF.Silu), reads=[ycn], writes=[mtn])
            ptt, ptn = pT4.next()
            for kc in range(KC):
                P.op("pe", lambda e, kc=kc, mt=mt, ptt=ptt: e.transpose(ptt[:, kc, :], mt[:, kc * 128:(kc + 1) * 128], identb[:]), reads=[mtn, "identb"], writes=[ptn])
            P.op("act", lambda e, ptt=ptt: e.copy(mixT[:], ptt[:]), reads=[], writes=[ptn, "mixT"])
            xm, xmn = xms.next()
            for hf in range(2):
                pot, pon = po.next()
                for kc in range(KC):
                    P.op("pe", lambda e, kc=kc, pot=pot, hf=hf: e.matmul(pot[:], mixT[:, kc, :], woB[:, kc, hf * 512:(hf + 1) * 512], start=(kc == 0), stop=(kc == KC - 1)),
                         reads=["mixT", "woB"], writes=[pon])
                P.op("dve", lambda e, pot=pot, hf=hf, xm=xm: e.tensor_tensor(out=xm[:, hf * 512:(hf + 1) * 512], in0=pot[:], in1=g1B[:, hf * 512:(hf + 1) * 512], op=ALU.mult),
                     reads=["g1B"], writes=[pon, xmn])
            P.op("pool", lambda e, xm=xm, xt_=xt_: e.tensor_tensor(out=xm[:], in0=xm[:], in1=xt_[:], op=ALU.add), reads=[xtn], writes=[xmn])
            P.dma("sp", scr["xmid"][r0:r0 + 128, :], xm[:], reads=[xmn], writes=["s_xmid"])
            P.op("act", lambda e, xm=xm: e.activation(junk4[:], xm[:], AF.Square, accum_out=st4[:, 4:5]), reads=[xmn], writes=["junk4", "st4e"])
            P.op("act", lambda e: e.activation(st4[:, 5:6], st4[:, 4:5], AF.Sqrt, bias=1e-6, scale=1.0 / D), reads=["st4e"], writes=["st4f"])
            P.op("dve", lambda e: e.reciprocal(st4[:, 5:6], st4[:, 5:6]), reads=["st4f"], writes=["st4f"])
            h2f, h2fn = h2fs.next()
            h2b, h2bn = h2bs.next()
            P.op("dve", lambda e, h2f=h2f, xm=xm: e.scalar_tensor_tensor(out=h2f[:], in0=xm[:], scalar=st4[:, 5:6], in1=gm2B[:], op0=ALU.mult, op1=ALU.mult), reads=[xmn, "st4f", "gm2B"], writes=[h2fn])
            P.op("pool", lambda e, h2f=h2f: e.tensor_tensor(out=h2f[:], in0=h2f[:], in1=sh2B[:], op=ALU.add), reads=["sh2B"], writes=[h2fn])
            P.op("act", lambda e, h2f=h2f, h2b=h2b: e.copy(h2b[:], h2f[:]), reads=[h2fn], writes=[h2bn])
            P.dma("sp", scr["h2"][r0:r0 + 128, :], h2b[:], reads=[h2bn], writes=["s_h2"])
            yield
            for q4 in range(2):
                pft, pfn = pf.next()
                for k4 in range(4):
                    kc = q4 * 4 + k4
                    P.op("pe", lambda e, pft=pft, k4=k4, kc=kc, h2f=h2f: e.transpose(pft[:, k4, :], h2f[:, kc * 128:(kc + 1) * 128], identf[:]), reads=[h2fn, "identf"], writes=[pfn])
                if q4 == 0:
                    P.op("act", lambda e, pft=pft, q4=q4: e.copy(h2T[:, q4 * 4:(q4 + 1) * 4, :], pft[:]), reads=[], writes=[pfn, "h2T"])
                else:
                    P.op("dve", lambda e, pft=pft, q4=q4: e.tensor_copy(h2T[:, q4 * 4:(q4 + 1) * 4, :], pft[:]), reads=[], writes=[pfn, "h2T"])
            for kc in range(KC):
                P.op("pe", lambda e, kc=kc: e.matmul(pl[:, 0:36], h2T[:, kc, :], wr[:, kc, :], start=(kc == 0), stop=(kc == KC - 1)), reads=["h2T", "wr"], writes=["pl"])
            P.op("dve", lambda e: e.tensor_tensor(out=lg[:], in0=pl[:, 0:36], in1=rbB[:], op=ALU.add), reads=["rbB"], writes=["pl", "lg"])
            V_ = lambda a, b: rt[:, a:b]
            P.op("dve", lambda e: e.tensor_reduce(out=V_(0, 1), in_=lg[:, 0:4], axis=AX.X, op=ALU.max), reads=["lg"], writes=["rt0"])
            P.op("dve", lambda e: e.tensor_scalar(V_(1, 2), V_(0, 1), -1.0, None, op0=ALU.mult), reads=["rt0"], writes=["rt1"])
            P.op("act", lambda e: e.activation(V_(40, 44), lg[:, 0:4], AF.Exp, bias=V_(1, 2), accum_out=V_(2, 3)), reads=["lg", "rt1"], writes=["rt40", "rt2"])
            P.op("dve", lambda e: e.reciprocal(V_(3, 4), V_(2, 3)), reads=["rt2"], writes=["rt3"])
            P.op("dve", lambda e: e.tensor_scalar(V_(4, 8), lg[:, 0:4], V_(0, 1), None, op0=ALU.is_equal), reads=["lg", "rt0"], writes=["rt4"])
            P.op("dve", lambda e: e.tensor_tensor(out=V_(48, 80).rearrange("p (g x) -> p g x", x=8), in0=lg[:, 4:36].rearrange("p (g x) -> p g x", x=8), in1=V_(4, 8)[:, :, None].to_broadcast([128, 4, 8]), op=ALU.mult),
                 reads=["lg", "rt4"], writes=["rt48"])
            P.op("dve", lambda e: e.tensor_reduce(out=V_(8, 16), in_=V_(48, 80).rearrange("p (g x) -> p x g", x=8), axis=AX.X, op=ALU.add), reads=["rt48"], writes=["rt8"])
            P.op("dve", lambda e: e.tensor_reduce(out=V_(16, 17), in_=V_(8, 16), axis=AX.X, op=ALU.max), reads=["rt8"], writes=["rt16"])
            P.op("dve", lambda e: e.tensor_scalar(V_(17, 18), V_(16, 17), -1.0, None, op0=ALU.mult), reads=["rt16"], writes=["rt17"])
            P.op("dve", lambda e: e.tensor_scalar(V_(18, 26), V_(8, 16), V_(16, 17), None, op0=ALU.is_equal), reads=["rt8", "rt16"], writes=["rt18"])
            P.op("dve", lambda e: e.scalar_tensor_tensor(out=V_(26, 34), in0=V_(18, 26), scalar=-1e30, in1=V_(8, 16), op0=ALU.mult, op1=ALU.add), reads=["rt18", "rt8"], writes=["rt26"])
            P.op("dve", lambda e: e.tensor_reduce(out=V_(34, 35), in_=V_(26, 34), axis=AX.X, op=ALU.max), reads=["rt26"], writes=["rt34"])
            P.op("dve", lambda e: e.tensor_scalar(V_(80, 88), V_(26, 34), V_(34, 35), None, op0=ALU.is_equal), reads=["rt26", "rt34"], writes=["rt80"])
            P.op("act", lambda e: e.activation(V_(35, 36), V_(34, 35), AF.Exp, bias=V_(17, 18)), reads=["rt34", "rt17"], writes=["rt35"])
            P.op("dve", lambda e: e.tensor_scalar(V_(36, 37), V_(35, 36), 1.0, None, op0=ALU.add), reads=["rt35"], writes=["rt36"])
            P.op("dve", lambda e: e.reciprocal(V_(36, 37), V_(36, 37)), reads=["rt36"], writes=["rt36"])
            P.op("dve", lambda e, i=i: e.tensor_tensor(out=GATES[:, i, 0:1], in0=V_(36, 37), in1=V_(3, 4), op=ALU.mult), reads=["rt36", "rt3"], writes=["GATES"])
            P.op("dve", lambda e, i=i: e.tensor_tensor(out=GATES[:, i, 1:2], in0=V_(3, 4), in1=GATES[:, i, 0:1], op=ALU.subtract), reads=["rt3"], writes=["GATES"])
            P.op("dve", lambda e, i=i: e.tensor_tensor(out=SEL1[:, i, :].rearrange("p (g x) -> p g x", x=8), in0=V_(4, 8)[:, :, None].to_broadcast([128, 4, 8]), in1=V_(18, 26)[:, None, :].to_broadcast([128, 4, 8]), op=ALU.mult),
                 reads=["rt4", "rt18"], writes=["SEL1"])
            P.op("dve", lambda e, i=i: e.tensor_tensor(out=SEL2[:, i, :].rearrange("p (g x) -> p g x", x=8), in0=V_(4, 8)[:, :, None].to_broadcast([128, 4, 8]), in1=V_(80, 88)[:, None, :].to_broadcast([128, 4, 8]), op=ALU.mult),
                 reads=["rt4", "rt80"], writes=["SEL2"])
        gens = [tile_gen(i) for i in range(NT)]
        next(gens[0])
        for i in range(NT):
            if i + 1 < NT:
                next(gens[i + 1])
            next(gens[i])
            next(gens[i], None)
        if "s_sel" in dbg:
            P.dma("sp", d_sel[0], SEL1[:], reads=["SEL1"])
            P.dma("sp", d_sel[1], SEL2[:], reads=["SEL2"])
            P.dma("sp", d_gates, GATES[:], reads=["GATES"])
        P.phase_end()

        P.phase_begin()
        W1I = P.sb("W1I", [128, NBLK, 8], I32)
        W2I = P.sb("W2I", [128, NBLK, 4], I32)
        DESTI = P.sb("DESTI", [128, NT, 2], I32)
        P.phase_begin()
        SELS = P.sb("SELS", [128, NT, 32], F32)
        CUM = P.sb("CUM", [128, NT + 1, 32], F32)
        P.op("dve", lambda e: e.tensor_tensor(out=SELS[:], in0=SEL1[:], in1=SEL2[:], op=ALU.add), reads=["SEL1", "SEL2"], writes=["SELS"])
        P.op("pool", lambda e: e.memset(CUM[:, 0, :], 0.0), writes=["CUM"])
        for i in range(NT):
            P.op("dve", lambda e, i=i: e.tensor_tensor(out=CUM[:, i + 1, :], in0=CUM[:, i, :], in1=SELS[:, i, :], op=ALU.add), reads=["SELS"], writes=["CUM"])
        onesf = P.sb("onesf", [128, 128], F32)
        UT = P.sb("UT", [128, 128], F32)
        P.op("pool", lambda e: e.memset(onesf[:], 1.0), writes=["onesf"])
        P.op("dve", lambda e: e.tensor_scalar(UT[:], io[:], pid[:, 0:1], None, op0=ALU.is_gt), reads=["io", "pid"], writes=["UT"])
        pq = Rot([(P.ps("pq", [128, 512], F32), "pq%d" % i) for i in range(2)])
        pqt, pqn = pq.next()
        P.op("pe", lambda e, pqt=pqt: e.matmul(pqt[:, 0:32], onesf[:], CUM[:, NT, :], start=True, stop=True), reads=["onesf", "CUM"], writes=[pqn])
        ms = P.sb("ms", [128, 512], F32)
        cntE = ms[:, 0:32]
        nblk = ms[:, 32:64]
        pendA = ms[:, 64:96]
        pendB = ms[:, 96:128]
        pst = ms[:, 128:160]
        thr = ms[:, 160:192]
        j32 = ms[:, 192:224]
        NM = S // 512 + 1
        P.op("dve", lambda e, pqt=pqt: e.tensor_copy(cntE, pqt[:, 0:32]), reads=[], writes=[pqn, "cntE"])
        P.op("dve", lambda e: e.tensor_scalar(thr[:, 0:NM], io[:, 0:NM], 512.0, None, op0=ALU.mult), reads=["io"], writes=["thr"])
        P.op("pool", lambda e: e.memset(nblk, 0.0), writes=["nblk"])
        for ex in range(32):
            P.op("dve", lambda e, ex=ex: e.tensor_scalar(j32[:, 0:NM], thr[:, 0:NM], cntE[:, ex:ex + 1], 0.0, op0=ALU.is_lt, op1=ALU.add, accum_out=nblk[:, ex:ex + 1]),
                 reads=["thr", "cntE"], writes=["j32", "nblk"])
        src, srcn, dst, dstn = nblk, "nblk", pendA, "pendA"
        for d_ in (1, 2, 4, 8, 16):
            P.op("dve", lambda e, src=src, dst=dst, d_=d_: e.tensor_copy(dst[:, 0:d_], src[:, 0:d_]), reads=[srcn], writes=[dstn])
            P.op("dve", lambda e, src=src, dst=dst, d_=d_: e.tensor_tensor(out=dst[:, d_:32], in0=src[:, d_:32], in1=src[:, 0:32 - d_], op=ALU.add), reads=[srcn], writes=[dstn])
            if dstn == "pendA":
                src, srcn, dst, dstn = pendA, "pendA", pendB, "pendB"
            else:
                src, srcn, dst, dstn = pendB, "pendB", pendA, "pendA"
        pend, pendn = src, srcn
        P.op("dve", lambda e, pend=pend: e.tensor_tensor(out=pst, in0=pend, in1=nblk, op=ALU.subtract), reads=[pendn, "nblk"], writes=["pst"])
        P.op("dve", lambda e: e.tensor_scalar(pst, pst, 512.0, None, op0=ALU.mult), reads=[], writes=["pst"])
        BE = P.sb("BE", [128, NBLK], F32)
        P.op("pool", lambda e: e.memset(BE[:], 0.0), writes=["BE"])
        for b in range(NBLK):
            P.op("dve", lambda e, b=b, pend=pend: e.tensor_scalar(j32, pend, float(b), 0.0, op0=ALU.is_le, op1=ALU.add, accum_out=BE[:, b:b + 1]), reads=[pendn], writes=["j32", "BE"])
        P.op("dve", lambda e: e.tensor_scalar(BE[:], BE[:], 31.0, None, op0=ALU.min), reads=[], writes=["BE"])
        iotaK = P.sb("iotaK", [128, 8], F32)
        P.op("dve", lambda e: e.tensor_scalar(iotaK[:], io[:, 0:8], 128.0, pid[:, 0:1], op0=ALU.mult, op1=ALU.add), reads=["io", "pid"], writes=["iotaK"])
        WF = P.sb("WF", [128, NBLK, 8], F32)
        BEs = P.sb("BEs", [128, NBLK], F32)
        SAME = P.sb("SAME", [128, NBLK], F32)
        P.op("pool", lambda e: e.memset(SAME[:], 0.0), writes=["SAME"])
        P.op("dve", lambda e: e.tensor_tensor(out=SAME[:, 1:NBLK], in0=BE[:, 1:NBLK], in1=BE[:, 0:NBLK - 1], op=ALU.is_equal), reads=["BE"], writes=["SAME"])
        P.op("dve", lambda e: e.tensor_scalar(BEs[:], BE[:], 1024.0, float(l * 32 * 1024), op0=ALU.mult, op1=ALU.add), reads=["BE"], writes=["BEs"])
        P.op("dve", lambda e: e.scalar_tensor_tensor(out=BEs[:], in0=SAME[:], scalar=200000.0, in1=BEs[:], op0=ALU.mult, op1=ALU.add), reads=["SAME"], writes=["BEs"])
        P.op("dve", lambda e: e.tensor_tensor(out=WF[:], in0=BEs[:, :, None].to_broadcast([128, NBLK, 8]), in1=iotaK[:, None, :].to_broadcast([128, NBLK, 8]), op=ALU.add), reads=["BEs", "iotaK"], writes=["WF"])
        P.op("dve", lambda e: e.tensor_copy(W1I[:], WF[:]), reads=["WF"], writes=["W1I"])
        P.op("dve", lambda e: e.tensor_scalar(BEs[:], BE[:], 512.0, float(l * 32 * 512), op0=ALU.mult, op1=ALU.add), reads=["BE", "WF"], writes=["BEs"])
        P.op("dve", lambda e: e.scalar_tensor_tensor(out=BEs[:], in0=SAME[:], scalar=200000.0, in1=BEs[:], op0=ALU.mult, op1=ALU.add), reads=["SAME"], writes=["BEs"])
        P.op("dve", lambda e: e.tensor_tensor(out=WF[:, :, 0:4], in0=BEs[:, :, None].to_broadcast([128, NBLK, 4]), in1=iotaK[:, None, 0:4].to_broadcast([128, NBLK, 4]), op=ALU.add), reads=["BEs", "iotaK", "W1I"], writes=["WF"])
        P.op("dve", lambda e: e.tensor_copy(W2I[:], WF[:, :, 0:4]), reads=["WF"], writes=["W2I"])
        DEST = P.sb("DEST", [128, NT, 2], F32)
        P.op("pool", lambda e: e.memset(DEST[:], 0.0), writes=["DEST"])
        tq = Rot([(P.sb("tq", [128, 32], F32), "tq%d" % i) for i in range(2)])
        for i in range(NT):
            pqt, pqn = pq.next()
            tqt, tqn = tq.next()
            P.op("pe", lambda e, pqt=pqt, i=i: e.matmul(pqt[:, 0:32], UT[:], SELS[:, i, :], start=True, stop=False), reads=["UT", "SELS"], writes=[pqn])
            P.op("pe", lambda e, pqt=pqt, i=i: e.matmul(pqt[:, 0:32], onesf[:], CUM[:, i, :], start=False, stop=True), reads=["onesf", "CUM"], writes=[pqn])
            P.op("dve", lambda e, pqt=pqt, tqt=tqt: e.tensor_tensor(out=tqt[:], in0=pqt[:, 0:32], in1=pst, op=ALU.add), reads=["pst"], writes=[pqn, tqn])
            for sl, SEL, seln in ((0, SEL1, "SEL1"), (1, SEL2, "SEL2")):
                P.op("dve", lambda e, tqt=tqt, i=i, sl=sl, SEL=SEL: e.scalar_tensor_tensor(out=j32, in0=tqt[:], scalar=1.0, in1=SEL[:, i, :], op0=ALU.mult, op1=ALU.mult, accum_out=DEST[:, i, sl:sl + 1]),
                     reads=[tqn, seln], writes=["j32", "DEST"])
        P.op("dve", lambda e: e.tensor_copy(DESTI[:], DEST[:]), reads=["DEST"], writes=["DESTI"])
        if "s_dest" in dbg:
            P.dma("sp", d_dest, DESTI[:], reads=["DESTI"])
            P.dma("sp", d_be, BE[:], reads=["BE"])
        zb = P.sb("zb", [128, 4, D], BF16)
        P.op("pool", lambda e: e.memset(zb[:], 0.0), writes=["zb"])
        for b in range(NBLK):
            P.dma("sp", scr["buf"][b * 512:(b + 1) * 512, :].rearrange("(s p) d -> p s d", p=128), zb[:], reads=["zb"], writes=["s_buf"])
        h2l = Rot([(P.sb("h2l", [128, D], BF16), "h2l%d" % i) for i in range(3)])
        for i in range(NT):
            ht, htn = h2l.next()
            P.dma("sp", ht[:], scr["h2"][i * 128:(i + 1) * 128, :], reads=["s_h2"], writes=[htn])
            for sl in range(2):
                P.op("pool", lambda e, ht=ht, i=i, sl=sl: e.indirect_dma_start(out=scr["buf"], out_offset=bass.IndirectOffsetOnAxis(ap=DESTI[:, i, sl:sl + 1], axis=0), in_=ht[:], in_offset=None),
                     reads=[htn, "DESTI"], writes=["s_buf"], dma=True)
        P.phase_end()
        P.phase_begin()
        w1v = w1
        w3v = w3
        w2v = w2
        w1f = Rot([(P.sb("w1f", [128, KC, 512], F32), "w1f%d" % i) for i in range(1)])
        w3f = Rot([(P.sb("w3f", [128, KC, 512], F32), "w3f%d" % i) for i in range(1)])
        w2f = Rot([(P.sb("w2f", [128, 4, D], F32), "w2f%d" % i) for i in range(1)])
        w1b = Rot([(P.sb("w1b", [128, KC, 512], BF16), "w1b%d" % i) for i in range(2)])
        w3b = Rot([(P.sb("w3b", [128, KC, 512], BF16), "w3b%d" % i) for i in range(2)])
        w2b = Rot([(P.sb("w2b", [128, 4, D], BF16), "w2b%d" % i) for i in range(2)])
        hbs = Rot([(P.sb("hb", [128, 4, D], BF16), "hb%d" % i) for i in range(2)])
        hTs = Rot([(P.sb("hT", [128, KC, 512], BF16), "hT%d" % i) for i in range(2)])
        sgs = Rot([(P.sb("sg", [128, 512], F32), "sg%d" % i) for i in range(2)])
        aTs = Rot([(P.sb("aT", [128, 4, 512], BF16), "aT%d" % i) for i in range(2)])
        ybs = Rot([(P.sb("yb", [128, 512], F32), "yb%d" % i) for i in range(4)])
        pT5 = Rot([(P.ps("pT5", [128, KC, 128], BF16), "pT5%d" % i) for i in range(2)])
        ph1 = Rot([(P.ps("ph1", [128, 512], F32), "ph1%d" % i) for i in range(1)])
        ph3 = Rot([(P.ps("ph3", [128, 512], F32), "ph3%d" % i) for i in range(1)])
        py = Rot([(P.ps("py", [128, 512], F32), "py%d" % i) for i in range(2)])
        for b in range(NBLK):
            a1, a1n = w1f.next()
            a3, a3n = w3f.next()
            a2, a2n = w2f.next()
            for kc in range(KC):
                P.op("pool", lambda e, a1=a1, b=b, kc=kc: e.indirect_dma_start(out=a1[:, kc, :], out_offset=None, in_=w1v, in_offset=bass.IndirectOffsetOnAxis(ap=W1I[:, b, kc:kc + 1], axis=0), bounds_check=_breg(e, NL * 32 * 1024 - 1), oob_is_err=False),
                     reads=["W1I"], writes=[a1n], dma=True)
                P.op("pool", lambda e, a3=a3, b=b, kc=kc: e.indirect_dma_start(out=a3[:, kc, :], out_offset=None, in_=w3v, in_offset=bass.IndirectOffsetOnAxis(ap=W1I[:, b, kc:kc + 1], axis=0), bounds_check=_breg(e, NL * 32 * 1024 - 1), oob_is_err=False),
                     reads=["W1I"], writes=[a3n], dma=True)
            for dc in range(4):
                P.op("pool", lambda e, a2=a2, b=b, dc=dc: e.indirect_dma_start(out=a2[:, dc, :], out_offset=None, in_=w2v, in_offset=bass.IndirectOffsetOnAxis(ap=W2I[:, b, dc:dc + 1], axis=0), bounds_check=_breg(e, NL * 32 * 512 - 1), oob_is_err=False),
                     reads=["W2I"], writes=[a2n], dma=True)
            hb, hbn = hbs.next()
            P.dma("sp", hb[:], scr["buf"][b * 512:(b + 1) * 512, :].rearrange("(s p) d -> p s d", p=128), reads=["s_buf"], writes=[hbn])
            b1, b1n = w1b.next()
            b3, b3n = w3b.next()
            b2, b2n = w2b.next()
            P.op("act", lambda e, a1=a1, b1=b1: e.copy(b1[:], a1[:]), reads=[a1n], writes=[b1n])
            P.op("dve", lambda e, a3=a3, b3=b3: e.tensor_copy(b3[:], a3[:]), reads=[a3n], writes=[b3n])
            P.op("dve", lambda e, a2=a2, b2=b2: e.tensor_copy(b2[:], a2[:]), reads=[a2n], writes=[b2n])
            hT, hTn = hTs.next()
            for sub in range(4):
                ptt, ptn = pT5.next()
                for kc in range(KC):
                    P.op("pe", lambda e, ptt=ptt, hb=hb, sub=sub, kc=kc: e.transpose(ptt[:, kc, :], hb[:, sub, kc * 128:(kc + 1) * 128], identb[:]), reads=[hbn, "identb"], writes=[ptn])
                if sub % 2 == 0:
                    P.op("act", lambda e, ptt=ptt, hT=hT, sub=sub: e.copy(hT[:, :, sub * 128:(sub + 1) * 128], ptt[:]), reads=[], writes=[ptn, hTn])
                else:
                    P.op("dve", lambda e, ptt=ptt, hT=hT, sub=sub: e.tensor_copy(hT[:, :, sub * 128:(sub + 1) * 128], ptt[:]), reads=[], writes=[ptn, hTn])
            aT, aTn = aTs.next()
            for dc in range(4):
                p1, p1n = ph1.next()
                p3, p3n = ph3.next()
                sg, sgn = sgs.next()
                for kc in range(KC):
                    P.op("pe", lambda e, p1=p1, b1=b1, hT=hT, kc=kc, dc=dc: e.matmul(p1[:], b1[:, kc, dc * 128:(dc + 1) * 128], hT[:, kc, :], start=(kc == 0), stop=(kc == KC - 1)), reads=[b1n, hTn], writes=[p1n])
                for kc in range(KC):
                    P.op("pe", lambda e, p3=p3, b3=b3, hT=hT, kc=kc, dc=dc: e.matmul(p3[:], b3[:, kc, dc * 128:(dc + 1) * 128], hT[:, kc, :], start=(kc == 0), stop=(kc == KC - 1)), reads=[b3n, hTn], writes=[p3n])
                P.op("act", lambda e, p1=p1, sg=sg: e.activation(sg[:], p1[:], AF.Silu), reads=[], writes=[p1n, sgn])
                P.op("dve", lambda e, p3=p3, sg=sg, aT=aT, dc=dc: e.tensor_tensor(out=aT[:, dc, :], in0=p3[:], in1=sg[:], op=ALU.mult), reads=[sgn], writes=[p3n, aTn])
            for sub in range(4):
                for hf in range(2):
                    pyt, pyn = py.next()
                    yb, ybn = ybs.next()
                    for dc in range(4):
                        P.op("pe", lambda e, pyt=pyt, aT=aT, b2=b2, dc=dc, sub=sub, hf=hf: e.matmul(pyt[:], aT[:, dc, sub * 128:(sub + 1) * 128], b2[:, dc, hf * 512:(hf + 1) * 512], start=(dc == 0), stop=(dc == 3)), reads=[aTn, b2n], writes=[pyn])
                    if hf == 0:
                        P.op("act", lambda e, pyt=pyt, yb=yb: e.copy(yb[:], pyt[:]), reads=[], writes=[pyn, ybn])
                    else:
                        P.op("dve", lambda e, pyt=pyt, yb=yb: e.tensor_copy(yb[:], pyt[:]), reads=[], writes=[pyn, ybn])
                    P.dma("sp", scr["ybuf"][b * 512 + sub * 128:b * 512 + (sub + 1) * 128, hf * 512:(hf + 1) * 512], yb[:], reads=[ybn], writes=["s_ybuf"])
        P.phase_end()
        P.phase_begin()
        g2B = P.sb("g2B", [128, D], F32)
        fgB = P.sb("fgB", [128, D], F32)
        P.dma("sp", g2B[:], d_modB[:, 5 * D:6 * D], reads=["s_modB"], writes=["g2B"])
        P.dma("sp", fgB[:], final_gB, writes=["fgB"])
        y1s = Rot([(P.sb("y1", [128, D], F32), "y1%d" % i) for i in range(2)])
        y2s = Rot([(P.sb("y2", [128, D], F32), "y2%d" % i) for i in range(2)])
        xls = Rot([(P.sb("xl", [128, D], F32), "xl%d" % i) for i in range(2)])
        xos = Rot([(P.sb("xo", [128, D], F32), "xo%d" % i) for i in range(2)])
        junk5 = P.sb("junk5", [128, D], F32)
        st5 = P.sb("st5", [128, 4], F32)
        last = (l == NL - 1)
        for i in range(NT):
            r0 = i * 128
            y1, y1n = y1s.next()
            y2, y2n = y2s.next()
            xl, xln = xls.next()
            xo, xon = xos.next()
            P.op("pool", lambda e, y1=y1, i=i: e.indirect_dma_start(out=y1[:], out_offset=None, in_=scr["ybuf"], in_offset=bass.IndirectOffsetOnAxis(ap=DESTI[:, i, 0:1], axis=0)),
                 reads=["DESTI", "s_ybuf"], writes=[y1n], dma=True)
            P.op("pool", lambda e, y2=y2, i=i: e.indirect_dma_start(out=y2[:], out_offset=None, in_=scr["ybuf"], in_offset=bass.IndirectOffsetOnAxis(ap=DESTI[:, i, 1:2], axis=0)),
                 reads=["DESTI", "s_ybuf"], writes=[y2n], dma=True)
            P.dma("sp", xl[:], scr["xmid"][r0:r0 + 128, :], reads=["s_xmid"], writes=[xln])
            P.op("dve", lambda e, y1=y1, i=i: e.tensor_scalar(y1[:], y1[:], GATES[:, i, 0:1], None, op0=ALU.mult), reads=["GATES"], writes=[y1n])
            P.op("dve", lambda e, y1=y1, y2=y2, i=i: e.scalar_tensor_tensor(out=y1[:], in0=y2[:], scalar=GATES[:, i, 1:2], in1=y1[:], op0=ALU.mult, op1=ALU.add), reads=["GATES", y2n], writes=[y1n])
            P.op("pool", lambda e, y1=y1: e.tensor_tensor(out=y1[:], in0=y1[:], in1=g2B[:], op=ALU.mult), reads=["g2B"], writes=[y1n])
            P.op("pool", lambda e, y1=y1, xl=xl, xo=xo: e.tensor_tensor(out=xo[:], in0=y1[:], in1=xl[:], op=ALU.add), reads=[y1n, xln], writes=[xon])
            if not last:
                P.dma("sp", x_dst[r0:r0 + 128, :], xo[:], reads=[xon], writes=["xdst"])
            else:
                P.op("act", lambda e, xo=xo: e.activation(junk5[:], xo[:], AF.Square, accum_out=st5[:, 0:1]), reads=[xon], writes=["junk5", "st5a"])
                P.op("act", lambda e: e.activation(st5[:, 1:2], st5[:, 0:1], AF.Sqrt, bias=1e-6, scale=1.0 / D), reads=["st5a"], writes=["st5b"])
                P.op("dve", lambda e: e.reciprocal(st5[:, 1:2], st5[:, 1:2]), reads=["st5b"], writes=["st5b"])
                P.op("dve", lambda e, xo=xo: e.scalar_tensor_tensor(out=xo[:], in0=xo[:], scalar=st5[:, 1:2], in1=fgB[:], op0=ALU.mult, op1=ALU.mult), reads=["st5b", "fgB"], writes=[xon])
                P.dma("sp", y_out[r0:r0 + 128, :], xo[:], reads=[xon], writes=["y"])
        P.phase_end()
        P.phase_end()
        P.phase_end()

    P.emit()
    return nc, P


def prep_inputs(inp, b, NL):
    f = lambda a: np.ascontiguousarray(np.asarray(a, dtype=np.float32))
    rep = lambda a: f(np.broadcast_to(np.asarray(a)[:, None, :], (a.shape[0], 128, a.shape[1])))
    d = {}
    d["x"] = f(inp["x"][b])
    d["c_col"] = f(np.asarray(inp["c"][b]).reshape(8, 128).T)
    d["ada_w"] = f(inp["ada_w"][:NL])
    d["ada_bB"] = rep(inp["ada_b"][:NL])
    d["n1g_col"] = f(np.asarray(inp["norm1_g"][:NL]).reshape(NL, 8, 128).transpose(0, 2, 1))
    d["w_in"] = f(inp["w_in"][:NL])
    d["conv_wT"] = f(np.asarray(inp["conv_w"][:NL]).transpose(0, 2, 1).reshape(NL, 2, 128, 31).transpose(0, 2, 1, 3))
    d["conv_bB"] = rep(inp["conv_b"][:NL])
    d["ln_gB"] = rep(inp["conv_ln_g"][:NL])
    d["ln_bB"] = rep(inp["conv_ln_b"][:NL])
    d["w_out"] = f(inp["w_out"][:NL])
    d["n2gB"] = rep(inp["norm2_g"][:NL])
    d["rw"] = f(np.concatenate([inp["router_group_w"][:NL], inp["router_expert_w"][:NL]], axis=-1))
    d["rbB"] = rep(np.concatenate([inp["router_group_b"][:NL], inp["router_expert_b"][:NL]], axis=-1))
    d["w1"] = f(np.asarray(inp["expert_w1"][:NL]).reshape(NL * 32 * 1024, 512))
    d["w3"] = f(np.asarray(inp["expert_w3"][:NL]).reshape(NL * 32 * 1024, 512))
    d["w2"] = f(np.asarray(inp["expert_w2"][:NL]).reshape(NL * 32 * 512, 1024))
    d["final_gB"] = f(np.broadcast_to(np.asarray(inp["final_g"])[None, :], (128, 1024)))
    d["mgB"] = rep(inp["moba_norm_g"][:NL])
    d["dgB"] = rep(inp["dsa_norm_g"][:NL])
    return d


_CACHE = {}


def kernel(**inputs):
    S, NL, NB_ = 8192, 2, 4
    if "nc" not in _CACHE:
        _CACHE["nc"] = build(S, NL)[0]
    nc = _CACHE["nc"]
    shared = prep_inputs(inputs, 0, NL)
    in_maps = []
    for b in range(NB_):
        d = dict(shared)
        d["x"] = np.ascontiguousarray(np.asarray(inputs["x"][b], dtype=np.float32))
        d["c_col"] = np.ascontiguousarray(np.asarray(inputs["c"][b], dtype=np.float32).reshape(8, 128).T)
        in_maps.append(d)
    res = run_bass_kernel_spmd(nc, in_maps, core_ids=list(range(NB_)))
    return np.stack([np.asarray(r["y"], dtype=np.float32) for r in res.results], axis=0)
```
